# Optimizing a Trainium2 kernel written in Bass

```python
import jax, jax.numpy as jnp
from jax import lax
import numpy as np

D_MODEL = 2048
BATCH = 2
SEQ = 8192
DEPTH = 4

CONV_W = D_MODEL // 4
CONV_K = 31
FOX_HEAD_DIM = 128
FOX_W = D_MODEL // 2
FOX_HEADS = FOX_W // FOX_HEAD_DIM
Q_BLOCK = 128
LRU_W = D_MODEL // 4
LRU_BLOCKS = 8
LRU_BW = LRU_W // LRU_BLOCKS
LRU_CONV_K = 4
LRU_C = 8.0
D_MIX = CONV_W + FOX_W + LRU_W

OFF_CONV = 0
OFF_Q = OFF_CONV + 2 * CONV_W
OFF_K = OFF_Q + FOX_W
OFF_V = OFF_K + FOX_W
OFF_F = OFF_V + FOX_W
OFF_LRU_X = OFF_F + FOX_HEADS
OFF_LRU_G = OFF_LRU_X + LRU_W
IN_COLS = OFF_LRU_G + LRU_W

N_GROUPS = 4
EXPERTS_PER_GROUP = 8
N_EXPERTS = N_GROUPS * EXPERTS_PER_GROUP
TOP_K = 2
D_FF_EXPERT = D_MODEL // 4
MOE_BLOCK = 128

EPS = 1e-6

kernel_name = 'hymba_style_conv_fox_rglru_hmoe_adaln'


def rms_norm(x, g):
    xf = x.astype(jnp.float32)
    y = xf * lax.rsqrt(jnp.mean(xf * xf, axis=-1, keepdims=True) + EPS)
    return (y * g.astype(jnp.float32)).astype(x.dtype)


def layer_norm(x, g, b):
    xf = x.astype(jnp.float32)
    mu = jnp.mean(xf, axis=-1, keepdims=True)
    var = jnp.mean(jnp.square(xf - mu), axis=-1, keepdims=True)
    y = (xf - mu) * lax.rsqrt(var + EPS) * g.astype(jnp.float32) + b.astype(jnp.float32)
    return y.astype(x.dtype)


def causal_depthwise_conv(x, w, b):
    k = w.shape[0]
    ch = x.shape[-1]
    y = lax.conv_general_dilated(
        x, w[:, None, :].astype(x.dtype), window_strides=(1,), padding=[(k - 1, 0)],
        dimension_numbers=('NWC', 'WIO', 'NWC'), feature_group_count=ch)
    return y + b.astype(x.dtype)


def conformer_conv(u, dw_w, dw_b, ln_g, ln_b):
    a, gate = jnp.split(u, 2, axis=-1)
    y = a * jax.nn.sigmoid(gate)
    y = causal_depthwise_conv(y, dw_w, dw_b)
    y = layer_norm(y, ln_g, ln_b)
    return jax.nn.silu(y)


def forgetting_attention(q, k, v, log_f):
    b, s, h, d = q.shape
    n_blk = s // Q_BLOCK
    scale = d ** -0.5
    cum = jnp.cumsum(log_f, axis=1).transpose(0, 2, 1)
    kh = k.transpose(0, 2, 1, 3)
    vh = v.transpose(0, 2, 1, 3)
    qb = q.reshape(b, n_blk, Q_BLOCK, h, d).transpose(1, 0, 3, 2, 4)
    cqb = cum.reshape(b, h, n_blk, Q_BLOCK).transpose(2, 0, 1, 3)
    kpos = jnp.arange(s)

    def one_block(args):
        qi, ci, blk = args
        logits = jnp.einsum('bhqd,bhkd->bhqk', qi, kh, preferred_element_type=jnp.float32) * scale
        logits = logits + ci[..., :, None] - cum[:, :, None, :]
        qpos = blk * Q_BLOCK + jnp.arange(Q_BLOCK)
        logits = jnp.where(kpos[None, :] <= qpos[:, None], logits, -jnp.inf)
        p = jax.nn.softmax(logits, axis=-1).astype(vh.dtype)
        return jnp.einsum('bhqk,bhkd->bhqd', p, vh)

    o = lax.map(one_block, (qb, cqb, jnp.arange(n_blk)))
    return o.transpose(1, 0, 3, 2, 4).reshape(b, s, h * d)


def rg_lru(u, conv_w, conv_b, w_r, b_r, w_i, b_i, lam):
    b, s, w = u.shape
    xc = causal_depthwise_conv(u, conv_w, conv_b)
    xr = xc.reshape(b, s, LRU_BLOCKS, LRU_BW)
    r = jax.nn.sigmoid((jnp.einsum('bsnc,ncd->bsnd', xr, w_r).reshape(b, s, w) + b_r).astype(jnp.float32))
    i = jax.nn.sigmoid(jnp.einsum('bsnc,ncd->bsnd', xr, w_i).reshape(b, s, w) + b_i)
    log_a = -LRU_C * r * jax.nn.softplus(-lam.astype(jnp.float32))
    a = jnp.exp(log_a)
    bt = jnp.sqrt(-jnp.expm1(2.0 * log_a)) * (i * xc).astype(jnp.float32)

    def combine(left, right):
        a1, b1 = left
        a2, b2 = right
        return a1 * a2, a2 * b1 + b2

    _, h = lax.associative_scan(combine, (a, bt), axis=1)
    return h.astype(u.dtype)


def hierarchical_moe(xn, w_rg, b_rg, w_re, b_re, w_gate, w_up, w_down):
    b, s, d = xn.shape
    t = b * s
    xt = xn.reshape(t, d)
    lg1 = jnp.einsum('td,dg->tg', xt, w_rg, preferred_element_type=jnp.float32) + b_rg.astype(jnp.float32)
    p_grp, grp = lax.top_k(jax.nn.softmax(lg1, axis=-1), 1)
    lg2 = jnp.einsum('td,gde->tge', xt, w_re, preferred_element_type=jnp.float32) + b_re.astype(jnp.float32)
    lg2 = jnp.take_along_axis(lg2, grp[:, :, None], axis=1)[:, 0]
    p_exp, eidx = lax.top_k(jax.nn.softmax(lg2, axis=-1), TOP_K)
    p_exp = p_exp / jnp.sum(p_exp, axis=-1, keepdims=True)
    gates = (p_grp * p_exp).reshape(-1)
    eid = (grp * EXPERTS_PER_GROUP + eidx).reshape(-1)
    tok = jnp.repeat(jnp.arange(t), TOP_K)
    n_assign = t * TOP_K

    order = jnp.argsort(eid)
    eid_s, tok_s, gate_s = eid[order], tok[order], gates[order]
    counts = jnp.bincount(eid, length=N_EXPERTS)
    starts = jnp.cumsum(counts) - counts
    padded = (counts + MOE_BLOCK - 1) // MOE_BLOCK * MOE_BLOCK
    pends = jnp.cumsum(padded)
    pstarts = pends - padded
    dest = pstarts[eid_s] + jnp.arange(n_assign) - starts[eid_s]
    n_blocks = -(-n_assign // MOE_BLOCK) + N_EXPERTS
    n_rows = n_blocks * MOE_BLOCK
    row_tok = jnp.zeros((n_rows,), jnp.int32).at[dest].set(tok_s.astype(jnp.int32))
    row_gate = jnp.zeros((n_rows,), jnp.float32).at[dest].set(gate_s)
    block_exp = jnp.minimum(
        jnp.searchsorted(pends, jnp.arange(n_blocks) * MOE_BLOCK, side='right'), N_EXPERTS - 1)

    def expert_block(args):
        rows, e = args
        xb = xt[rows]
        hb = jax.nn.silu(xb @ w_gate[e]) * (xb @ w_up[e])
        return hb @ w_down[e]

    y = lax.map(expert_block, (row_tok.reshape(n_blocks, MOE_BLOCK), block_exp))
    y = (y.reshape(n_rows, d) * row_gate[:, None]).astype(xt.dtype)
    out = jnp.zeros((t, d), xt.dtype).at[row_tok].add(y)
    return out.reshape(b, s, d)


def setup_inputs(seed: int = 0) -> dict:
    key = jax.random.key(seed)
    ks = jax.random.split(key, 40)
    f32 = jnp.float32

    def nrm(k, shape, scale):
        return jax.random.normal(k, shape, f32) * scale

    u = jax.random.uniform(ks[20], (DEPTH, LRU_W), f32, 0.9, 0.999)
    a0 = u ** (1.0 / LRU_C)
    lru_lambda = jnp.log(a0) - jnp.log1p(-a0)
    return {
        'x': nrm(ks[0], (BATCH, SEQ, D_MODEL), 1.0),
        'c': nrm(ks[1], (BATCH, D_MODEL), 1.0),
        'w_ada': nrm(ks[2], (DEPTH, D_MODEL, 6 * D_MODEL), 0.5 * D_MODEL ** -0.5),
        'b_ada': nrm(ks[3], (DEPTH, 6 * D_MODEL), 0.01),
        'ln1_g': 1.0 + nrm(ks[4], (DEPTH, D_MODEL), 0.02),
        'w_in': nrm(ks[5], (DEPTH, D_MODEL, IN_COLS), D_MODEL ** -0.5),
        'conv_dw_w': nrm(ks[6], (DEPTH, CONV_K, CONV_W), CONV_K ** -0.5),
        'conv_dw_b': nrm(ks[7], (DEPTH, CONV_W), 0.01),
        'conv_ln_g': 1.0 + nrm(ks[8], (DEPTH, CONV_W), 0.02),
        'conv_ln_b': nrm(ks[9], (DEPTH, CONV_W), 0.01),
        'fox_f_bias': jax.random.uniform(ks[10], (DEPTH, FOX_HEADS), f32, 1.0, 4.0),
        'fox_out_g': 1.0 + nrm(ks[11], (DEPTH, FOX_W), 0.02),
        'lru_conv_w': nrm(ks[12], (DEPTH, LRU_CONV_K, LRU_W), LRU_CONV_K ** -0.5),
        'lru_conv_b': nrm(ks[13], (DEPTH, LRU_W), 0.01),
        'lru_w_r': nrm(ks[14], (DEPTH, LRU_BLOCKS, LRU_BW, LRU_BW), LRU_BW ** -0.5),
        'lru_b_r': nrm(ks[15], (DEPTH, LRU_W), 0.01),
        'lru_w_i': nrm(ks[16], (DEPTH, LRU_BLOCKS, LRU_BW, LRU_BW), LRU_BW ** -0.5),
        'lru_b_i': nrm(ks[17], (DEPTH, LRU_W), 0.01),
        'lru_lambda': lru_lambda,
        'lru_out_g': 1.0 + nrm(ks[18], (DEPTH, LRU_W), 0.02),
        'w_out': nrm(ks[19], (DEPTH, D_MIX, D_MODEL), D_MIX ** -0.5),
        'ln2_g': 1.0 + nrm(ks[21], (DEPTH, D_MODEL), 0.02),
        'w_router_group': nrm(ks[22], (DEPTH, D_MODEL, N_GROUPS), D_MODEL ** -0.5),
        'b_router_group': nrm(ks[23], (DEPTH, N_GROUPS), 0.01),
        'w_router_expert': nrm(ks[24], (DEPTH, N_GROUPS, D_MODEL, EXPERTS_PER_GROUP), D_MODEL ** -0.5),
        'b_router_expert': nrm(ks[25], (DEPTH, N_GROUPS, EXPERTS_PER_GROUP), 0.01),
        'w_gate': nrm(ks[26], (DEPTH, N_EXPERTS, D_MODEL, D_FF_EXPERT), D_MODEL ** -0.5),
        'w_up': nrm(ks[27], (DEPTH, N_EXPERTS, D_MODEL, D_FF_EXPERT), D_MODEL ** -0.5),
        'w_down': nrm(ks[28], (DEPTH, N_EXPERTS, D_FF_EXPERT, D_MODEL), D_FF_EXPERT ** -0.5),
        'final_g': 1.0 + nrm(ks[29], (D_MODEL,), 0.02),
    }


def reference(x, c, w_ada, b_ada, ln1_g, w_in, conv_dw_w, conv_dw_b, conv_ln_g, conv_ln_b,
              fox_f_bias, fox_out_g, lru_conv_w, lru_conv_b, lru_w_r, lru_b_r, lru_w_i, lru_b_i,
              lru_lambda, lru_out_g, w_out, ln2_g, w_router_group, b_router_group,
              w_router_expert, b_router_expert, w_gate, w_up, w_down, final_g):
    b, s, _ = x.shape
    cond = jax.nn.silu(c)
    for l in range(DEPTH):
        mod = cond @ w_ada[l] + b_ada[l]
        sh1, sc1, g1, sh2, sc2, g2 = jnp.split(mod[:, None, :], 6, axis=-1)

        h = rms_norm(x, ln1_g[l]) * (1 + sc1) + sh1
        z = jnp.einsum('bsd,dn->bsn', h, w_in[l])

        y_conv = conformer_conv(z[..., OFF_CONV:OFF_Q], conv_dw_w[l], conv_dw_b[l],
                                conv_ln_g[l], conv_ln_b[l])

        q = z[..., OFF_Q:OFF_K].reshape(b, s, FOX_HEADS, FOX_HEAD_DIM)
        k = z[..., OFF_K:OFF_V].reshape(b, s, FOX_HEADS, FOX_HEAD_DIM)
        v = z[..., OFF_V:OFF_F].reshape(b, s, FOX_HEADS, FOX_HEAD_DIM)
        log_f = jax.nn.log_sigmoid(z[..., OFF_F:OFF_LRU_X].astype(jnp.float32)
                                   + fox_f_bias[l].astype(jnp.float32))
        y_fox = rms_norm(forgetting_attention(q, k, v, log_f), fox_out_g[l])

        h_lru = rg_lru(z[..., OFF_LRU_X:OFF_LRU_G], lru_conv_w[l], lru_conv_b[l], lru_w_r[l],
                       lru_b_r[l], lru_w_i[l], lru_b_i[l], lru_lambda[l])
        y_lru = rms_norm(h_lru * jax.nn.gelu(z[..., OFF_LRU_G:IN_COLS]), lru_out_g[l])

        y = jnp.einsum('bsm,md->bsd', jnp.concatenate([y_conv, y_fox, y_lru], axis=-1), w_out[l])
        x = x + g1 * y

        h2 = rms_norm(x, ln2_g[l]) * (1 + sc2) + sh2
        x = x + g2 * hierarchical_moe(h2, w_router_group[l], b_router_group[l], w_router_expert[l],
                                      b_router_expert[l], w_gate[l], w_up[l], w_down[l])
    return rms_norm(x, final_g)
```

```python
import contextlib
import numpy as np
import concourse.bass as bass
import concourse.mybir as mybir
from concourse.bass_utils import run_bass_kernel_spmd

F32 = mybir.dt.float32
BF16 = mybir.dt.bfloat16
AF = mybir.ActivationFunctionType
ALU = mybir.AluOpType
AX = mybir.AxisListType

EPOCH = 30000
N_DMA_SEMS = 36
D = 2048
KC = 16
E = 32
EPS = 1e-6
SKIP_ROUTER = False
C_LEVEL = 9
C_SKIP = set()


class Buf:
    __slots__ = ("w", "r")

    def __init__(self):
        self.w = None
        self.r = {}


class Sched:
    ENGS = ("pe", "act", "dve", "pool", "sp")
    ENGMAP = {"pe": "tensor", "act": "scalar", "dve": "vector", "pool": "gpsimd", "sp": "sync"}

    def __init__(self, nc, stack):
        self.nc = nc
        n_sems = {"pe": 24, "act": 8, "dve": 12, "pool": 6, "sp": 4}
        self.sems = {e: [stack.enter_context(nc.semaphore(f"s_{e}{i}")) for i in range(n_sems[e])]
                     for e in self.ENGS}
        self.cnt = {e: 0 for e in self.ENGS}
        self.q = {e: [] for e in self.ENGS}
        self.known = {e: {} for e in self.ENGS}
        self.dma_sems = [stack.enter_context(nc.semaphore(f"s_dma{i}")) for i in range(N_DMA_SEMS)]
        self.dma_val = [0] * N_DMA_SEMS
        self.dma_rr = 0
        self.own = {}
        for e in self.ENGS:
            for s in self.sems[e]:
                self.own[id(s)] = e

    def _deps(self, reads, writes):
        deps = []
        for b in reads:
            if b.w is not None:
                deps.append(b.w)
        for b in writes:
            if b.w is not None:
                deps.append(b.w)
            deps.extend(b.r.values())
        return deps

    def _commit(self, tok, reads, writes):
        k = id(tok[0])
        for b in reads:
            if k not in b.r or b.r[k][1] < tok[1]:
                b.r[k] = tok
        for b in writes:
            b.w = tok
            b.r = {}

    def _waits(self, eng, deps):
        need = {}
        kn = self.known[eng]
        for (sem, val) in deps:
            k = id(sem)
            if eng == "pe" and self.own.get(k) == "pe":
                continue
            if kn.get(k, 0) >= val:
                continue
            if k not in need or need[k][1] < val:
                need[k] = (sem, val)
        out = []
        for k, (sem, val) in need.items():
            kn[k] = val
            out.append((sem, val))
        return out

    def op(self, eng, fn, reads=(), writes=()):
        waits = self._waits(eng, self._deps(reads, writes))
        c = self.cnt[eng]
        sem = self.sems[eng][c // EPOCH]
        tok = (sem, (c % EPOCH) + 1)
        self.cnt[eng] = c + 1
        self.q[eng].append((waits, fn, sem, 1))
        self._commit(tok, reads, writes)
        return tok

    def dma(self, eng, out, in_, reads=(), writes=()):
        deps = self._deps(reads, writes)
        i = self.dma_rr
        self.dma_rr = (i + 1) % N_DMA_SEMS
        sem = self.dma_sems[i]
        if self.dma_val[i] > 0:
            deps.append((sem, self.dma_val[i]))
        waits = self._waits(eng, deps)
        self.dma_val[i] += 16
        tok = (sem, self.dma_val[i])

        def fn(e, out=out, in_=in_):
            return e.dma_start(out=out, in_=in_)
        self.q[eng].append((waits, fn, sem, 16))
        self._commit(tok, reads, writes)
        return tok

    def all_tokens(self):
        toks = [(self.dma_sems[i], self.dma_val[i]) for i in range(N_DMA_SEMS) if self.dma_val[i] > 0]
        for e in self.ENGS:
            c = self.cnt[e]
            if c > 0:
                toks.append((self.sems[e][(c - 1) // EPOCH], ((c - 1) % EPOCH) + 1))
        return toks

    def emit_block(self):
        toks = self.all_tokens()
        fin = {e: self._waits(e, [t for t in toks if self.own.get(id(t[0])) != e]) for e in self.ENGS}
        with self.nc.Block() as block:
            for e in self.ENGS:
                def body(engobj, lst=self.q[e], extra=fin[e]):
                    for (waits, fn, sem, inc) in lst:
                        for (s, v) in waits:
                            engobj.wait_ge(s, v)
                        fn(engobj).then_inc(sem, inc)
                    for (s, v) in extra:
                        engobj.wait_ge(s, v)
                getattr(block, self.ENGMAP[e])(body)
        self.q = {e: [] for e in self.ENGS}


def build(S, L, DFF, stop=None, dbg=False):
    NT = S // 128
    NFC = DFF // 128
    TGA = min(1024, S)
    nc = bass.Bass("TRN2", target_bir_lowering=False)

    def din(name, shape, dt=F32):
        return nc.dram_tensor(name, shape, dt, kind="ExternalInput").ap()

    def dscr(name, shape, dt=F32):
        if dbg:
            return nc.dram_tensor(name, shape, dt, kind="ExternalOutput").ap()
        return nc.dram_tensor(name, shape, dt).ap()

    x_in = din("x", [S, D])
    cT = din("cT", [128, KC])
    w_ada = din("w_ada", [L, 24, 128, KC, 512])
    b_ada = din("b_ada", [L, 128, 6 * D])
    ln1g = din("ln1g", [L, 128, D])
    ln2g = din("ln2g", [L, 128, D])
    fing = din("fing", [128, D])
    w_fm = din("w_fm", [L, 32, 128, KC, 128])
    w_v = din("w_v", [L, 2, 128, KC, 512])
    w_f = din("w_f", [L, 128, KC, 8])
    fb = din("fb", [L, 128, 8])
    cw = din("cw", [L, 4, 128, 31])
    cvec = din("cvec", [L, 4, 128, 3])
    fg = din("fg", [L, 128, 8])
    lw = din("lw", [L, 4, 128, 4])
    lvec = din("lvec", [L, 4, 128, 5])
    lwr = din("lwr", [L, 4, 128, 128])
    lwi = din("lwi", [L, 4, 128, 128])
    w_out = din("w_out", [L, 128, KC, D])
    w_rt = din("w_rt", [L, 128, KC, 36])
    b_rt = din("b_rt", [L, 128, 36])
    wg = din("wg", [L, E, 128, KC, DFF])
    wu = din("wu", [L, E, 128, KC, DFF])
    wd = din("wd", [L, E, 128, NFC, D])
    out = nc.dram_tensor("out", [S, D], F32, kind="ExternalOutput").ap()

    xs = dscr("resid", [S, D])
    mods = dscr("mods", [L, 128, 6, D])
    gluT = dscr("gluT", [512, S])
    qT = dscr("qT", [1024, S], BF16)
    kT = dscr("kT", [1024, S], BF16)
    vS = dscr("vS", [S, 1024], BF16)
    lxT = dscr("lxT", [512, S])
    lgT = dscr("lgT", [512, S])
    oT = dscr("oT", [1024, S])
    ymT = dscr("ymT", [D, S], BF16)
    h2T = dscr("h2T", [D, S], BF16)
    wg16 = dscr("wg16", [E, 128, KC, DFF], BF16)
    wu16 = dscr("wu16", [E, 128, KC, DFF], BF16)
    wd16 = dscr("wd16", [E, 128, NFC, D], BF16)

    with contextlib.ExitStack() as top:
        sc = Sched(nc, top)
        op, dma = sc.op, sc.dma

        uid = [0]

        def alloc(st, name, shape, dt):
            uid[0] += 1
            return st.enter_context(nc.sbuf_tensor(f"t{uid[0]}_{name}", shape, dt)), Buf()

        class Ring:
            def __init__(self, st, name, n, shape, dt):
                self.items = [alloc(st, f"{name}{i}", shape, dt) for i in range(n)]
                self.i = 0

            def next(self):
                it = self.items[self.i % len(self.items)]
                self.i += 1
                return it

        ps = [(top.enter_context(nc.psum_tensor(f"ps{i}", [128, 512], F32)), Buf()) for i in range(8)]
        psi = [0]

        def nps():
            it = ps[psi[0] % 4]
            psi[0] += 1
            return it

        lpi = [0]

        def lps2():
            k = 4 + 2 * (lpi[0] % 2)
            lpi[0] += 1
            return ps[k], ps[k + 1]

        ident, identB = alloc(top, "ident", [128, 128], F32)
        ones, onesB = alloc(top, "ones", [128, 128], F32)
        utri, utriB = alloc(top, "utri", [128, 128], F32)
        ones16, ones16B = alloc(top, "ones16", [128, 128], BF16)
        negcum, negcumB = alloc(top, "negcum", [128, NT, 8], F32)
        totS, totSB = alloc(top, "totS", [128, NT + 1, 8], F32)
        rmid, rmidB = alloc(top, "rmid", [128, NT, 8], F32)
        Gt, GtB = alloc(top, "Gt", [128, NT, E], F32)
        epsT, epsB = alloc(top, "epsT", [128, 1], F32)

        op("pool", lambda e: e.memset(ident[:], 0.0), writes=[identB])
        op("pool", lambda e: e.affine_select(out=ident[:], in_=ident[:], pattern=[[-1, 128]],
                                             compare_op=ALU.not_equal, fill=1.0, base=0, channel_multiplier=1),
           reads=[identB], writes=[identB])
        op("pool", lambda e: e.memset(ones[:], 1.0), writes=[onesB])
        op("pool", lambda e: e.memset(ones16[:], 1.0), writes=[ones16B])
        op("pool", lambda e: e.memset(utri[:], 1.0), writes=[utriB])
        op("pool", lambda e: e.affine_select(out=utri[:], in_=utri[:], pattern=[[1, 128]],
                                             compare_op=ALU.is_ge, fill=0.0, base=0, channel_multiplier=-1),
           reads=[utriB], writes=[utriB])
        op("pool", lambda e: e.memset(epsT[:], EPS), writes=[epsB])

        def rstd_from(dst, dstB, src, srcB, scale, tmp, tmpB):
            op("dve", lambda e: e.tensor_scalar(out=tmp, in0=src, scalar1=scale, scalar2=EPS,
                                                op0=ALU.mult, op1=ALU.add), reads=[srcB], writes=[tmpB])
            op("act", lambda e: e.activation(out=tmp, in_=tmp, func=AF.Sqrt), reads=[tmpB], writes=[tmpB])
            op("dve", lambda e: e.reciprocal(out=dst, in_=tmp), reads=[tmpB], writes=[dstB])

        with contextlib.ExitStack() as ph:
            cTt, cTB = alloc(ph, "cTt", [128, KC], F32)
            sg, sgB = alloc(ph, "sg0", [128, KC], F32)
            condB, condBB = alloc(ph, "condB", [128, KC, 128], F32)
            war = Ring(ph, "wa", 2, [128, KC, 512], F32)
            bar = Ring(ph, "ba", 2, [128, 512], F32)
            modb, modbB = alloc(ph, "modb", [128, 6, D], F32)
            lng, lngB = alloc(ph, "lng", [128, D], F32)
            dma("sp", cTt[:], cT[:], writes=[cTB])
            op("act", lambda e: e.activation(out=sg[:], in_=cTt[:], func=AF.Sigmoid), reads=[cTB], writes=[sgB])
            op("dve", lambda e: e.tensor_tensor(out=sg[:], in0=sg[:], in1=cTt[:], op=ALU.mult),
               reads=[sgB, cTB], writes=[sgB])
            for k in range(KC):
                op("dve", lambda e, k=k: e.tensor_scalar(out=condB[:, k, :], in0=ones[:], scalar1=sg[:, k:k + 1],
                                                         scalar2=None, op0=ALU.mult),
                   reads=[onesB, sgB], writes=[condBB])
            for l in range(L):
                for j in range(24):
                    wa, waB = war.next()
                    ba, baB = bar.next()
                    dma("sp", wa[:], w_ada[l, j], writes=[waB])
                    dma("sp", ba[:], b_ada[l, :, j * 512:(j + 1) * 512], writes=[baB])
                    p, pB = nps()
                    for k in range(KC):
                        op("pe", lambda e, k=k, p=p, wa=wa: e.matmul(p[:], lhsT=condB[:, k, :], rhs=wa[:, k, :],
                                                                  start=(k == 0), stop=(k == KC - 1)),
                           reads=[condBB, waB], writes=[pB])
                    s6, c4 = j // 4, j % 4
                    op("dve", lambda e, p=p, ba=ba, s6=s6, c4=c4: e.tensor_tensor(
                        out=modb[:, s6, c4 * 512:(c4 + 1) * 512], in0=p[:], in1=ba[:], op=ALU.add),
                       reads=[pB, baB], writes=[modbB])
                for (slot_sc, gsrc) in ((1, ln1g), (4, ln2g)):
                    dma("sp", lng[:], gsrc[l], writes=[lngB])
                    op("dve", lambda e, s=slot_sc: e.scalar_tensor_tensor(
                        out=modb[:, s, :], in0=modb[:, s, :], scalar=1.0, in1=lng[:], op0=ALU.add, op1=ALU.mult),
                       reads=[modbB, lngB], writes=[modbB])
                for (dst, src) in ((0, 1), (1, 0), (2, 2), (3, 4), (4, 3), (5, 5)):
                    dma("sp", mods[l, :, dst, :], modb[:, src, :], reads=[modbB])
            sc.emit_block()
            if stop == 'p0':
                return nc

        for l in range(L):
            xsrc = x_in if l == 0 else xs

            with contextlib.ExitStack() as ph:
                A1, A1B = alloc(ph, "A1", [128, D], F32)
                sh1, sh1B = alloc(ph, "sh1", [128, D], F32)
                fbt, fbB = alloc(ph, "fbt", [128, 8], F32)
                wft, wfB = alloc(ph, "wft", [128, KC, 8], BF16)
                xr = Ring(ph, "xa", 2, [128, D], F32)
                tr = Ring(ph, "ta", 2, [128, D], F32)
                hr = Ring(ph, "ha", 2, [128, D], F32)
                sr = Ring(ph, "sa", 4, [128, 4], F32)
                hT, hTB = alloc(ph, "hT", [128, KC, TGA], BF16)
                wcr = Ring(ph, "wc", 4, [128, KC, 128], BF16)
                wvr = Ring(ph, "wv", 2, [128, KC, 512], BF16)
                zr = Ring(ph, "za", 4, [128, 512], F32)
                z16r = Ring(ph, "zb", 4, [128, 512], BF16)
                sgr = Ring(ph, "zs", 2, [128, 512], F32)
                f8r = Ring(ph, "f8", 2, [128, 8], F32)
                tot, totB = alloc(ph, "tot", [128, 8], F32)
                junk, junkB = alloc(ph, "junkA", [128, D], F32)
                dma("sp", A1[:], mods[l, :, 0, :], writes=[A1B])
                dma("sp", sh1[:], mods[l, :, 1, :], writes=[sh1B])
                dma("sp", fbt[:], fb[l], writes=[fbB])
                dma("pool", wft[:], w_f[l], writes=[wfB])
                op("pool", lambda e: e.memset(totS[:, 0, :], 0.0), writes=[totSB])
                for g in range(S // TGA):
                    ntl = TGA // 128
                    for tt in range(ntl):
                        t = g * ntl + tt
                        xt, xB = xr.next()
                        tmp, tmpB = tr.next()
                        h32, hB = hr.next()
                        st4, st4B = sr.next()
                        dma("sp", xt[:], xsrc[t * 128:(t + 1) * 128, :], writes=[xB])
                        op("act", lambda e, xt=xt, st4=st4: e.activation(out=junk[:], in_=xt[:], func=AF.Square,
                                                                         accum_out=st4[:, 0:1]),
                           reads=[xB], writes=[junkB, st4B])
                        rstd_from(st4[:, 1:2], st4B, st4[:, 0:1], st4B, 1.0 / D, st4[:, 2:3], st4B)
                        op("dve", lambda e, xt=xt, st4=st4, tmp=tmp: e.scalar_tensor_tensor(
                            out=tmp[:], in0=xt[:], scalar=st4[:, 1:2], in1=A1[:], op0=ALU.mult, op1=ALU.mult),
                           reads=[xB, st4B, A1B], writes=[tmpB])
                        op("pool", lambda e, tmp=tmp, h32=h32: e.tensor_tensor(out=h32[:], in0=tmp[:], in1=sh1[:],
                                                                             op=ALU.add),
                           reads=[tmpB, sh1B], writes=[hB])
                        for q4 in range(4):
                            p, pB = nps()
                            for i in range(4):
                                kc = q4 * 4 + i
                                op("pe", lambda e, p=p, i=i, kc=kc, h32=h32: e.transpose(
                                    p[:, i * 128:(i + 1) * 128], h32[:, kc * 128:(kc + 1) * 128], ident[:]),
                                   reads=[hB, identB], writes=[pB])
                            op("act", lambda e, p=p, q4=q4, tt=tt: e.activation(
                                out=hT[:, q4 * 4:(q4 + 1) * 4, tt * 128:(tt + 1) * 128],
                                in_=p[:].rearrange("p (a b) -> p a b", a=4), func=AF.Copy),
                               reads=[pB], writes=[hTB])
                    nsub = TGA // 512
                    for j in range(32):
                        wc, wcB = wcr.next()
                        dma("pool", wc[:], w_fm[l, j], writes=[wcB])
                        for sub in range(nsub):
                            t0 = g * TGA + sub * 512
                            p, pB = nps()
                            for k in range(KC):
                                op("pe", lambda e, p=p, k=k, wc=wc, sub=sub: e.matmul(
                                    p[:], lhsT=wc[:, k, :], rhs=hT[:, k, sub * 512:(sub + 1) * 512],
                                    start=(k == 0), stop=(k == KC - 1)), reads=[wcB, hTB], writes=[pB])
                            if j < 8:
                                if j % 2 == 0:
                                    za, zaB = zr.next()
                                    op("dve", lambda e, za=za, p=p: e.tensor_copy(out=za[:], in_=p[:]),
                                       reads=[pB], writes=[zaB])
                                    if sub == 0:
                                        held = []
                                    held.append((za, zaB))
                                else:
                                    za, zaB = held[sub]
                                    sgm, sgmB = sgr.next()
                                    op("act", lambda e, sgm=sgm, p=p: e.activation(out=sgm[:], in_=p[:], func=AF.Sigmoid),
                                       reads=[pB], writes=[sgmB])
                                    op("dve", lambda e, za=za, sgm=sgm: e.tensor_tensor(out=za[:], in0=za[:], in1=sgm[:],
                                                                                     op=ALU.mult),
                                       reads=[zaB, sgmB], writes=[zaB])
                                    cc = j // 2
                                    dma("sp", gluT[cc * 128:(cc + 1) * 128, t0:t0 + 512], za[:], reads=[zaB])
                            elif j < 24:
                                z16, z16B = z16r.next()
                                scale = 128.0 ** -0.5 if j < 16 else 1.0
                                op("act", lambda e, z16=z16, p=p, scale=scale: e.activation(
                                    out=z16[:], in_=p[:], func=AF.Copy, scale=scale), reads=[pB], writes=[z16B])
                                dst = qT if j < 16 else kT
                                hh = (j - 8) % 8
                                dma("sp", dst[hh * 128:(hh + 1) * 128, t0:t0 + 512], z16[:], reads=[z16B])
                            else:
                                za, zaB = zr.next()
                                op("dve", lambda e, za=za, p=p: e.tensor_copy(out=za[:], in_=p[:]),
                                   reads=[pB], writes=[zaB])
                                dst = lxT if j < 28 else lgT
                                cc = (j - 24) % 4
                                dma("sp", dst[cc * 128:(cc + 1) * 128, t0:t0 + 512], za[:], reads=[zaB])
                    for vg in range(2):
                        wv, wvB = wvr.next()
                        dma("pool", wv[:], w_v[l, vg], writes=[wvB])
                        for tt in range(ntl):
                            t = g * ntl + tt
                            p, pB = nps()
                            for k in range(KC):
                                op("pe", lambda e, p=p, k=k, wv=wv, tt=tt: e.matmul(
                                    p[:], lhsT=hT[:, k, tt * 128:(tt + 1) * 128], rhs=wv[:, k, :],
                                    start=(k == 0), stop=(k == KC - 1)), reads=[wvB, hTB], writes=[pB])
                            z16, z16B = z16r.next()
                            op("act", lambda e, z16=z16, p=p: e.activation(out=z16[:], in_=p[:], func=AF.Copy),
                               reads=[pB], writes=[z16B])
                            dma("sp", vS[t * 128:(t + 1) * 128, vg * 512:(vg + 1) * 512], z16[:], reads=[z16B])
                    for tt in range(ntl):
                        t = g * ntl + tt
                        p, pB = nps()
                        for k in range(KC):
                            op("pe", lambda e, p=p, k=k, tt=tt: e.matmul(
                                p[:, 0:8], lhsT=hT[:, k, tt * 128:(tt + 1) * 128], rhs=wft[:, k, :],
                                start=(k == 0), stop=(k == KC - 1)), reads=[wfB, hTB], writes=[pB])
                        f8, f8B = f8r.next()
                        op("dve", lambda e, f8=f8, p=p: e.tensor_tensor(out=f8[:], in0=p[:, 0:8], in1=fbt[:], op=ALU.add),
                           reads=[pB, fbB], writes=[f8B])
                        op("act", lambda e, f8=f8: e.activation(out=f8[:], in_=f8[:], func=AF.Exp, scale=-1.0),
                           reads=[f8B], writes=[f8B])
                        op("act", lambda e, f8=f8: e.activation(out=f8[:], in_=f8[:], func=AF.Ln, bias=1.0),
                           reads=[f8B], writes=[f8B])
                        p2, p2B = nps()
                        op("pe", lambda e, p2=p2, f8=f8: e.matmul(p2[:, 0:8], lhsT=utri[:], rhs=f8[:], start=True, stop=True),
                           reads=[utriB, f8B], writes=[p2B])
                        op("pe", lambda e, p2=p2, f8=f8: e.matmul(p2[:, 8:16], lhsT=ones[:], rhs=f8[:], start=True, stop=True),
                           reads=[onesB, f8B], writes=[p2B])
                        op("dve", lambda e, p2=p2, t=t: e.tensor_tensor(out=negcum[:, t, :], in0=p2[:, 0:8],
                                                                      in1=totS[:, t, :], op=ALU.add),
                           reads=[p2B, totSB], writes=[negcumB])
                        op("dve", lambda e, p2=p2, t=t: e.tensor_tensor(out=totS[:, t + 1, :], in0=p2[:, 8:16],
                                                                      in1=totS[:, t, :], op=ALU.add),
                           reads=[p2B, totSB], writes=[totSB])
                op("dve", lambda e: e.tensor_tensor(out=rmid[:], in0=totS[:, 0:NT, :], in1=totS[:, 1:NT + 1, :], op=ALU.add),
                   reads=[totSB], writes=[rmidB])
                op("dve", lambda e: e.tensor_scalar(out=rmid[:], in0=rmid[:], scalar1=0.5, scalar2=None, op0=ALU.mult),
                   reads=[rmidB], writes=[rmidB])
                sc.emit_block()
                if stop == 'A':
                    return nc

            with contextlib.ExitStack() as ph:
                cwt, cwB = alloc(ph, "cwt", [128, 4, 31], F32)
                cvt, cvB = alloc(ph, "cvt", [128, 4, 3], F32)
                gr = Ring(ph, "gl", 3, [128, 30 + 512], F32)
                ar = Ring(ph, "ac", 8, [128, 512], F32)
                sqr = Ring(ph, "sq", 2, [128, 512], F32)
                mr = Ring(ph, "mm", 2, [128, 512], F32)
                vr = Ring(ph, "vv", 2, [128, 512], F32)
                yr = Ring(ph, "yy", 3, [128, 512], BF16)
                for ex in range(E):
                    dma("pool", wg16[ex], wg[l, ex])
                    dma("pool", wu16[ex], wu[l, ex])
                    dma("pool", wd16[ex], wd[l, ex])
                for cc in range(4):
                    dma("sp", cwt[:, cc, :], cw[l, cc], writes=[cwB])
                    dma("sp", cvt[:, cc, :], cvec[l, cc], writes=[cvB])
                for tb in range(S // 512):
                    t0 = tb * 512
                    accs = []
                    (pm, pmB), (pq, pqB) = lps2()
                    for cc in range(4):
                        gl, glB = gr.next()
                        if tb == 0:
                            op("pool", lambda e, gl=gl: e.memset(gl[:, 0:30], 0.0), writes=[glB])
                            dma("sp", gl[:, 30:542], gluT[cc * 128:(cc + 1) * 128, 0:512], writes=[glB])
                        else:
                            dma("sp", gl[:], gluT[cc * 128:(cc + 1) * 128, t0 - 30:t0 + 512], writes=[glB])
                        acc, accB = ar.next()
                        op("dve", lambda e, acc=acc, gl=gl, cc=cc: e.tensor_scalar(
                            out=acc[:], in0=gl[:, 0:512], scalar1=cwt[:, cc, 0:1], scalar2=cvt[:, cc, 0:1],
                            op0=ALU.mult, op1=ALU.add), reads=[glB, cwB, cvB], writes=[accB])
                        for k in range(1, 31):
                            op("dve", lambda e, acc=acc, gl=gl, cc=cc, k=k: e.scalar_tensor_tensor(
                                out=acc[:], in0=gl[:, k:k + 512], scalar=cwt[:, cc, k:k + 1], in1=acc[:],
                                op0=ALU.mult, op1=ALU.add), reads=[glB, cwB, accB], writes=[accB])
                        sq, sqB = sqr.next()
                        op("act", lambda e, sq=sq, acc=acc: e.activation(out=sq[:], in_=acc[:], func=AF.Square),
                           reads=[accB], writes=[sqB])
                        op("pe", lambda e, acc=acc, cc=cc, pm=pm: e.matmul(pm[:], lhsT=ones[:], rhs=acc[:],
                                                                         start=(cc == 0), stop=(cc == 3)),
                           reads=[onesB, accB], writes=[pmB])
                        op("pe", lambda e, sq=sq, cc=cc, pq=pq: e.matmul(pq[:], lhsT=ones[:], rhs=sq[:],
                                                                       start=(cc == 0), stop=(cc == 3)),
                           reads=[onesB, sqB], writes=[pqB])
                        accs.append((acc, accB))
                    mean, meanB = mr.next()
                    var, varB = vr.next()
                    op("dve", lambda e, mean=mean, pm=pm: e.tensor_scalar(out=mean[:], in0=pm[:], scalar1=1.0 / 512,
                                                                        scalar2=None, op0=ALU.mult),
                       reads=[pmB], writes=[meanB])
                    op("dve", lambda e, var=var, mean=mean: e.tensor_tensor(out=var[:], in0=mean[:], in1=mean[:], op=ALU.mult),
                       reads=[meanB], writes=[varB])
                    op("dve", lambda e, var=var, pq=pq: e.scalar_tensor_tensor(
                        out=var[:], in0=pq[:], scalar=1.0 / 512, in1=var[:], op0=ALU.mult, op1=ALU.subtract),
                       reads=[pqB, varB], writes=[varB])
                    op("dve", lambda e, var=var: e.tensor_scalar(out=var[:], in0=var[:], scalar1=EPS, scalar2=None, op0=ALU.add),
                       reads=[varB], writes=[varB])
                    op("act", lambda e, var=var: e.activation(out=var[:], in_=var[:], func=AF.Sqrt), reads=[varB], writes=[varB])
                    op("dve", lambda e, var=var: e.reciprocal(out=var[:], in_=var[:]), reads=[varB], writes=[varB])
                    for cc in range(4):
                        acc, accB = accs[cc]
                        op("pool", lambda e, acc=acc, mean=mean: e.tensor_tensor(out=acc[:], in0=acc[:], in1=mean[:],
                                                                               op=ALU.subtract),
                           reads=[accB, meanB], writes=[accB])
                        op("pool", lambda e, acc=acc, var=var: e.tensor_tensor(out=acc[:], in0=acc[:], in1=var[:], op=ALU.mult),
                           reads=[accB, varB], writes=[accB])
                        y, yB = yr.next()
                        op("act", lambda e, y=y, acc=acc, cc=cc: e.activation(
                            out=y[:], in_=acc[:], func=AF.Silu, scale=cvt[:, cc, 1:2], bias=cvt[:, cc, 2:3]),
                           reads=[accB, cvB], writes=[yB])
                        dma("sp", ymT[cc * 128:(cc + 1) * 128, t0:t0 + 512], y[:], reads=[yB])
                sc.emit_block()
                if stop == 'B1':
                    return nc

            with contextlib.ExitStack() as ph:
                qh, qhB = alloc(ph, "qh", [128, S], BF16)
                kh, khB = alloc(ph, "kh", [128, S], BF16)
                vh, vhB = alloc(ph, "vh", [128, NT, 128], BF16)
                biasT, biasTB = alloc(ph, "biasT", [128, NT, NT], F32)
                pr = Ring(ph, "pt", 4, [128, 512], BF16)
                rr = Ring(ph, "rl", 2, [128, 512], F32)
                orr = Ring(ph, "oo", 2, [128, 512], F32)
                fgt, fgB = alloc(ph, "fgt", [128, 8], F32)
                dma("sp", fgt[:], fg[l], writes=[fgB])
                for h in range(8):
                    dma("sp", qh[:], qT[h * 128:(h + 1) * 128, :], writes=[qhB])
                    dma("sp", kh[:], kT[h * 128:(h + 1) * 128, :], writes=[khB])
                    for v0 in range(0, NT, 16):
                        v1 = min(NT, v0 + 16)
                        dma("sp", vh[:, v0:v1, :], vS.rearrange("(t p) c -> p t c", p=128)[:, v0:v1, h * 128:(h + 1) * 128],
                            writes=[vhB])
                    for t in range(NT):
                        op("dve", lambda e, t=t, h=h: e.tensor_scalar(
                            out=biasT[:, t, :], in0=negcum[:, :, h], scalar1=rmid[:, t, h:h + 1], scalar2=None,
                            op0=ALU.subtract), reads=[negcumB, rmidB], writes=[biasTB])
                    for I in range(S // 512):
                        (po, poB), (pl, plB) = lps2()
                        nJ = I * 4 + 4
                        for J in range(nJ):
                            pS, pSB = nps()
                            op("pe", lambda e, pS=pS, J=J, I=I: e.matmul(
                                pS[:], lhsT=kh[:, J * 128:(J + 1) * 128], rhs=qh[:, I * 512:(I + 1) * 512],
                                start=True, stop=True), reads=[khB, qhB], writes=[pSB])
                            pt, ptB = pr.next()
                            for i4 in range(4):
                                t = I * 4 + i4
                                blk = pt[:, i4 * 128:(i4 + 1) * 128]
                                if J > t:
                                    op("pool", lambda e, blk=blk: e.memset(blk, 0.0), writes=[ptB])
                                    continue
                                bcol = biasT[:, t, J:J + 1]
                                op("act", lambda e, blk=blk, pS=pS, i4=i4, bcol=bcol: e.activation(
                                    out=blk, in_=pS[:, i4 * 128:(i4 + 1) * 128], func=AF.Exp, bias=bcol),
                                   reads=[pSB, biasTB], writes=[ptB])
                                if J == t:
                                    op("pool", lambda e, blk=blk: e.affine_select(
                                        out=blk, in_=blk, pattern=[[1, 128]], compare_op=ALU.is_ge, fill=0.0,
                                        base=0, channel_multiplier=-1), reads=[ptB], writes=[ptB])
                            op("pe", lambda e, po=po, pt=pt, J=J, nJ=nJ: e.matmul(
                                po[:], lhsT=vh[:, J, :], rhs=pt[:], start=(J == 0), stop=(J == nJ - 1)),
                               reads=[vhB, ptB], writes=[poB])
                            op("pe", lambda e, pl=pl, pt=pt, J=J, nJ=nJ: e.matmul(
                                pl[:], lhsT=ones16[:], rhs=pt[:], start=(J == 0), stop=(J == nJ - 1)),
                               reads=[ones16B, ptB], writes=[plB])
                        rl, rlB = rr.next()
                        op("dve", lambda e, rl=rl, pl=pl: e.reciprocal(out=rl[:], in_=pl[:]), reads=[plB], writes=[rlB])
                        oo, ooB = orr.next()
                        op("dve", lambda e, oo=oo, po=po, rl=rl: e.tensor_tensor(out=oo[:], in0=po[:], in1=rl[:], op=ALU.mult),
                           reads=[poB, rlB], writes=[ooB])
                        dma("sp", oT[h * 128:(h + 1) * 128, I * 512:(I + 1) * 512], oo[:], reads=[ooB])
                sc.emit_block()
                if stop == 'B2':
                    return nc

            with contextlib.ExitStack() as ph:
                fgt, fgB = alloc(ph, "fgt2", [128, 8], F32)
                o8r = Ring(ph, "o8", 2, [128, 8, 512], F32)
                sqr = Ring(ph, "sq2", 2, [128, 512], F32)
                rsr = Ring(ph, "rs2", 2, [128, 512], F32)
                yr = Ring(ph, "yy2", 3, [128, 512], BF16)
                dma("sp", fgt[:], fg[l], writes=[fgB])
                for tb in range(S // 512):
                    t0 = tb * 512
                    o8, o8B = o8r.next()
                    dma("sp", o8[:], oT.rearrange("(h p) s -> p h s", p=128)[:, :, t0:t0 + 512], writes=[o8B])
                    (pq, pqB), _unused = lps2()
                    for h in range(8):
                        sq, sqB = sqr.next()
                        op("act", lambda e, sq=sq, o8=o8, h=h: e.activation(out=sq[:], in_=o8[:, h, :], func=AF.Square),
                           reads=[o8B], writes=[sqB])
                        op("pe", lambda e, sq=sq, pq=pq, h=h: e.matmul(pq[:], lhsT=ones[:], rhs=sq[:],
                                                                     start=(h == 0), stop=(h == 7)),
                           reads=[onesB, sqB], writes=[pqB])
                    rs, rsB = rsr.next()
                    rstd_from(rs[:], rsB, pq[:], pqB, 1.0 / 1024, rs[:], rsB)
                    for h in range(8):
                        y, yB = yr.next()
                        op("dve", lambda e, y=y, o8=o8, h=h, rs=rs: e.scalar_tensor_tensor(
                            out=y[:], in0=o8[:, h, :], scalar=fgt[:, h:h + 1], in1=rs[:], op0=ALU.mult, op1=ALU.mult),
                           reads=[o8B, fgB, rsB], writes=[yB])
                        dma("sp", ymT[512 + h * 128:512 + (h + 1) * 128, t0:t0 + 512], y[:], reads=[yB])
                sc.emit_block()
                if stop == 'B2b':
                    return nc

            with contextlib.ExitStack() as ph:
                lwt, lwB = alloc(ph, "lwt", [128, 4, 4], F32)
                lvt, lvB = alloc(ph, "lvt", [128, 4, 5], F32)
                nsp, nspB = alloc(ph, "nsp", [128, 4, 2], F32)
                wrt, wrB = alloc(ph, "wrt", [128, 4, 128], F32)
                wit, wiB = alloc(ph, "wit", [128, 4, 128], F32)
                carry, carryB = alloc(ph, "carry", [128, 4], F32)
                xr_ = Ring(ph, "lx", 3, [128, 3 + 512], F32)
                gr_ = Ring(ph, "lg", 3, [128, 512], F32)
                xcr = Ring(ph, "xc", 3, [128, 512], F32)
                rr_ = Ring(ph, "rr", 2, [128, 512], F32)
                ir_ = Ring(ph, "ii", 2, [128, 512], F32)
                ar_ = Ring(ph, "aa", 2, [128, 512], F32)
                br_ = Ring(ph, "bb", 2, [128, 512], F32)
                hr_ = Ring(ph, "hh", 2, [128, 512], F32)
                mr_ = Ring(ph, "mm3", 8, [128, 512], F32)
                sqr = Ring(ph, "sq3", 2, [128, 512], F32)
                rsr = Ring(ph, "rs3", 2, [128, 512], F32)
                yr = Ring(ph, "yy3", 3, [128, 512], BF16)
                for cc in range(4):
                    dma("sp", lwt[:, cc, :], lw[l, cc], writes=[lwB])
                    dma("sp", lvt[:, cc, :], lvec[l, cc], writes=[lvB])
                    dma("sp", wrt[:, cc, :], lwr[l, cc], writes=[wrB])
                    dma("sp", wit[:, cc, :], lwi[l, cc], writes=[wiB])
                op("act", lambda e: e.activation(out=nsp[:, :, 0], in_=lvt[:, :, 3], func=AF.Exp, scale=-1.0),
                   reads=[lvB], writes=[nspB])
                op("act", lambda e: e.activation(out=nsp[:, :, 0], in_=nsp[:, :, 0], func=AF.Ln, bias=1.0),
                   reads=[nspB], writes=[nspB])
                op("dve", lambda e: e.tensor_scalar(out=nsp[:, :, 1], in0=nsp[:, :, 0], scalar1=-16.0, scalar2=None, op0=ALU.mult),
                   reads=[nspB], writes=[nspB])
                op("dve", lambda e: e.tensor_scalar(out=nsp[:, :, 0], in0=nsp[:, :, 0], scalar1=-8.0, scalar2=None, op0=ALU.mult),
                   reads=[nspB], writes=[nspB])
                op("pool", lambda e: e.memset(carry[:], 0.0), writes=[carryB])
                for tb in range(S // 512):
                    t0 = tb * 512
                    (pq, pqB), _unused = lps2()
                    ms = []
                    for cc in range(4):
                        lx, lxB = xr_.next()
                        if tb == 0:
                            op("pool", lambda e, lx=lx: e.memset(lx[:, 0:3], 0.0), writes=[lxB])
                            dma("sp", lx[:, 3:515], lxT[cc * 128:(cc + 1) * 128, 0:512], writes=[lxB])
                        else:
                            dma("sp", lx[:], lxT[cc * 128:(cc + 1) * 128, t0 - 3:t0 + 512], writes=[lxB])
                        lg_, lgB = gr_.next()
                        dma("sp", lg_[:], lgT[cc * 128:(cc + 1) * 128, t0:t0 + 512], writes=[lgB])
                        xc, xcB = xcr.next()
                        op("dve", lambda e, xc=xc, lx=lx, cc=cc: e.tensor_scalar(
                            out=xc[:], in0=lx[:, 0:512], scalar1=lwt[:, cc, 0:1], scalar2=lvt[:, cc, 0:1],
                            op0=ALU.mult, op1=ALU.add), reads=[lxB, lwB, lvB], writes=[xcB])
                        for k in range(1, 4):
                            op("dve", lambda e, xc=xc, lx=lx, cc=cc, k=k: e.scalar_tensor_tensor(
                                out=xc[:], in0=lx[:, k:k + 512], scalar=lwt[:, cc, k:k + 1], in1=xc[:],
                                op0=ALU.mult, op1=ALU.add), reads=[lxB, lwB, xcB], writes=[xcB])
                        pr_, prB = nps()
                        pi_, piB = nps()
                        op("pe", lambda e, pr_=pr_, xc=xc, cc=cc: e.matmul(pr_[:], lhsT=wrt[:, cc, :], rhs=xc[:], start=True, stop=True),
                           reads=[wrB, xcB], writes=[prB])
                        op("pe", lambda e, pi_=pi_, xc=xc, cc=cc: e.matmul(pi_[:], lhsT=wit[:, cc, :], rhs=xc[:], start=True, stop=True),
                           reads=[wiB, xcB], writes=[piB])
                        r_, rB = rr_.next()
                        i_, iB = ir_.next()
                        op("act", lambda e, r_=r_, pr_=pr_, cc=cc: e.activation(out=r_[:], in_=pr_[:], func=AF.Sigmoid,
                                                                             bias=lvt[:, cc, 1:2]),
                           reads=[prB, lvB], writes=[rB])
                        op("act", lambda e, i_=i_, pi_=pi_, cc=cc: e.activation(out=i_[:], in_=pi_[:], func=AF.Sigmoid,
                                                                             bias=lvt[:, cc, 2:3]),
                           reads=[piB, lvB], writes=[iB])
                        a_, aB = ar_.next()
                        b_, bB = br_.next()
                        op("act", lambda e, a_=a_, r_=r_, cc=cc: e.activation(out=a_[:], in_=r_[:], func=AF.Exp,
                                                                           scale=nsp[:, cc, 0:1]),
                           reads=[rB, nspB], writes=[aB])
                        op("act", lambda e, b_=b_, r_=r_, cc=cc: e.activation(out=b_[:], in_=r_[:], func=AF.Exp,
                                                                           scale=nsp[:, cc, 1:2]),
                           reads=[rB, nspB], writes=[bB])
                        op("dve", lambda e, b_=b_: e.tensor_scalar(out=b_[:], in0=b_[:], scalar1=-1.0, scalar2=1.0,
                                                                  op0=ALU.mult, op1=ALU.add), reads=[bB], writes=[bB])
                        op("dve", lambda e, b_=b_: e.tensor_scalar(out=b_[:], in0=b_[:], scalar1=0.0, scalar2=None,
                                                                  op0=ALU.max), reads=[bB], writes=[bB])
                        op("act", lambda e, b_=b_: e.activation(out=b_[:], in_=b_[:], func=AF.Sqrt), reads=[bB], writes=[bB])
                        op("pool", lambda e, i_=i_, xc=xc: e.tensor_tensor(out=i_[:], in0=i_[:], in1=xc[:], op=ALU.mult),
                           reads=[iB, xcB], writes=[iB])
                        op("pool", lambda e, b_=b_, i_=i_: e.tensor_tensor(out=b_[:], in0=b_[:], in1=i_[:], op=ALU.mult),
                           reads=[bB, iB], writes=[bB])
                        hh, hhB = hr_.next()
                        op("dve", lambda e, hh=hh, a_=a_, b_=b_, cc=cc: e.tensor_tensor_scan(
                            out=hh[:], data0=a_[:], data1=b_[:], initial=carry[:, cc:cc + 1], op0=ALU.mult, op1=ALU.add),
                           reads=[aB, bB, carryB], writes=[hhB])
                        op("dve", lambda e, hh=hh, cc=cc: e.tensor_copy(out=carry[:, cc:cc + 1], in_=hh[:, 511:512]),
                           reads=[hhB], writes=[carryB])
                        m_, mB = mr_.next()
                        op("act", lambda e, m_=m_, lg_=lg_: e.activation(out=m_[:], in_=lg_[:], func=AF.Square),
                           reads=[lgB], writes=[mB])
                        op("dve", lambda e, m_=m_: e.tensor_scalar(out=m_[:], in0=m_[:], scalar1=0.044715, scalar2=1.0,
                                                                  op0=ALU.mult, op1=ALU.add), reads=[mB], writes=[mB])
                        op("pool", lambda e, m_=m_, lg_=lg_: e.tensor_tensor(out=m_[:], in0=m_[:], in1=lg_[:], op=ALU.mult),
                           reads=[mB, lgB], writes=[mB])
                        op("act", lambda e, m_=m_: e.activation(out=m_[:], in_=m_[:], func=AF.Sigmoid, scale=1.5957691216),
                           reads=[mB], writes=[mB])
                        op("pool", lambda e, m_=m_, lg_=lg_: e.tensor_tensor(out=m_[:], in0=m_[:], in1=lg_[:], op=ALU.mult),
                           reads=[mB, lgB], writes=[mB])
                        op("pool", lambda e, m_=m_, hh=hh: e.tensor_tensor(out=m_[:], in0=m_[:], in1=hh[:], op=ALU.mult),
                           reads=[mB, hhB], writes=[mB])
                        sq, sqB = sqr.next()
                        op("act", lambda e, sq=sq, m_=m_: e.activation(out=sq[:], in_=m_[:], func=AF.Square),
                           reads=[mB], writes=[sqB])
                        op("pe", lambda e, sq=sq, pq=pq, cc=cc: e.matmul(pq[:], lhsT=ones[:], rhs=sq[:],
                                                                       start=(cc == 0), stop=(cc == 3)),
                           reads=[onesB, sqB], writes=[pqB])
                        ms.append((m_, mB))
                    rs, rsB = rsr.next()
                    rstd_from(rs[:], rsB, pq[:], pqB, 1.0 / 512, rs[:], rsB)
                    for cc in range(4):
                        m_, mB = ms[cc]
                        y, yB = yr.next()
                        op("dve", lambda e, y=y, m_=m_, cc=cc, rs=rs: e.scalar_tensor_tensor(
                            out=y[:], in0=m_[:], scalar=lvt[:, cc, 4:5], in1=rs[:], op0=ALU.mult, op1=ALU.mult),
                           reads=[mB, lvB, rsB], writes=[yB])
                        dma("sp", ymT[1536 + cc * 128:1536 + (cc + 1) * 128, t0:t0 + 512], y[:], reads=[yB])
                sc.emit_block()
                if stop == 'B3':
                    return nc

            with contextlib.ExitStack() as ph:
                wo, woB = alloc(ph, "wo", [128, KC, D], BF16)
                g1, g1B = alloc(ph, "g1", [128, D], F32)
                A2, A2B = alloc(ph, "A2", [128, D], F32)
                sh2, sh2B = alloc(ph, "sh2", [128, D], F32)
                wrt32, wrtB = alloc(ph, "wrt32", [128, KC, 36], F32)
                brt, brtB = alloc(ph, "brt", [128, 36], F32)
                ymr = Ring(ph, "ym", 1, [128, KC, 512], BF16)
                xr = Ring(ph, "xc_", 2, [128, D], F32)
                tr = Ring(ph, "tc_", 1, [128, D], F32)
                hr = Ring(ph, "hc_", 1, [128, D], F32)
                sr = Ring(ph, "sc_", 4, [128, 4], F32)
                h2r = Ring(ph, "h2", 2, [128, KC, 128], BF16)
                h32r = Ring(ph, "h32", 1, [128, KC, 128], F32)
                rtr = Ring(ph, "rt", 2, [128, 64], F32)
                for k4 in range(4):
                    dma("pool", wo[:, k4 * 4:(k4 + 1) * 4, :], w_out[l, :, k4 * 4:(k4 + 1) * 4, :], writes=[woB])
                dma("sp", g1[:], mods[l, :, 2, :], writes=[g1B])
                dma("sp", A2[:], mods[l, :, 3, :], writes=[A2B])
                dma("sp", sh2[:], mods[l, :, 4, :], writes=[sh2B])
                dma("sp", wrt32[:], w_rt[l], writes=[wrtB])
                dma("sp", brt[:], b_rt[l], writes=[brtB])
                for tg in range(S // 512):
                    ym, ymB = ymr.next()
                    dma("sp", ym[:], ymT.rearrange("(k p) s -> p k s", p=128)[:, :, tg * 512:(tg + 1) * 512], writes=[ymB])
                    for tt in range(4):
                        t = tg * 4 + tt
                        xt, xB = xr.next()
                        tmp, tmpB = tr.next()
                        h32, hB = hr.next()
                        st4, st4B = sr.next()
                        dma("sp", xt[:], xsrc[t * 128:(t + 1) * 128, :], writes=[xB])
                        if C_LEVEL < 1:
                            continue
                        for cg in range(4):
                            p, pB = nps()
                            for k in range(KC):
                                op("pe", lambda e, p=p, k=k, ym=ym, tt=tt, cg=cg: e.matmul(
                                    p[:], lhsT=ym[:, k, tt * 128:(tt + 1) * 128], rhs=wo[:, k, cg * 512:(cg + 1) * 512],
                                    start=(k == 0), stop=(k == KC - 1)), reads=[ymB, woB], writes=[pB])
                            op("dve", lambda e, p=p, tmp=tmp, cg=cg: e.tensor_tensor(
                                out=tmp[:, cg * 512:(cg + 1) * 512], in0=p[:], in1=g1[:, cg * 512:(cg + 1) * 512], op=ALU.mult),
                               reads=[pB, g1B], writes=[tmpB])
                        if "pool" not in C_SKIP:
                            op("pool", lambda e, xt=xt, tmp=tmp: e.tensor_tensor(out=xt[:], in0=xt[:], in1=tmp[:], op=ALU.add),
                               reads=[xB, tmpB], writes=[xB])
                        if "xs" not in C_SKIP:
                            dma("sp", (out if "toout" in C_SKIP else xs)[t * 128:(t + 1) * 128, :], xt[:], reads=[xB])
                        if C_LEVEL < 2:
                            continue
                        op("act", lambda e, xt=xt, st4=st4, h32=h32: e.activation(out=h32[:], in_=xt[:], func=AF.Square,
                                                                         accum_out=st4[:, 0:1]),
                           reads=[xB], writes=[hB, st4B])
                        rstd_from(st4[:, 1:2], st4B, st4[:, 0:1], st4B, 1.0 / D, st4[:, 2:3], st4B)
                        if C_LEVEL < 1.3:
                            continue
                        op("dve", lambda e, xt=xt, st4=st4, tmp=tmp: e.scalar_tensor_tensor(
                            out=tmp[:], in0=xt[:], scalar=st4[:, 1:2], in1=A2[:], op0=ALU.mult, op1=ALU.mult),
                           reads=[xB, st4B, A2B], writes=[tmpB])
                        op("pool", lambda e, tmp=tmp, h32=h32: e.tensor_tensor(out=h32[:], in0=tmp[:], in1=sh2[:], op=ALU.add),
                           reads=[tmpB, sh2B], writes=[hB])
                        if C_LEVEL < 1.6:
                            continue
                        h2, h2B = h2r.next()
                        hT32, hT32B = h32r.next()
                        for q4 in range(4):
                            p, pB = nps()
                            for i in range(4):
                                kc = q4 * 4 + i
                                op("pe", lambda e, p=p, i=i, kc=kc, h32=h32: e.transpose(
                                    p[:, i * 128:(i + 1) * 128], h32[:, kc * 128:(kc + 1) * 128], ident[:]),
                                   reads=[hB, identB], writes=[pB])
                            op("act", lambda e, p=p, q4=q4, hT32=hT32: e.activation(
                                out=hT32[:, q4 * 4:(q4 + 1) * 4, :].rearrange("p a b -> p (a b)"), in_=p[:], func=AF.Copy),
                               reads=[pB], writes=[hT32B])
                        op("pool", lambda e, h2=h2, hT32=hT32: e.tensor_copy(out=h2[:], in_=hT32[:]),
                           reads=[hT32B], writes=[h2B])
                        if C_LEVEL < 3:
                            continue
                        dma("sp", h2T.rearrange("(k p) s -> p k s", p=128)[:, :, t * 128:(t + 1) * 128], h2[:], reads=[h2B])
                        if SKIP_ROUTER:
                            continue
                        p, pB = nps()
                        for k in range(KC):
                            op("pe", lambda e, p=p, k=k, hT32=hT32: e.matmul(
                                p[:, 0:36], lhsT=hT32[:, k, :], rhs=wrt32[:, k, :], start=(k == 0), stop=(k == KC - 1)),
                               reads=[hT32B, wrtB], writes=[pB])
                        rt, rtB = rtr.next()
                        op("dve", lambda e, rt=rt, p=p: e.tensor_tensor(out=rt[:, 0:36], in0=p[:, 0:36], in1=brt[:], op=ALU.add),
                           reads=[pB, brtB], writes=[rtB])
                        op("dve", lambda e, rt=rt: e.tensor_reduce(out=rt[:, 56:57], in_=rt[:, 0:4], axis=AX.X, op=ALU.max),
                           reads=[rtB], writes=[rtB])
                        op("dve", lambda e, rt=rt: e.tensor_scalar(out=rt[:, 36:40], in0=rt[:, 0:4], scalar1=rt[:, 56:57],
                                                                  scalar2=None, op0=ALU.is_equal), reads=[rtB], writes=[rtB])
                        op("dve", lambda e, rt=rt: e.tensor_scalar(out=rt[:, 57:58], in0=rt[:, 56:57], scalar1=-1.0, scalar2=None,
                                                                  op0=ALU.mult), reads=[rtB], writes=[rtB])
                        op("act", lambda e, rt=rt: e.activation(out=rt[:, 60:64], in_=rt[:, 0:4], func=AF.Exp, bias=rt[:, 57:58],
                                                               accum_out=rt[:, 58:59]), reads=[rtB], writes=[rtB])
                        op("dve", lambda e, rt=rt: e.reciprocal(out=rt[:, 58:59], in_=rt[:, 58:59]), reads=[rtB], writes=[rtB])
                        op("dve", lambda e, rt=rt: e.tensor_scalar(out=rt[:, 40:48], in0=rt[:, 4:12], scalar1=rt[:, 36:37],
                                                                  scalar2=None, op0=ALU.mult), reads=[rtB], writes=[rtB])
                        for gi in range(1, 4):
                            op("dve", lambda e, rt=rt, gi=gi: e.scalar_tensor_tensor(
                                out=rt[:, 40:48], in0=rt[:, 4 + gi * 8:12 + gi * 8], scalar=rt[:, 36 + gi:37 + gi],
                                in1=rt[:, 40:48], op0=ALU.mult, op1=ALU.add), reads=[rtB], writes=[rtB])
                        op("dve", lambda e, rt=rt: e.max(out=rt[:, 48:56], in_=rt[:, 40:48]), reads=[rtB], writes=[rtB])
                        op("dve", lambda e, rt=rt: e.tensor_tensor(out=rt[:, 59:60], in0=rt[:, 49:50], in1=rt[:, 48:49], op=ALU.subtract),
                           reads=[rtB], writes=[rtB])
                        op("act", lambda e, rt=rt: e.activation(out=rt[:, 59:60], in_=rt[:, 59:60], func=AF.Exp),
                           reads=[rtB], writes=[rtB])
                        op("dve", lambda e, rt=rt: e.tensor_scalar(out=rt[:, 60:61], in0=rt[:, 59:60], scalar1=1.0, scalar2=None,
                                                                  op0=ALU.add), reads=[rtB], writes=[rtB])
                        op("dve", lambda e, rt=rt: e.reciprocal(out=rt[:, 60:61], in_=rt[:, 60:61]), reads=[rtB], writes=[rtB])
                        op("dve", lambda e, rt=rt: e.tensor_tensor(out=rt[:, 60:61], in0=rt[:, 60:61], in1=rt[:, 58:59], op=ALU.mult),
                           reads=[rtB], writes=[rtB])
                        op("dve", lambda e, rt=rt: e.tensor_tensor(out=rt[:, 61:62], in0=rt[:, 60:61], in1=rt[:, 59:60], op=ALU.mult),
                           reads=[rtB], writes=[rtB])
                        op("dve", lambda e, rt=rt: e.tensor_scalar(out=rt[:, 62:63], in0=rt[:, 48:49], scalar1=1.0, scalar2=None,
                                                                  op0=ALU.mult), reads=[rtB], writes=[rtB])
                        op("dve", lambda e, rt=rt: e.tensor_scalar(out=rt[:, 63:64], in0=rt[:, 49:50], scalar1=1.0, scalar2=None,
                                                                  op0=ALU.mult), reads=[rtB], writes=[rtB])
                        op("dve", lambda e, rt=rt: e.tensor_scalar(out=rt[:, 48:56], in0=rt[:, 40:48], scalar1=rt[:, 62:63],
                                                                  scalar2=rt[:, 60:61], op0=ALU.is_equal, op1=ALU.mult),
                           reads=[rtB], writes=[rtB])
                        op("dve", lambda e, rt=rt: e.tensor_scalar(out=rt[:, 40:48], in0=rt[:, 40:48], scalar1=rt[:, 63:64],
                                                                  scalar2=rt[:, 61:62], op0=ALU.is_equal, op1=ALU.mult),
                           reads=[rtB], writes=[rtB])
                        op("dve", lambda e, rt=rt: e.tensor_tensor(out=rt[:, 48:56], in0=rt[:, 48:56], in1=rt[:, 40:48], op=ALU.add),
                           reads=[rtB], writes=[rtB])
                        for gi in range(4):
                            op("dve", lambda e, rt=rt, gi=gi, t=t: e.tensor_scalar(
                                out=Gt[:, t, gi * 8:(gi + 1) * 8], in0=rt[:, 48:56], scalar1=rt[:, 36 + gi:37 + gi],
                                scalar2=None, op0=ALU.mult), reads=[rtB], writes=[GtB])
                sc.emit_block()
                if stop == 'C':
                    return nc

            with contextlib.ExitStack() as ph:
                g2, g2B = alloc(ph, "g2", [128, D], F32)
                ps8i = [0]

                def nps8():
                    it = ps[ps8i[0] % 8]
                    ps8i[0] += 1
                    return it
                h2g, h2gB = alloc(ph, "h2g", [128, KC, 512], BF16)
                accs = [alloc(ph, f"macc{i}", [128, D], F32) for i in range(4)]
                wgr = Ring(ph, "wgt", 2, [128, KC, DFF], BF16)
                wur = Ring(ph, "wut", 2, [128, KC, DFF], BF16)
                wdr = Ring(ph, "wdt", 1, [128, NFC, D], BF16)
                her = Ring(ph, "het", 2 * NFC, [128, 512], BF16)
                slr = Ring(ph, "slt", 2, [128, 512], F32)
                xr = Ring(ph, "xd_", 1, [128, D], F32)
                dma("sp", g2[:], mods[l, :, 5, :], writes=[g2B])
                for tg in range(S // 512):
                    dma("sp", h2g[:], h2T.rearrange("(k p) s -> p k s", p=128)[:, :, tg * 512:(tg + 1) * 512], writes=[h2gB])
                    for ex in range(E):
                        wgt, wgB = wgr.next()
                        wut, wuB = wur.next()
                        wdt, wdB = wdr.next()
                        dma("sp", wgt[:], wg16[ex], writes=[wgB])
                        dma("sp", wut[:], wu16[ex], writes=[wuB])
                        dma("sp", wdt[:], wd16[ex], writes=[wdB])
                        hes = []
                        for fc in range(NFC):
                            pg, pgB = nps8()
                            pu, puB = nps8()
                            for k in range(KC):
                                op("pe", lambda e, pg=pg, k=k, wgt=wgt, fc=fc: e.matmul(
                                    pg[:], lhsT=wgt[:, k, fc * 128:(fc + 1) * 128], rhs=h2g[:, k, :],
                                    start=(k == 0), stop=(k == KC - 1)), reads=[wgB, h2gB], writes=[pgB])
                            for k in range(KC):
                                op("pe", lambda e, pu=pu, k=k, wut=wut, fc=fc: e.matmul(
                                    pu[:], lhsT=wut[:, k, fc * 128:(fc + 1) * 128], rhs=h2g[:, k, :],
                                    start=(k == 0), stop=(k == KC - 1)), reads=[wuB, h2gB], writes=[puB])
                            sl, slB = slr.next()
                            op("act", lambda e, sl=sl, pg=pg: e.activation(out=sl[:], in_=pg[:], func=AF.Silu),
                               reads=[pgB], writes=[slB])
                            he, heB = her.next()
                            op("dve", lambda e, he=he, sl=sl, pu=pu: e.tensor_tensor(out=he[:], in0=pu[:], in1=sl[:], op=ALU.mult),
                               reads=[puB, slB], writes=[heB])
                            hes.append((he, heB))
                        for tt in range(4):
                            t = tg * 4 + tt
                            acc, accB = accs[tt]
                            for cg in range(4):
                                p, pB = nps8()
                                for fc in range(NFC):
                                    he, heB = hes[fc]
                                    op("pe", lambda e, p=p, he=he, fc=fc, tt=tt, cg=cg, wdt=wdt: e.matmul(
                                        p[:], lhsT=he[:, tt * 128:(tt + 1) * 128], rhs=wdt[:, fc, cg * 512:(cg + 1) * 512],
                                        start=(fc == 0), stop=(fc == NFC - 1)), reads=[heB, wdB], writes=[pB])
                                sl_ = acc[:, cg * 512:(cg + 1) * 512]
                                if ex == 0:
                                    op("dve", lambda e, p=p, sl_=sl_, t=t, ex=ex: e.tensor_scalar(
                                        out=sl_, in0=p[:], scalar1=Gt[:, t, ex:ex + 1], scalar2=None, op0=ALU.mult),
                                       reads=[pB, GtB], writes=[accB])
                                else:
                                    op("dve", lambda e, p=p, sl_=sl_, t=t, ex=ex: e.scalar_tensor_tensor(
                                        out=sl_, in0=p[:], scalar=Gt[:, t, ex:ex + 1], in1=sl_, op0=ALU.mult, op1=ALU.add),
                                       reads=[pB, GtB, accB], writes=[accB])
                    for tt in range(4):
                        t = tg * 4 + tt
                        acc, accB = accs[tt]
                        xt, xB = xr.next()
                        dma("sp", xt[:], xs[t * 128:(t + 1) * 128, :], writes=[xB])
                        op("pool", lambda e, acc=acc: e.tensor_tensor(out=acc[:], in0=acc[:], in1=g2[:], op=ALU.mult),
                           reads=[accB, g2B], writes=[accB])
                        op("pool", lambda e, acc=acc, xt=xt: e.tensor_tensor(out=xt[:], in0=xt[:], in1=acc[:], op=ALU.add),
                           reads=[accB, xB], writes=[xB])
                        dma("sp", xs[t * 128:(t + 1) * 128, :], xt[:], reads=[xB])
                sc.emit_block()
                if stop == 'D':
                    return nc

        with contextlib.ExitStack() as ph:
            fgn, fgnB = alloc(ph, "fgn", [128, D], F32)
            xr = Ring(ph, "xe_", 3, [128, D], F32)
            sr = Ring(ph, "se_", 4, [128, 4], F32)
            junk, junkB = alloc(ph, "junkE", [128, D], F32)
            dma("sp", fgn[:], fing[:], writes=[fgnB])
            for t in range(NT):
                xt, xB = xr.next()
                st4, st4B = sr.next()
                dma("sp", xt[:], xs[t * 128:(t + 1) * 128, :], writes=[xB])
                op("act", lambda e, xt=xt, st4=st4: e.activation(out=junk[:], in_=xt[:], func=AF.Square, accum_out=st4[:, 0:1]),
                   reads=[xB], writes=[junkB, st4B])
                rstd_from(st4[:, 1:2], st4B, st4[:, 0:1], st4B, 1.0 / D, st4[:, 2:3], st4B)
                op("dve", lambda e, xt=xt, st4=st4: e.scalar_tensor_tensor(
                    out=xt[:], in0=xt[:], scalar=st4[:, 1:2], in1=fgn[:], op0=ALU.mult, op1=ALU.mult),
                   reads=[xB, st4B, fgnB], writes=[xB])
                dma("sp", out[t * 128:(t + 1) * 128, :], xt[:], reads=[xB])
            sc.emit_block()
            if stop == 'E':
                return nc
    return nc


def _layout(inp, S, L, DFF):
    f = lambda a: np.ascontiguousarray(a, dtype=np.float32)
    NFC = DFF // 128
    rep = lambda a: f(np.broadcast_to(a[:, None, :], (a.shape[0], 128, a.shape[1])))
    w_in = inp["w_in"]
    cols = []
    for i in range(4):
        cols += list(range(i * 128, (i + 1) * 128))
        cols += list(range(512 + i * 128, 512 + (i + 1) * 128))
    cols += list(range(1024, 2048)) + list(range(2048, 3072))
    cols += list(range(4104, 4616)) + list(range(4616, 5128))
    cols = np.asarray(cols)
    sh = {}
    sh["w_ada"] = f(inp["w_ada"].reshape(L, KC, 128, 24, 512).transpose(0, 3, 2, 1, 4))
    sh["b_ada"] = rep(inp["b_ada"])
    sh["ln1g"] = rep(inp["ln1_g"])
    sh["ln2g"] = rep(inp["ln2_g"])
    sh["fing"] = f(np.broadcast_to(inp["final_g"][None, :], (128, D)))
    sh["w_fm"] = f(w_in[:, :, cols].reshape(L, KC, 128, 32, 128).transpose(0, 3, 2, 1, 4))
    sh["w_v"] = f(w_in[:, :, 3072:4096].reshape(L, KC, 128, 2, 512).transpose(0, 3, 2, 1, 4))
    sh["w_f"] = f(w_in[:, :, 4096:4104].reshape(L, KC, 128, 8).transpose(0, 2, 1, 3))
    sh["fb"] = rep(inp["fox_f_bias"])
    sh["cw"] = f(inp["conv_dw_w"].transpose(0, 2, 1).reshape(L, 4, 128, 31))
    sh["cvec"] = f(np.stack([inp["conv_dw_b"], inp["conv_ln_g"], inp["conv_ln_b"]], axis=-1).reshape(L, 4, 128, 3))
    sh["fg"] = f(inp["fox_out_g"].reshape(L, 8, 128).transpose(0, 2, 1))
    sh["lw"] = f(inp["lru_conv_w"].transpose(0, 2, 1).reshape(L, 4, 128, 4))
    sh["lvec"] = f(np.stack([inp["lru_conv_b"], inp["lru_b_r"], inp["lru_b_i"], inp["lru_lambda"], inp["lru_out_g"]],
                            axis=-1).reshape(L, 4, 128, 5))
    for nm, src in (("lwr", inp["lru_w_r"]), ("lwi", inp["lru_w_i"])):
        bd = np.zeros((L, 4, 128, 128), np.float32)
        for n in range(8):
            cc, n2 = n // 2, n % 2
            bd[:, cc, n2 * 64:(n2 + 1) * 64, n2 * 64:(n2 + 1) * 64] = src[:, n]
        sh[nm] = bd
    sh["w_out"] = f(inp["w_out"].reshape(L, KC, 128, D).transpose(0, 2, 1, 3))
    wr = np.concatenate([inp["w_router_group"], inp["w_router_expert"].transpose(0, 2, 1, 3).reshape(L, D, 32)], axis=-1)
    sh["w_rt"] = f(wr.reshape(L, KC, 128, 36).transpose(0, 2, 1, 3))
    sh["b_rt"] = rep(np.concatenate([inp["b_router_group"], inp["b_router_expert"].reshape(L, 32)], axis=-1))
    sh["wg"] = f(inp["w_gate"].reshape(L, E, KC, 128, DFF).transpose(0, 1, 3, 2, 4))
    sh["wu"] = f(inp["w_up"].reshape(L, E, KC, 128, DFF).transpose(0, 1, 3, 2, 4))
    sh["wd"] = f(inp["w_down"].reshape(L, E, NFC, 128, D).transpose(0, 1, 3, 2, 4))
    maps = []
    for b in range(inp["x"].shape[0]):
        m = dict(sh)
        m["x"] = f(inp["x"][b])
        m["cT"] = f(inp["c"][b].reshape(KC, 128).T)
        maps.append(m)
    return maps


def kernel(**inputs):
    inp = {k: np.asarray(v) for k, v in inputs.items()}
    B, S, _ = inp["x"].shape
    L = inp["w_ada"].shape[0]
    DFF = inp["w_gate"].shape[-1]
    nc = build(S, L, DFF)
    maps = _layout(inp, S, L, DFF)
    res = run_bass_kernel_spmd(nc, maps, core_ids=list(range(B)))
    return np.stack([np.asarray(res.results[b]["out"]) for b in range(B)], axis=0).astype(np.float32)
```

```python
import contextlib
import numpy as np
import concourse.bass as bass
import concourse.mybir as mybir
from concourse.bass_utils import run_bass_kernel_spmd

F32 = mybir.dt.float32
BF16 = mybir.dt.bfloat16
I32 = mybir.dt.int32
AF = mybir.ActivationFunctionType
ALU = mybir.AluOpType
AX = mybir.AxisListType

EPOCH = 30000
N_DMA_SEMS = 36
D = 2048
KC = 16
E = 32
EPS = 1e-6
SKIP_ROUTER = False
C_LEVEL = 9
C_SKIP = set()


class Buf:
    __slots__ = ("w", "r")

    def __init__(self):
        self.w = None
        self.r = {}


class Sched:
    ENGS = ("pe", "act", "dve", "pool", "sp")
    ENGMAP = {"pe": "tensor", "act": "scalar", "dve": "vector", "pool": "gpsimd", "sp": "sync"}

    def __init__(self, nc, stack):
        self.nc = nc
        n_sems = {"pe": 24, "act": 8, "dve": 12, "pool": 6, "sp": 4}
        self.sems = {e: [stack.enter_context(nc.semaphore(f"s_{e}{i}")) for i in range(n_sems[e])]
                     for e in self.ENGS}
        self.cnt = {e: 0 for e in self.ENGS}
        self.q = {e: [] for e in self.ENGS}
        self.known = {e: {} for e in self.ENGS}
        self.dma_sems = [stack.enter_context(nc.semaphore(f"s_dma{i}")) for i in range(N_DMA_SEMS)]
        self.dma_val = [0] * N_DMA_SEMS
        self.dma_rr = 0
        self.own = {}
        for e in self.ENGS:
            for s in self.sems[e]:
                self.own[id(s)] = e

    def _deps(self, reads, writes):
        deps = []
        for b in reads:
            if b.w is not None:
                deps.append(b.w)
        for b in writes:
            if b.w is not None:
                deps.append(b.w)
            deps.extend(b.r.values())
        return deps

    def _commit(self, tok, reads, writes):
        k = id(tok[0])
        for b in reads:
            if k not in b.r or b.r[k][1] < tok[1]:
                b.r[k] = tok
        for b in writes:
            b.w = tok
            b.r = {}

    def _waits(self, eng, deps):
        need = {}
        kn = self.known[eng]
        for (sem, val) in deps:
            k = id(sem)
            if eng == "pe" and self.own.get(k) == "pe":
                continue
            if kn.get(k, 0) >= val:
                continue
            if k not in need or need[k][1] < val:
                need[k] = (sem, val)
        out = []
        for k, (sem, val) in need.items():
            kn[k] = val
            out.append((sem, val))
        return out

    def op(self, eng, fn, reads=(), writes=()):
        waits = self._waits(eng, self._deps(reads, writes))
        c = self.cnt[eng]
        sem = self.sems[eng][c // EPOCH]
        tok = (sem, (c % EPOCH) + 1)
        self.cnt[eng] = c + 1
        self.q[eng].append((waits, fn, sem, 1))
        self._commit(tok, reads, writes)
        return tok

    def dma(self, eng, out, in_, reads=(), writes=()):
        deps = self._deps(reads, writes)
        i = self.dma_rr
        self.dma_rr = (i + 1) % N_DMA_SEMS
        sem = self.dma_sems[i]
        if self.dma_val[i] > 0:
            deps.append((sem, self.dma_val[i]))
        waits = self._waits(eng, deps)
        self.dma_val[i] += 16
        tok = (sem, self.dma_val[i])

        def fn(e, out=out, in_=in_):
            return e.dma_start(out=out, in_=in_)
        self.q[eng].append((waits, fn, sem, 16))
        self._commit(tok, reads, writes)
        return tok

    def idma(self, out, in_, idx, scatter, reads=(), writes=()):
        deps = self._deps(reads, writes)
        i = self.dma_rr
        self.dma_rr = (i + 1) % N_DMA_SEMS
        sem = self.dma_sems[i]
        if self.dma_val[i] > 0:
            deps.append((sem, self.dma_val[i]))
        waits = self._waits("pool", deps)
        self.dma_val[i] += 16
        tok = (sem, self.dma_val[i])

        def fn(e, out=out, in_=in_, idx=idx, scatter=scatter):
            off = bass.IndirectOffsetOnAxis(ap=idx, axis=0)
            if scatter:
                return e.indirect_dma_start(out=out, out_offset=off, in_=in_, in_offset=None)
            return e.indirect_dma_start(out=out, out_offset=None, in_=in_, in_offset=off)
        self.q["pool"].append((waits, fn, sem, 16))
        self._commit(tok, reads, writes)
        return tok

    def all_tokens(self):
        toks = [(self.dma_sems[i], self.dma_val[i]) for i in range(N_DMA_SEMS) if self.dma_val[i] > 0]
        for e in self.ENGS:
            c = self.cnt[e]
            if c > 0:
                toks.append((self.sems[e][(c - 1) // EPOCH], ((c - 1) % EPOCH) + 1))
        return toks

    def emit_block(self):
        toks = self.all_tokens()
        fin = {e: self._waits(e, [t for t in toks if self.own.get(id(t[0])) != e]) for e in self.ENGS}
        with self.nc.Block() as block:
            for e in self.ENGS:
                def body(engobj, lst=self.q[e], extra=fin[e]):
                    for (waits, fn, sem, inc) in lst:
                        for (s, v) in waits:
                            engobj.wait_ge(s, v)
                        fn(engobj).then_inc(sem, inc)
                    for (s, v) in extra:
                        engobj.wait_ge(s, v)
                getattr(block, self.ENGMAP[e])(body)
        self.q = {e: [] for e in self.ENGS}


def build(S, L, DFF, stop=None, dbg=False):
    NT = S // 128
    NFC = DFF // 128
    TGA = min(1024, S)
    nc = bass.Bass("TRN2", target_bir_lowering=False)

    def din(name, shape, dt=F32):
        return nc.dram_tensor(name, shape, dt, kind="ExternalInput").ap()

    def dscr(name, shape, dt=F32):
        if dbg:
            return nc.dram_tensor(name, shape, dt, kind="ExternalOutput").ap()
        return nc.dram_tensor(name, shape, dt).ap()

    x_in = din("x", [S, D])
    cT = din("cT", [128, KC])
    w_ada = din("w_ada", [L, 24, 128, KC, 512])
    b_ada = din("b_ada", [L, 128, 6 * D])
    ln1g = din("ln1g", [L, 128, D])
    ln2g = din("ln2g", [L, 128, D])
    fing = din("fing", [128, D])
    w_fm = din("w_fm", [L, 32, 128, KC, 128])
    w_v = din("w_v", [L, 2, 128, KC, 512])
    w_f = din("w_f", [L, 128, KC, 8])
    fb = din("fb", [L, 128, 8])
    cw = din("cw", [L, 4, 128, 31])
    cvec = din("cvec", [L, 4, 128, 3])
    fg = din("fg", [L, 128, 8])
    lw = din("lw", [L, 4, 128, 4])
    lvec = din("lvec", [L, 4, 128, 5])
    lwr = din("lwr", [L, 4, 128, 128])
    lwi = din("lwi", [L, 4, 128, 128])
    w_out = din("w_out", [L, 128, KC, D])
    w_rt = din("w_rt", [L, 128, KC, 36])
    b_rt = din("b_rt", [L, 128, 36])
    wg = din("wg", [L, E, 128, KC, DFF])
    wu = din("wu", [L, E, 128, KC, DFF])
    wd = din("wd", [L, E, 128, NFC, D])
    out = nc.dram_tensor("out", [S, D], F32, kind="ExternalOutput").ap()

    xs = dscr("resid", [S, D])
    mods = dscr("mods", [L, 128, 6, D])
    gluT = dscr("gluT", [512, S])
    qT = dscr("qT", [1024, S], BF16)
    kT = dscr("kT", [1024, S], BF16)
    vS = dscr("vS", [S, 1024], BF16)
    lxT = dscr("lxT", [512, S])
    lgT = dscr("lgT", [512, S])
    oT = dscr("oT", [1024, S])
    ymT = dscr("ymT", [D, S], BF16)
    h2T = dscr("h2T", [D, S], BF16)
    NB = 2 * S // 128 + E
    h2S = dscr("h2S", [S, D])
    tab = dscr("rtab", [NB * 128, 2])
    ysort = dscr("ysort", [NB * 128, D])
    wg16 = dscr("wg16", [E, 128, KC, DFF], BF16)
    wu16 = dscr("wu16", [E, 128, KC, DFF], BF16)
    wd16 = dscr("wd16", [E, 128, NFC, D], BF16)

    with contextlib.ExitStack() as top:
        sc = Sched(nc, top)
        op, dma = sc.op, sc.dma

        uid = [0]

        def alloc(st, name, shape, dt):
            uid[0] += 1
            return st.enter_context(nc.sbuf_tensor(f"t{uid[0]}_{name}", shape, dt)), Buf()

        class Ring:
            def __init__(self, st, name, n, shape, dt):
                self.items = [alloc(st, f"{name}{i}", shape, dt) for i in range(n)]
                self.i = 0

            def next(self):
                it = self.items[self.i % len(self.items)]
                self.i += 1
                return it

        ps = [(top.enter_context(nc.psum_tensor(f"ps{i}", [128, 512], F32)), Buf()) for i in range(8)]
        psi = [0]

        def nps():
            it = ps[psi[0] % 4]
            psi[0] += 1
            return it

        lpi = [0]

        def lps2():
            k = 4 + 2 * (lpi[0] % 2)
            lpi[0] += 1
            return ps[k], ps[k + 1]

        ident, identB = alloc(top, "ident", [128, 128], F32)
        ones, onesB = alloc(top, "ones", [128, 128], F32)
        utri, utriB = alloc(top, "utri", [128, 128], F32)
        ones16, ones16B = alloc(top, "ones16", [128, 128], BF16)
        negcum, negcumB = alloc(top, "negcum", [128, NT, 8], F32)
        totS, totSB = alloc(top, "totS", [128, NT + 1, 8], F32)
        rmid, rmidB = alloc(top, "rmid", [128, NT, 8], F32)
        Gt, GtB = alloc(top, "Gt", [128, NT, E], F32)
        epsT, epsB = alloc(top, "epsT", [128, 1], F32)
        rankI, rankIB = alloc(top, "rankI", [128, NT, E], F32)
        cnt, cntB = alloc(top, "cnt", [128, E], F32)
        d01, d01B = alloc(top, "d01", [128, NT, 2], I32)
        widx, widxB = alloc(top, "widx", [128, NB], I32)
        pidx, pidxB = alloc(top, "pidx", [128, 1], F32)
        pidxi, pidxiB = alloc(top, "pidxi", [128, 1], I32)
        tabB = Buf()
        op("pool", lambda e: e.iota(out=pidxi[:], pattern=[[0, 1]], base=0, channel_multiplier=1), writes=[pidxiB])
        op("dve", lambda e: e.tensor_copy(out=pidx[:], in_=pidxi[:]), reads=[pidxiB], writes=[pidxB])

        op("pool", lambda e: e.memset(ident[:], 0.0), writes=[identB])
        op("pool", lambda e: e.affine_select(out=ident[:], in_=ident[:], pattern=[[-1, 128]],
                                             compare_op=ALU.not_equal, fill=1.0, base=0, channel_multiplier=1),
           reads=[identB], writes=[identB])
        op("pool", lambda e: e.memset(ones[:], 1.0), writes=[onesB])
        op("pool", lambda e: e.memset(ones16[:], 1.0), writes=[ones16B])
        op("pool", lambda e: e.memset(utri[:], 1.0), writes=[utriB])
        op("pool", lambda e: e.affine_select(out=utri[:], in_=utri[:], pattern=[[1, 128]],
                                             compare_op=ALU.is_ge, fill=0.0, base=0, channel_multiplier=-1),
           reads=[utriB], writes=[utriB])
        op("pool", lambda e: e.memset(epsT[:], EPS), writes=[epsB])

        def rstd_from(dst, dstB, src, srcB, scale, tmp, tmpB):
            op("dve", lambda e: e.tensor_scalar(out=tmp, in0=src, scalar1=scale, scalar2=EPS,
                                                op0=ALU.mult, op1=ALU.add), reads=[srcB], writes=[tmpB])
            op("act", lambda e: e.activation(out=tmp, in_=tmp, func=AF.Sqrt), reads=[tmpB], writes=[tmpB])
            op("dve", lambda e: e.reciprocal(out=dst, in_=tmp), reads=[tmpB], writes=[dstB])

        with contextlib.ExitStack() as ph:
            cTt, cTB = alloc(ph, "cTt", [128, KC], F32)
            sg, sgB = alloc(ph, "sg0", [128, KC], F32)
            condB, condBB = alloc(ph, "condB", [128, KC, 128], F32)
            war = Ring(ph, "wa", 2, [128, KC, 512], F32)
            bar = Ring(ph, "ba", 2, [128, 512], F32)
            modb, modbB = alloc(ph, "modb", [128, 6, D], F32)
            lng, lngB = alloc(ph, "lng", [128, D], F32)
            dma("sp", cTt[:], cT[:], writes=[cTB])
            op("act", lambda e: e.activation(out=sg[:], in_=cTt[:], func=AF.Sigmoid), reads=[cTB], writes=[sgB])
            op("dve", lambda e: e.tensor_tensor(out=sg[:], in0=sg[:], in1=cTt[:], op=ALU.mult),
               reads=[sgB, cTB], writes=[sgB])
            for k in range(KC):
                op("dve", lambda e, k=k: e.tensor_scalar(out=condB[:, k, :], in0=ones[:], scalar1=sg[:, k:k + 1],
                                                         scalar2=None, op0=ALU.mult),
                   reads=[onesB, sgB], writes=[condBB])
            for l in range(L):
                for j in range(24):
                    wa, waB = war.next()
                    ba, baB = bar.next()
                    dma("sp", wa[:], w_ada[l, j], writes=[waB])
                    dma("sp", ba[:], b_ada[l, :, j * 512:(j + 1) * 512], writes=[baB])
                    p, pB = nps()
                    for k in range(KC):
                        op("pe", lambda e, k=k, p=p, wa=wa: e.matmul(p[:], lhsT=condB[:, k, :], rhs=wa[:, k, :],
                                                                  start=(k == 0), stop=(k == KC - 1)),
                           reads=[condBB, waB], writes=[pB])
                    s6, c4 = j // 4, j % 4
                    op("dve", lambda e, p=p, ba=ba, s6=s6, c4=c4: e.tensor_tensor(
                        out=modb[:, s6, c4 * 512:(c4 + 1) * 512], in0=p[:], in1=ba[:], op=ALU.add),
                       reads=[pB, baB], writes=[modbB])
                for (slot_sc, gsrc) in ((1, ln1g), (4, ln2g)):
                    dma("sp", lng[:], gsrc[l], writes=[lngB])
                    op("dve", lambda e, s=slot_sc: e.scalar_tensor_tensor(
                        out=modb[:, s, :], in0=modb[:, s, :], scalar=1.0, in1=lng[:], op0=ALU.add, op1=ALU.mult),
                       reads=[modbB, lngB], writes=[modbB])
                for (dst, src) in ((0, 1), (1, 0), (2, 2), (3, 4), (4, 3), (5, 5)):
                    dma("sp", mods[l, :, dst, :], modb[:, src, :], reads=[modbB])
            sc.emit_block()
            if stop == 'p0':
                return nc

        for l in range(L):
            xsrc = x_in if l == 0 else xs

            with contextlib.ExitStack() as ph:
                A1, A1B = alloc(ph, "A1", [128, D], F32)
                sh1, sh1B = alloc(ph, "sh1", [128, D], F32)
                fbt, fbB = alloc(ph, "fbt", [128, 8], F32)
                wft, wfB = alloc(ph, "wft", [128, KC, 8], BF16)
                xr = Ring(ph, "xa", 2, [128, D], F32)
                tr = Ring(ph, "ta", 2, [128, D], F32)
                hr = Ring(ph, "ha", 2, [128, D], F32)
                sr = Ring(ph, "sa", 4, [128, 4], F32)
                hT, hTB = alloc(ph, "hT", [128, KC, TGA], BF16)
                wcr = Ring(ph, "wc", 4, [128, KC, 128], BF16)
                wvr = Ring(ph, "wv", 2, [128, KC, 512], BF16)
                zr = Ring(ph, "za", 4, [128, 512], F32)
                z16r = Ring(ph, "zb", 4, [128, 512], BF16)
                sgr = Ring(ph, "zs", 2, [128, 512], F32)
                f8r = Ring(ph, "f8", 2, [128, 8], F32)
                tot, totB = alloc(ph, "tot", [128, 8], F32)
                junk, junkB = alloc(ph, "junkA", [128, D], F32)
                dma("sp", A1[:], mods[l, :, 0, :], writes=[A1B])
                dma("sp", sh1[:], mods[l, :, 1, :], writes=[sh1B])
                dma("sp", fbt[:], fb[l], writes=[fbB])
                dma("pool", wft[:], w_f[l], writes=[wfB])
                op("pool", lambda e: e.memset(totS[:, 0, :], 0.0), writes=[totSB])
                for g in range(S // TGA):
                    ntl = TGA // 128
                    for tt in range(ntl):
                        t = g * ntl + tt
                        xt, xB = xr.next()
                        tmp, tmpB = tr.next()
                        h32, hB = hr.next()
                        st4, st4B = sr.next()
                        dma("sp", xt[:], xsrc[t * 128:(t + 1) * 128, :], writes=[xB])
                        op("act", lambda e, xt=xt, st4=st4: e.activation(out=junk[:], in_=xt[:], func=AF.Square,
                                                                         accum_out=st4[:, 0:1]),
                           reads=[xB], writes=[junkB, st4B])
                        rstd_from(st4[:, 1:2], st4B, st4[:, 0:1], st4B, 1.0 / D, st4[:, 2:3], st4B)
                        op("dve", lambda e, xt=xt, st4=st4, tmp=tmp: e.scalar_tensor_tensor(
                            out=tmp[:], in0=xt[:], scalar=st4[:, 1:2], in1=A1[:], op0=ALU.mult, op1=ALU.mult),
                           reads=[xB, st4B, A1B], writes=[tmpB])
                        op("pool", lambda e, tmp=tmp, h32=h32: e.tensor_tensor(out=h32[:], in0=tmp[:], in1=sh1[:],
                                                                             op=ALU.add),
                           reads=[tmpB, sh1B], writes=[hB])
                        for q4 in range(4):
                            p, pB = nps()
                            for i in range(4):
                                kc = q4 * 4 + i
                                op("pe", lambda e, p=p, i=i, kc=kc, h32=h32: e.transpose(
                                    p[:, i * 128:(i + 1) * 128], h32[:, kc * 128:(kc + 1) * 128], ident[:]),
                                   reads=[hB, identB], writes=[pB])
                            op("act", lambda e, p=p, q4=q4, tt=tt: e.activation(
                                out=hT[:, q4 * 4:(q4 + 1) * 4, tt * 128:(tt + 1) * 128],
                                in_=p[:].rearrange("p (a b) -> p a b", a=4), func=AF.Copy),
                               reads=[pB], writes=[hTB])
                    nsub = TGA // 512
                    for j in range(32):
                        wc, wcB = wcr.next()
                        dma("pool", wc[:], w_fm[l, j], writes=[wcB])
                        for sub in range(nsub):
                            t0 = g * TGA + sub * 512
                            p, pB = nps()
                            for k in range(KC):
                                op("pe", lambda e, p=p, k=k, wc=wc, sub=sub: e.matmul(
                                    p[:], lhsT=wc[:, k, :], rhs=hT[:, k, sub * 512:(sub + 1) * 512],
                                    start=(k == 0), stop=(k == KC - 1)), reads=[wcB, hTB], writes=[pB])
                            if j < 8:
                                if j % 2 == 0:
                                    za, zaB = zr.next()
                                    op("dve", lambda e, za=za, p=p: e.tensor_copy(out=za[:], in_=p[:]),
                                       reads=[pB], writes=[zaB])
                                    if sub == 0:
                                        held = []
                                    held.append((za, zaB))
                                else:
                                    za, zaB = held[sub]
                                    sgm, sgmB = sgr.next()
                                    op("act", lambda e, sgm=sgm, p=p: e.activation(out=sgm[:], in_=p[:], func=AF.Sigmoid),
                                       reads=[pB], writes=[sgmB])
                                    op("dve", lambda e, za=za, sgm=sgm: e.tensor_tensor(out=za[:], in0=za[:], in1=sgm[:],
                                                                                     op=ALU.mult),
                                       reads=[zaB, sgmB], writes=[zaB])
                                    cc = j // 2
                                    dma("sp", gluT[cc * 128:(cc + 1) * 128, t0:t0 + 512], za[:], reads=[zaB])
                            elif j < 24:
                                z16, z16B = z16r.next()
                                scale = 128.0 ** -0.5 if j < 16 else 1.0
                                op("act", lambda e, z16=z16, p=p, scale=scale: e.activation(
                                    out=z16[:], in_=p[:], func=AF.Copy, scale=scale), reads=[pB], writes=[z16B])
                                dst = qT if j < 16 else kT
                                hh = (j - 8) % 8
                                dma("sp", dst[hh * 128:(hh + 1) * 128, t0:t0 + 512], z16[:], reads=[z16B])
                            else:
                                za, zaB = zr.next()
                                op("dve", lambda e, za=za, p=p: e.tensor_copy(out=za[:], in_=p[:]),
                                   reads=[pB], writes=[zaB])
                                dst = lxT if j < 28 else lgT
                                cc = (j - 24) % 4
                                dma("sp", dst[cc * 128:(cc + 1) * 128, t0:t0 + 512], za[:], reads=[zaB])
                    for vg in range(2):
                        wv, wvB = wvr.next()
                        dma("pool", wv[:], w_v[l, vg], writes=[wvB])
                        for tt in range(ntl):
                            t = g * ntl + tt
                            p, pB = nps()
                            for k in range(KC):
                                op("pe", lambda e, p=p, k=k, wv=wv, tt=tt: e.matmul(
                                    p[:], lhsT=hT[:, k, tt * 128:(tt + 1) * 128], rhs=wv[:, k, :],
                                    start=(k == 0), stop=(k == KC - 1)), reads=[wvB, hTB], writes=[pB])
                            z16, z16B = z16r.next()
                            op("act", lambda e, z16=z16, p=p: e.activation(out=z16[:], in_=p[:], func=AF.Copy),
                               reads=[pB], writes=[z16B])
                            dma("sp", vS[t * 128:(t + 1) * 128, vg * 512:(vg + 1) * 512], z16[:], reads=[z16B])
                    for tt in range(ntl):
                        t = g * ntl + tt
                        p, pB = nps()
                        for k in range(KC):
                            op("pe", lambda e, p=p, k=k, tt=tt: e.matmul(
                                p[:, 0:8], lhsT=hT[:, k, tt * 128:(tt + 1) * 128], rhs=wft[:, k, :],
                                start=(k == 0), stop=(k == KC - 1)), reads=[wfB, hTB], writes=[pB])
                        f8, f8B = f8r.next()
                        op("dve", lambda e, f8=f8, p=p: e.tensor_tensor(out=f8[:], in0=p[:, 0:8], in1=fbt[:], op=ALU.add),
                           reads=[pB, fbB], writes=[f8B])
                        op("act", lambda e, f8=f8: e.activation(out=f8[:], in_=f8[:], func=AF.Exp, scale=-1.0),
                           reads=[f8B], writes=[f8B])
                        op("act", lambda e, f8=f8: e.activation(out=f8[:], in_=f8[:], func=AF.Ln, bias=1.0),
                           reads=[f8B], writes=[f8B])
                        p2, p2B = nps()
                        op("pe", lambda e, p2=p2, f8=f8: e.matmul(p2[:, 0:8], lhsT=utri[:], rhs=f8[:], start=True, stop=True),
                           reads=[utriB, f8B], writes=[p2B])
                        op("pe", lambda e, p2=p2, f8=f8: e.matmul(p2[:, 8:16], lhsT=ones[:], rhs=f8[:], start=True, stop=True),
                           reads=[onesB, f8B], writes=[p2B])
                        op("dve", lambda e, p2=p2, t=t: e.tensor_tensor(out=negcum[:, t, :], in0=p2[:, 0:8],
                                                                      in1=totS[:, t, :], op=ALU.add),
                           reads=[p2B, totSB], writes=[negcumB])
                        op("dve", lambda e, p2=p2, t=t: e.tensor_tensor(out=totS[:, t + 1, :], in0=p2[:, 8:16],
                                                                      in1=totS[:, t, :], op=ALU.add),
                           reads=[p2B, totSB], writes=[totSB])
                op("dve", lambda e: e.tensor_tensor(out=rmid[:], in0=totS[:, 0:NT, :], in1=totS[:, 1:NT + 1, :], op=ALU.add),
                   reads=[totSB], writes=[rmidB])
                op("dve", lambda e: e.tensor_scalar(out=rmid[:], in0=rmid[:], scalar1=0.5, scalar2=None, op0=ALU.mult),
                   reads=[rmidB], writes=[rmidB])
                sc.emit_block()
                if stop == 'A':
                    return nc

            with contextlib.ExitStack() as ph:
                cwt, cwB = alloc(ph, "cwt", [128, 4, 31], F32)
                cvt, cvB = alloc(ph, "cvt", [128, 4, 3], F32)
                gr = Ring(ph, "gl", 3, [128, 30 + 512], F32)
                ar = Ring(ph, "ac", 8, [128, 512], F32)
                sqr = Ring(ph, "sq", 2, [128, 512], F32)
                mr = Ring(ph, "mm", 2, [128, 512], F32)
                vr = Ring(ph, "vv", 2, [128, 512], F32)
                yr = Ring(ph, "yy", 3, [128, 512], BF16)
                for ex in range(E):
                    dma("pool", wg16[ex], wg[l, ex])
                    dma("pool", wu16[ex], wu[l, ex])
                    dma("pool", wd16[ex], wd[l, ex])
                for cc in range(4):
                    dma("sp", cwt[:, cc, :], cw[l, cc], writes=[cwB])
                    dma("sp", cvt[:, cc, :], cvec[l, cc], writes=[cvB])
                for tb in range(S // 512):
                    t0 = tb * 512
                    accs = []
                    (pm, pmB), (pq, pqB) = lps2()
                    for cc in range(4):
                        gl, glB = gr.next()
                        if tb == 0:
                            op("pool", lambda e, gl=gl: e.memset(gl[:, 0:30], 0.0), writes=[glB])
                            dma("sp", gl[:, 30:542], gluT[cc * 128:(cc + 1) * 128, 0:512], writes=[glB])
                        else:
                            dma("sp", gl[:], gluT[cc * 128:(cc + 1) * 128, t0 - 30:t0 + 512], writes=[glB])
                        acc, accB = ar.next()
                        op("dve", lambda e, acc=acc, gl=gl, cc=cc: e.tensor_scalar(
                            out=acc[:], in0=gl[:, 0:512], scalar1=cwt[:, cc, 0:1], scalar2=cvt[:, cc, 0:1],
                            op0=ALU.mult, op1=ALU.add), reads=[glB, cwB, cvB], writes=[accB])
                        for k in range(1, 31):
                            op("dve", lambda e, acc=acc, gl=gl, cc=cc, k=k: e.scalar_tensor_tensor(
                                out=acc[:], in0=gl[:, k:k + 512], scalar=cwt[:, cc, k:k + 1], in1=acc[:],
                                op0=ALU.mult, op1=ALU.add), reads=[glB, cwB, accB], writes=[accB])
                        sq, sqB = sqr.next()
                        op("act", lambda e, sq=sq, acc=acc: e.activation(out=sq[:], in_=acc[:], func=AF.Square),
                           reads=[accB], writes=[sqB])
                        op("pe", lambda e, acc=acc, cc=cc, pm=pm: e.matmul(pm[:], lhsT=ones[:], rhs=acc[:],
                                                                         start=(cc == 0), stop=(cc == 3)),
                           reads=[onesB, accB], writes=[pmB])
                        op("pe", lambda e, sq=sq, cc=cc, pq=pq: e.matmul(pq[:], lhsT=ones[:], rhs=sq[:],
                                                                       start=(cc == 0), stop=(cc == 3)),
                           reads=[onesB, sqB], writes=[pqB])
                        accs.append((acc, accB))
                    mean, meanB = mr.next()
                    var, varB = vr.next()
                    op("dve", lambda e, mean=mean, pm=pm: e.tensor_scalar(out=mean[:], in0=pm[:], scalar1=1.0 / 512,
                                                                        scalar2=None, op0=ALU.mult),
                       reads=[pmB], writes=[meanB])
                    op("dve", lambda e, var=var, mean=mean: e.tensor_tensor(out=var[:], in0=mean[:], in1=mean[:], op=ALU.mult),
                       reads=[meanB], writes=[varB])
                    op("dve", lambda e, var=var, pq=pq: e.scalar_tensor_tensor(
                        out=var[:], in0=pq[:], scalar=1.0 / 512, in1=var[:], op0=ALU.mult, op1=ALU.subtract),
                       reads=[pqB, varB], writes=[varB])
                    op("dve", lambda e, var=var: e.tensor_scalar(out=var[:], in0=var[:], scalar1=EPS, scalar2=None, op0=ALU.add),
                       reads=[varB], writes=[varB])
                    op("act", lambda e, var=var: e.activation(out=var[:], in_=var[:], func=AF.Sqrt), reads=[varB], writes=[varB])
                    op("dve", lambda e, var=var: e.reciprocal(out=var[:], in_=var[:]), reads=[varB], writes=[varB])
                    for cc in range(4):
                        acc, accB = accs[cc]
                        op("pool", lambda e, acc=acc, mean=mean: e.tensor_tensor(out=acc[:], in0=acc[:], in1=mean[:],
                                                                               op=ALU.subtract),
                           reads=[accB, meanB], writes=[accB])
                        op("pool", lambda e, acc=acc, var=var: e.tensor_tensor(out=acc[:], in0=acc[:], in1=var[:], op=ALU.mult),
                           reads=[accB, varB], writes=[accB])
                        y, yB = yr.next()
                        op("act", lambda e, y=y, acc=acc, cc=cc: e.activation(
                            out=y[:], in_=acc[:], func=AF.Silu, scale=cvt[:, cc, 1:2], bias=cvt[:, cc, 2:3]),
                           reads=[accB, cvB], writes=[yB])
                        dma("sp", ymT[cc * 128:(cc + 1) * 128, t0:t0 + 512], y[:], reads=[yB])
                sc.emit_block()
                if stop == 'B1':
                    return nc

            with contextlib.ExitStack() as ph:
                qh, qhB = alloc(ph, "qh", [128, S], BF16)
                kh, khB = alloc(ph, "kh", [128, S], BF16)
                vh, vhB = alloc(ph, "vh", [128, NT, 128], BF16)
                biasT, biasTB = alloc(ph, "biasT", [128, NT, NT], F32)
                pr = Ring(ph, "pt", 4, [128, 512], BF16)
                rr = Ring(ph, "rl", 2, [128, 512], F32)
                orr = Ring(ph, "oo", 2, [128, 512], F32)
                fgt, fgB = alloc(ph, "fgt", [128, 8], F32)
                dma("sp", fgt[:], fg[l], writes=[fgB])
                for h in range(8):
                    dma("sp", qh[:], qT[h * 128:(h + 1) * 128, :], writes=[qhB])
                    dma("sp", kh[:], kT[h * 128:(h + 1) * 128, :], writes=[khB])
                    for v0 in range(0, NT, 16):
                        v1 = min(NT, v0 + 16)
                        dma("sp", vh[:, v0:v1, :], vS.rearrange("(t p) c -> p t c", p=128)[:, v0:v1, h * 128:(h + 1) * 128],
                            writes=[vhB])
                    for t in range(NT):
                        op("dve", lambda e, t=t, h=h: e.tensor_scalar(
                            out=biasT[:, t, :], in0=negcum[:, :, h], scalar1=rmid[:, t, h:h + 1], scalar2=None,
                            op0=ALU.subtract), reads=[negcumB, rmidB], writes=[biasTB])
                    for I in range(S // 512):
                        (po, poB), (pl, plB) = lps2()
                        nJ = I * 4 + 4
                        scores = {}

                        def issue_scores(J, I=I):
                            pS, pSB = nps()
                            op("pe", lambda e, pS=pS, J=J, I=I: e.matmul(
                                pS[:], lhsT=kh[:, J * 128:(J + 1) * 128], rhs=qh[:, I * 512:(I + 1) * 512],
                                start=True, stop=True), reads=[khB, qhB], writes=[pSB])
                            scores[J] = (pS, pSB)
                        for J in range(min(2, nJ)):
                            issue_scores(J)
                        for J in range(nJ):
                            if J + 2 < nJ:
                                issue_scores(J + 2)
                            pS, pSB = scores.pop(J)
                            pt, ptB = pr.next()
                            for i4 in range(4):
                                t = I * 4 + i4
                                blk = pt[:, i4 * 128:(i4 + 1) * 128]
                                if J > t:
                                    op("pool", lambda e, blk=blk: e.memset(blk, 0.0), writes=[ptB])
                                    continue
                                bcol = biasT[:, t, J:J + 1]
                                op("act", lambda e, blk=blk, pS=pS, i4=i4, bcol=bcol: e.activation(
                                    out=blk, in_=pS[:, i4 * 128:(i4 + 1) * 128], func=AF.Exp, bias=bcol),
                                   reads=[pSB, biasTB], writes=[ptB])
                                if J == t:
                                    op("pool", lambda e, blk=blk: e.affine_select(
                                        out=blk, in_=blk, pattern=[[1, 128]], compare_op=ALU.is_ge, fill=0.0,
                                        base=0, channel_multiplier=-1), reads=[ptB], writes=[ptB])
                            op("pe", lambda e, po=po, pt=pt, J=J, nJ=nJ: e.matmul(
                                po[:], lhsT=vh[:, J, :], rhs=pt[:], start=(J == 0), stop=(J == nJ - 1)),
                               reads=[vhB, ptB], writes=[poB])
                            op("pe", lambda e, pl=pl, pt=pt, J=J, nJ=nJ: e.matmul(
                                pl[:], lhsT=ones16[:], rhs=pt[:], start=(J == 0), stop=(J == nJ - 1)),
                               reads=[ones16B, ptB], writes=[plB])
                        rl, rlB = rr.next()
                        op("dve", lambda e, rl=rl, pl=pl: e.reciprocal(out=rl[:], in_=pl[:]), reads=[plB], writes=[rlB])
                        oo, ooB = orr.next()
                        op("dve", lambda e, oo=oo, po=po, rl=rl: e.tensor_tensor(out=oo[:], in0=po[:], in1=rl[:], op=ALU.mult),
                           reads=[poB, rlB], writes=[ooB])
                        dma("sp", oT[h * 128:(h + 1) * 128, I * 512:(I + 1) * 512], oo[:], reads=[ooB])
                sc.emit_block()
                if stop == 'B2':
                    return nc

            with contextlib.ExitStack() as ph:
                fgt, fgB = alloc(ph, "fgt2", [128, 8], F32)
                o8r = Ring(ph, "o8", 2, [128, 8, 512], F32)
                sqr = Ring(ph, "sq2", 2, [128, 512], F32)
                rsr = Ring(ph, "rs2", 2, [128, 512], F32)
                yr = Ring(ph, "yy2", 3, [128, 512], BF16)
                dma("sp", fgt[:], fg[l], writes=[fgB])
                for tb in range(S // 512):
                    t0 = tb * 512
                    o8, o8B = o8r.next()
                    dma("sp", o8[:], oT.rearrange("(h p) s -> p h s", p=128)[:, :, t0:t0 + 512], writes=[o8B])
                    (pq, pqB), _unused = lps2()
                    for h in range(8):
                        sq, sqB = sqr.next()
                        op("act", lambda e, sq=sq, o8=o8, h=h: e.activation(out=sq[:], in_=o8[:, h, :], func=AF.Square),
                           reads=[o8B], writes=[sqB])
                        op("pe", lambda e, sq=sq, pq=pq, h=h: e.matmul(pq[:], lhsT=ones[:], rhs=sq[:],
                                                                     start=(h == 0), stop=(h == 7)),
                           reads=[onesB, sqB], writes=[pqB])
                    rs, rsB = rsr.next()
                    rstd_from(rs[:], rsB, pq[:], pqB, 1.0 / 1024, rs[:], rsB)
                    for h in range(8):
                        y, yB = yr.next()
                        op("dve", lambda e, y=y, o8=o8, h=h, rs=rs: e.scalar_tensor_tensor(
                            out=y[:], in0=o8[:, h, :], scalar=fgt[:, h:h + 1], in1=rs[:], op0=ALU.mult, op1=ALU.mult),
                           reads=[o8B, fgB, rsB], writes=[yB])
                        dma("sp", ymT[512 + h * 128:512 + (h + 1) * 128, t0:t0 + 512], y[:], reads=[yB])
                sc.emit_block()
                if stop == 'B2b':
                    return nc

            with contextlib.ExitStack() as ph:
                lwt, lwB = alloc(ph, "lwt", [128, 4, 4], F32)
                lvt, lvB = alloc(ph, "lvt", [128, 4, 5], F32)
                nsp, nspB = alloc(ph, "nsp", [128, 4, 2], F32)
                wrt, wrB = alloc(ph, "wrt", [128, 4, 128], F32)
                wit, wiB = alloc(ph, "wit", [128, 4, 128], F32)
                carry, carryB = alloc(ph, "carry", [128, 4], F32)
                xr_ = Ring(ph, "lx", 3, [128, 3 + 512], F32)
                gr_ = Ring(ph, "lg", 3, [128, 512], F32)
                xcr = Ring(ph, "xc", 3, [128, 512], F32)
                rr_ = Ring(ph, "rr", 2, [128, 512], F32)
                ir_ = Ring(ph, "ii", 2, [128, 512], F32)
                ar_ = Ring(ph, "aa", 2, [128, 512], F32)
                br_ = Ring(ph, "bb", 2, [128, 512], F32)
                hr_ = Ring(ph, "hh", 2, [128, 512], F32)
                mr_ = Ring(ph, "mm3", 8, [128, 512], F32)
                sqr = Ring(ph, "sq3", 2, [128, 512], F32)
                rsr = Ring(ph, "rs3", 2, [128, 512], F32)
                yr = Ring(ph, "yy3", 3, [128, 512], BF16)
                for cc in range(4):
                    dma("sp", lwt[:, cc, :], lw[l, cc], writes=[lwB])
                    dma("sp", lvt[:, cc, :], lvec[l, cc], writes=[lvB])
                    dma("sp", wrt[:, cc, :], lwr[l, cc], writes=[wrB])
                    dma("sp", wit[:, cc, :], lwi[l, cc], writes=[wiB])
                op("act", lambda e: e.activation(out=nsp[:, :, 0], in_=lvt[:, :, 3], func=AF.Exp, scale=-1.0),
                   reads=[lvB], writes=[nspB])
                op("act", lambda e: e.activation(out=nsp[:, :, 0], in_=nsp[:, :, 0], func=AF.Ln, bias=1.0),
                   reads=[nspB], writes=[nspB])
                op("dve", lambda e: e.tensor_scalar(out=nsp[:, :, 1], in0=nsp[:, :, 0], scalar1=-16.0, scalar2=None, op0=ALU.mult),
                   reads=[nspB], writes=[nspB])
                op("dve", lambda e: e.tensor_scalar(out=nsp[:, :, 0], in0=nsp[:, :, 0], scalar1=-8.0, scalar2=None, op0=ALU.mult),
                   reads=[nspB], writes=[nspB])
                op("pool", lambda e: e.memset(carry[:], 0.0), writes=[carryB])
                for tb in range(S // 512):
                    t0 = tb * 512
                    (pq, pqB), _unused = lps2()
                    ms = []
                    for cc in range(4):
                        lx, lxB = xr_.next()
                        if tb == 0:
                            op("pool", lambda e, lx=lx: e.memset(lx[:, 0:3], 0.0), writes=[lxB])
                            dma("sp", lx[:, 3:515], lxT[cc * 128:(cc + 1) * 128, 0:512], writes=[lxB])
                        else:
                            dma("sp", lx[:], lxT[cc * 128:(cc + 1) * 128, t0 - 3:t0 + 512], writes=[lxB])
                        lg_, lgB = gr_.next()
                        dma("sp", lg_[:], lgT[cc * 128:(cc + 1) * 128, t0:t0 + 512], writes=[lgB])
                        xc, xcB = xcr.next()
                        op("dve", lambda e, xc=xc, lx=lx, cc=cc: e.tensor_scalar(
                            out=xc[:], in0=lx[:, 0:512], scalar1=lwt[:, cc, 0:1], scalar2=lvt[:, cc, 0:1],
                            op0=ALU.mult, op1=ALU.add), reads=[lxB, lwB, lvB], writes=[xcB])
                        for k in range(1, 4):
                            op("dve", lambda e, xc=xc, lx=lx, cc=cc, k=k: e.scalar_tensor_tensor(
                                out=xc[:], in0=lx[:, k:k + 512], scalar=lwt[:, cc, k:k + 1], in1=xc[:],
                                op0=ALU.mult, op1=ALU.add), reads=[lxB, lwB, xcB], writes=[xcB])
                        pr_, prB = nps()
                        pi_, piB = nps()
                        op("pe", lambda e, pr_=pr_, xc=xc, cc=cc: e.matmul(pr_[:], lhsT=wrt[:, cc, :], rhs=xc[:], start=True, stop=True),
                           reads=[wrB, xcB], writes=[prB])
                        op("pe", lambda e, pi_=pi_, xc=xc, cc=cc: e.matmul(pi_[:], lhsT=wit[:, cc, :], rhs=xc[:], start=True, stop=True),
                           reads=[wiB, xcB], writes=[piB])
                        r_, rB = rr_.next()
                        i_, iB = ir_.next()
                        op("act", lambda e, r_=r_, pr_=pr_, cc=cc: e.activation(out=r_[:], in_=pr_[:], func=AF.Sigmoid,
                                                                             bias=lvt[:, cc, 1:2]),
                           reads=[prB, lvB], writes=[rB])
                        op("act", lambda e, i_=i_, pi_=pi_, cc=cc: e.activation(out=i_[:], in_=pi_[:], func=AF.Sigmoid,
                                                                             bias=lvt[:, cc, 2:3]),
                           reads=[piB, lvB], writes=[iB])
                        a_, aB = ar_.next()
                        b_, bB = br_.next()
                        op("act", lambda e, a_=a_, r_=r_, cc=cc: e.activation(out=a_[:], in_=r_[:], func=AF.Exp,
                                                                           scale=nsp[:, cc, 0:1]),
                           reads=[rB, nspB], writes=[aB])
                        op("act", lambda e, b_=b_, r_=r_, cc=cc: e.activation(out=b_[:], in_=r_[:], func=AF.Exp,
                                                                           scale=nsp[:, cc, 1:2]),
                           reads=[rB, nspB], writes=[bB])
                        op("dve", lambda e, b_=b_: e.tensor_scalar(out=b_[:], in0=b_[:], scalar1=-1.0, scalar2=1.0,
                                                                  op0=ALU.mult, op1=ALU.add), reads=[bB], writes=[bB])
                        op("dve", lambda e, b_=b_: e.tensor_scalar(out=b_[:], in0=b_[:], scalar1=0.0, scalar2=None,
                                                                  op0=ALU.max), reads=[bB], writes=[bB])
                        op("act", lambda e, b_=b_: e.activation(out=b_[:], in_=b_[:], func=AF.Sqrt), reads=[bB], writes=[bB])
                        op("pool", lambda e, i_=i_, xc=xc: e.tensor_tensor(out=i_[:], in0=i_[:], in1=xc[:], op=ALU.mult),
                           reads=[iB, xcB], writes=[iB])
                        op("pool", lambda e, b_=b_, i_=i_: e.tensor_tensor(out=b_[:], in0=b_[:], in1=i_[:], op=ALU.mult),
                           reads=[bB, iB], writes=[bB])
                        hh, hhB = hr_.next()
                        op("dve", lambda e, hh=hh, a_=a_, b_=b_, cc=cc: e.tensor_tensor_scan(
                            out=hh[:], data0=a_[:], data1=b_[:], initial=carry[:, cc:cc + 1], op0=ALU.mult, op1=ALU.add),
                           reads=[aB, bB, carryB], writes=[hhB])
                        op("dve", lambda e, hh=hh, cc=cc: e.tensor_copy(out=carry[:, cc:cc + 1], in_=hh[:, 511:512]),
                           reads=[hhB], writes=[carryB])
                        m_, mB = mr_.next()
                        op("act", lambda e, m_=m_, lg_=lg_: e.activation(out=m_[:], in_=lg_[:], func=AF.Square),
                           reads=[lgB], writes=[mB])
                        op("dve", lambda e, m_=m_: e.tensor_scalar(out=m_[:], in0=m_[:], scalar1=0.044715, scalar2=1.0,
                                                                  op0=ALU.mult, op1=ALU.add), reads=[mB], writes=[mB])
                        op("pool", lambda e, m_=m_, lg_=lg_: e.tensor_tensor(out=m_[:], in0=m_[:], in1=lg_[:], op=ALU.mult),
                           reads=[mB, lgB], writes=[mB])
                        op("act", lambda e, m_=m_: e.activation(out=m_[:], in_=m_[:], func=AF.Sigmoid, scale=1.5957691216),
                           reads=[mB], writes=[mB])
                        op("pool", lambda e, m_=m_, lg_=lg_: e.tensor_tensor(out=m_[:], in0=m_[:], in1=lg_[:], op=ALU.mult),
                           reads=[mB, lgB], writes=[mB])
                        op("pool", lambda e, m_=m_, hh=hh: e.tensor_tensor(out=m_[:], in0=m_[:], in1=hh[:], op=ALU.mult),
                           reads=[mB, hhB], writes=[mB])
                        sq, sqB = sqr.next()
                        op("act", lambda e, sq=sq, m_=m_: e.activation(out=sq[:], in_=m_[:], func=AF.Square),
                           reads=[mB], writes=[sqB])
                        op("pe", lambda e, sq=sq, pq=pq, cc=cc: e.matmul(pq[:], lhsT=ones[:], rhs=sq[:],
                                                                       start=(cc == 0), stop=(cc == 3)),
                           reads=[onesB, sqB], writes=[pqB])
                        ms.append((m_, mB))
                    rs, rsB = rsr.next()
                    rstd_from(rs[:], rsB, pq[:], pqB, 1.0 / 512, rs[:], rsB)
                    for cc in range(4):
                        m_, mB = ms[cc]
                        y, yB = yr.next()
                        op("dve", lambda e, y=y, m_=m_, cc=cc, rs=rs: e.scalar_tensor_tensor(
                            out=y[:], in0=m_[:], scalar=lvt[:, cc, 4:5], in1=rs[:], op0=ALU.mult, op1=ALU.mult),
                           reads=[mB, lvB, rsB], writes=[yB])
                        dma("sp", ymT[1536 + cc * 128:1536 + (cc + 1) * 128, t0:t0 + 512], y[:], reads=[yB])
                sc.emit_block()
                if stop == 'B3':
                    return nc

            with contextlib.ExitStack() as ph:
                wo, woB = alloc(ph, "wo", [128, KC, D], BF16)
                g1, g1B = alloc(ph, "g1", [128, D], F32)
                A2, A2B = alloc(ph, "A2", [128, D], F32)
                sh2, sh2B = alloc(ph, "sh2", [128, D], F32)
                wrt32, wrtB = alloc(ph, "wrt32", [128, KC, 36], F32)
                brt, brtB = alloc(ph, "brt", [128, 36], F32)
                ymr = Ring(ph, "ym", 1, [128, KC, 512], BF16)
                xr = Ring(ph, "xc_", 2, [128, D], F32)
                tr = Ring(ph, "tc_", 1, [128, D], F32)
                hr = Ring(ph, "hc_", 1, [128, D], F32)
                sr = Ring(ph, "sc_", 4, [128, 4], F32)
                h2r = Ring(ph, "h2", 2, [128, KC, 128], BF16)
                h32r = Ring(ph, "h32", 1, [128, KC, 128], F32)
                rtr = Ring(ph, "rt", 2, [128, 64], F32)
                arr = Ring(ph, "ar", 2, [128, E], F32)
                op("pool", lambda e: e.memset(cnt[:], 0.0), writes=[cntB])
                for k4 in range(4):
                    dma("pool", wo[:, k4 * 4:(k4 + 1) * 4, :], w_out[l, :, k4 * 4:(k4 + 1) * 4, :], writes=[woB])
                dma("sp", g1[:], mods[l, :, 2, :], writes=[g1B])
                dma("sp", A2[:], mods[l, :, 3, :], writes=[A2B])
                dma("sp", sh2[:], mods[l, :, 4, :], writes=[sh2B])
                dma("sp", wrt32[:], w_rt[l], writes=[wrtB])
                dma("sp", brt[:], b_rt[l], writes=[brtB])
                for tg in range(S // 512):
                    ym, ymB = ymr.next()
                    dma("sp", ym[:], ymT.rearrange("(k p) s -> p k s", p=128)[:, :, tg * 512:(tg + 1) * 512], writes=[ymB])
                    for tt in range(4):
                        t = tg * 4 + tt
                        xt, xB = xr.next()
                        tmp, tmpB = tr.next()
                        h32, hB = hr.next()
                        st4, st4B = sr.next()
                        dma("sp", xt[:], xsrc[t * 128:(t + 1) * 128, :], writes=[xB])
                        if C_LEVEL < 1:
                            continue
                        for cg in range(4):
                            p, pB = nps()
                            for k in range(KC):
                                op("pe", lambda e, p=p, k=k, ym=ym, tt=tt, cg=cg: e.matmul(
                                    p[:], lhsT=ym[:, k, tt * 128:(tt + 1) * 128], rhs=wo[:, k, cg * 512:(cg + 1) * 512],
                                    start=(k == 0), stop=(k == KC - 1)), reads=[ymB, woB], writes=[pB])
                            op("dve", lambda e, p=p, tmp=tmp, cg=cg: e.tensor_tensor(
                                out=tmp[:, cg * 512:(cg + 1) * 512], in0=p[:], in1=g1[:, cg * 512:(cg + 1) * 512], op=ALU.mult),
                               reads=[pB, g1B], writes=[tmpB])
                        if "pool" not in C_SKIP:
                            op("pool", lambda e, xt=xt, tmp=tmp: e.tensor_tensor(out=xt[:], in0=xt[:], in1=tmp[:], op=ALU.add),
                               reads=[xB, tmpB], writes=[xB])
                        if "xs" not in C_SKIP:
                            dma("sp", (out if "toout" in C_SKIP else xs)[t * 128:(t + 1) * 128, :], xt[:], reads=[xB])
                        if C_LEVEL < 2:
                            continue
                        op("act", lambda e, xt=xt, st4=st4, h32=h32: e.activation(out=h32[:], in_=xt[:], func=AF.Square,
                                                                         accum_out=st4[:, 0:1]),
                           reads=[xB], writes=[hB, st4B])
                        rstd_from(st4[:, 1:2], st4B, st4[:, 0:1], st4B, 1.0 / D, st4[:, 2:3], st4B)
                        if C_LEVEL < 1.3:
                            continue
                        op("dve", lambda e, xt=xt, st4=st4, tmp=tmp: e.scalar_tensor_tensor(
                            out=tmp[:], in0=xt[:], scalar=st4[:, 1:2], in1=A2[:], op0=ALU.mult, op1=ALU.mult),
                           reads=[xB, st4B, A2B], writes=[tmpB])
                        op("pool", lambda e, tmp=tmp, h32=h32: e.tensor_tensor(out=h32[:], in0=tmp[:], in1=sh2[:], op=ALU.add),
                           reads=[tmpB, sh2B], writes=[hB])
                        if C_LEVEL < 1.6:
                            continue
                        dma("sp", h2S[t * 128:(t + 1) * 128, :], h32[:], reads=[hB])
                        h2, h2B = h2r.next()
                        hT32, hT32B = h32r.next()
                        for q4 in range(4):
                            p, pB = nps()
                            for i in range(4):
                                kc = q4 * 4 + i
                                op("pe", lambda e, p=p, i=i, kc=kc, h32=h32: e.transpose(
                                    p[:, i * 128:(i + 1) * 128], h32[:, kc * 128:(kc + 1) * 128], ident[:]),
                                   reads=[hB, identB], writes=[pB])
                            op("act", lambda e, p=p, q4=q4, hT32=hT32: e.activation(
                                out=hT32[:, q4 * 4:(q4 + 1) * 4, :].rearrange("p a b -> p (a b)"), in_=p[:], func=AF.Copy),
                               reads=[pB], writes=[hT32B])
                        op("pool", lambda e, h2=h2, hT32=hT32: e.tensor_copy(out=h2[:], in_=hT32[:]),
                           reads=[hT32B], writes=[h2B])
                        if C_LEVEL < 3:
                            continue
                        dma("sp", h2T.rearrange("(k p) s -> p k s", p=128)[:, :, t * 128:(t + 1) * 128], h2[:], reads=[h2B])
                        if SKIP_ROUTER:
                            continue
                        p, pB = nps()
                        for k in range(KC):
                            op("pe", lambda e, p=p, k=k, hT32=hT32: e.matmul(
                                p[:, 0:36], lhsT=hT32[:, k, :], rhs=wrt32[:, k, :], start=(k == 0), stop=(k == KC - 1)),
                               reads=[hT32B, wrtB], writes=[pB])
                        rt, rtB = rtr.next()
                        op("dve", lambda e, rt=rt, p=p: e.tensor_tensor(out=rt[:, 0:36], in0=p[:, 0:36], in1=brt[:], op=ALU.add),
                           reads=[pB, brtB], writes=[rtB])
                        op("dve", lambda e, rt=rt: e.tensor_reduce(out=rt[:, 56:57], in_=rt[:, 0:4], axis=AX.X, op=ALU.max),
                           reads=[rtB], writes=[rtB])
                        op("dve", lambda e, rt=rt: e.tensor_scalar(out=rt[:, 36:40], in0=rt[:, 0:4], scalar1=rt[:, 56:57],
                                                                  scalar2=None, op0=ALU.is_equal), reads=[rtB], writes=[rtB])
                        op("dve", lambda e, rt=rt: e.tensor_scalar(out=rt[:, 57:58], in0=rt[:, 56:57], scalar1=-1.0, scalar2=None,
                                                                  op0=ALU.mult), reads=[rtB], writes=[rtB])
                        op("act", lambda e, rt=rt: e.activation(out=rt[:, 60:64], in_=rt[:, 0:4], func=AF.Exp, bias=rt[:, 57:58],
                                                               accum_out=rt[:, 58:59]), reads=[rtB], writes=[rtB])
                        op("dve", lambda e, rt=rt: e.reciprocal(out=rt[:, 58:59], in_=rt[:, 58:59]), reads=[rtB], writes=[rtB])
                        op("dve", lambda e, rt=rt: e.tensor_scalar(out=rt[:, 40:48], in0=rt[:, 4:12], scalar1=rt[:, 36:37],
                                                                  scalar2=None, op0=ALU.mult), reads=[rtB], writes=[rtB])
                        for gi in range(1, 4):
                            op("dve", lambda e, rt=rt, gi=gi: e.scalar_tensor_tensor(
                                out=rt[:, 40:48], in0=rt[:, 4 + gi * 8:12 + gi * 8], scalar=rt[:, 36 + gi:37 + gi],
                                in1=rt[:, 40:48], op0=ALU.mult, op1=ALU.add), reads=[rtB], writes=[rtB])
                        op("dve", lambda e, rt=rt: e.max(out=rt[:, 48:56], in_=rt[:, 40:48]), reads=[rtB], writes=[rtB])
                        op("dve", lambda e, rt=rt: e.tensor_tensor(out=rt[:, 59:60], in0=rt[:, 49:50], in1=rt[:, 48:49], op=ALU.subtract),
                           reads=[rtB], writes=[rtB])
                        op("act", lambda e, rt=rt: e.activation(out=rt[:, 59:60], in_=rt[:, 59:60], func=AF.Exp),
                           reads=[rtB], writes=[rtB])
                        op("dve", lambda e, rt=rt: e.tensor_scalar(out=rt[:, 60:61], in0=rt[:, 59:60], scalar1=1.0, scalar2=None,
                                                                  op0=ALU.add), reads=[rtB], writes=[rtB])
                        op("dve", lambda e, rt=rt: e.reciprocal(out=rt[:, 60:61], in_=rt[:, 60:61]), reads=[rtB], writes=[rtB])
                        op("dve", lambda e, rt=rt: e.tensor_tensor(out=rt[:, 60:61], in0=rt[:, 60:61], in1=rt[:, 58:59], op=ALU.mult),
                           reads=[rtB], writes=[rtB])
                        op("dve", lambda e, rt=rt: e.tensor_tensor(out=rt[:, 61:62], in0=rt[:, 60:61], in1=rt[:, 59:60], op=ALU.mult),
                           reads=[rtB], writes=[rtB])
                        op("dve", lambda e, rt=rt: e.tensor_scalar(out=rt[:, 62:63], in0=rt[:, 48:49], scalar1=1.0, scalar2=None,
                                                                  op0=ALU.mult), reads=[rtB], writes=[rtB])
                        op("dve", lambda e, rt=rt: e.tensor_scalar(out=rt[:, 63:64], in0=rt[:, 49:50], scalar1=1.0, scalar2=None,
                                                                  op0=ALU.mult), reads=[rtB], writes=[rtB])
                        op("dve", lambda e, rt=rt: e.tensor_scalar(out=rt[:, 48:56], in0=rt[:, 40:48], scalar1=rt[:, 62:63],
                                                                  scalar2=rt[:, 60:61], op0=ALU.is_equal, op1=ALU.mult),
                           reads=[rtB], writes=[rtB])
                        op("dve", lambda e, rt=rt: e.tensor_scalar(out=rt[:, 40:48], in0=rt[:, 40:48], scalar1=rt[:, 63:64],
                                                                  scalar2=rt[:, 61:62], op0=ALU.is_equal, op1=ALU.mult),
                           reads=[rtB], writes=[rtB])
                        op("dve", lambda e, rt=rt: e.tensor_tensor(out=rt[:, 48:56], in0=rt[:, 48:56], in1=rt[:, 40:48], op=ALU.add),
                           reads=[rtB], writes=[rtB])
                        for gi in range(4):
                            op("dve", lambda e, rt=rt, gi=gi, t=t: e.tensor_scalar(
                                out=Gt[:, t, gi * 8:(gi + 1) * 8], in0=rt[:, 48:56], scalar1=rt[:, 36 + gi:37 + gi],
                                scalar2=None, op0=ALU.mult), reads=[rtB], writes=[GtB])
                        ar, arB = arr.next()
                        op("dve", lambda e, ar=ar, t=t: e.tensor_scalar(out=ar[:], in0=Gt[:, t, :], scalar1=0.0, scalar2=None,
                                                                        op0=ALU.is_gt), reads=[GtB], writes=[arB])
                        p2, p2B = nps()
                        op("pe", lambda e, p2=p2, ar=ar: e.matmul(p2[:, 0:E], lhsT=utri[:], rhs=ar[:], start=True, stop=True),
                           reads=[utriB, arB], writes=[p2B])
                        op("pe", lambda e, p2=p2, ar=ar: e.matmul(p2[:, E:2 * E], lhsT=ones[:], rhs=ar[:], start=True, stop=True),
                           reads=[onesB, arB], writes=[p2B])
                        op("dve", lambda e, p2=p2, t=t: e.tensor_tensor(out=rankI[:, t, :], in0=p2[:, 0:E], in1=cnt[:], op=ALU.add),
                           reads=[p2B, cntB], writes=[rankIB])
                        op("dve", lambda e, p2=p2: e.tensor_tensor(out=cnt[:], in0=p2[:, E:2 * E], in1=cnt[:], op=ALU.add),
                           reads=[p2B, cntB], writes=[cntB])
                sc.emit_block()
                if stop == 'C':
                    return nc

            with contextlib.ExitStack() as ph:
                thri, thriB = alloc(ph, "thri", [128, 64], I32)
                thr, thrB = alloc(ph, "thr", [128, 64], F32)
                cmpt, cmpB = alloc(ph, "cmpt", [128, E, 64], F32)
                nblk, nblkB = alloc(ph, "nblk", [128, E], F32)
                pend, pendB = alloc(ph, "pend", [128, E], F32)
                pst, pstB = alloc(ph, "pst", [128, E], F32)
                bthi, bthiB = alloc(ph, "bthi", [128, NB], I32)
                bth, bthB = alloc(ph, "bth", [128, NB], F32)
                be, beB = alloc(ph, "be", [128, NB], F32)
                zt, ztB = alloc(ph, "zt", [128, NB * 2], F32)
                vr_ = Ring(ph, "vv4", 2, [128, E], F32)
                ar_ = Ring(ph, "aa4", 2, [128, E], F32)
                mr_ = Ring(ph, "mm4", 2, [128, E], F32)
                t8r = Ring(ph, "t84", 2, [128, 8], F32)
                scr_ = Ring(ph, "sc4", 4, [128, 4], F32)
                dfr = Ring(ph, "df4", 2, [128, 2], F32)
                op("pool", lambda e: e.memset(zt[:], 0.0), writes=[ztB])
                dma("sp", tab.rearrange("(p r) c -> p (r c)", p=128), zt[:], reads=[ztB], writes=[tabB])
                op("pool", lambda e: e.iota(out=thri[:], pattern=[[128, 64]], base=0, channel_multiplier=0), writes=[thriB])
                op("dve", lambda e: e.tensor_copy(out=thr[:], in_=thri[:]), reads=[thriB], writes=[thrB])
                op("pool", lambda e: e.iota(out=bthi[:], pattern=[[128, NB]], base=0, channel_multiplier=0), writes=[bthiB])
                op("dve", lambda e: e.tensor_copy(out=bth[:], in_=bthi[:]), reads=[bthiB], writes=[bthB])
                for ex in range(E):
                    op("dve", lambda e, ex=ex: e.tensor_scalar(out=cmpt[:, ex, :], in0=thr[:], scalar1=cnt[:, ex:ex + 1],
                                                                scalar2=None, op0=ALU.is_lt), reads=[thrB, cntB], writes=[cmpB])
                op("dve", lambda e: e.tensor_reduce(out=nblk[:], in_=cmpt[:], axis=AX.X, op=ALU.add), reads=[cmpB], writes=[nblkB])
                op("dve", lambda e: e.tensor_tensor_scan(out=pend[:], data0=ones[:, 0:E], data1=nblk[:], initial=0.0,
                                                         op0=ALU.mult, op1=ALU.add), reads=[onesB, nblkB], writes=[pendB])
                op("dve", lambda e: e.tensor_scalar(out=pend[:], in0=pend[:], scalar1=128.0, scalar2=None, op0=ALU.mult),
                   reads=[pendB], writes=[pendB])
                op("dve", lambda e: e.scalar_tensor_tensor(out=pst[:], in0=nblk[:], scalar=-128.0, in1=pend[:],
                                                           op0=ALU.mult, op1=ALU.add), reads=[nblkB, pendB], writes=[pstB])
                op("pool", lambda e: e.memset(be[:], 0.0), writes=[beB])
                for ex in range(E):
                    op("dve", lambda e, ex=ex: e.scalar_tensor_tensor(out=be[:], in0=bth[:], scalar=pend[:, ex:ex + 1], in1=be[:],
                                                                      op0=ALU.is_ge, op1=ALU.add), reads=[bthB, pendB, beB], writes=[beB])
                op("dve", lambda e: e.tensor_scalar(out=be[:], in0=be[:], scalar1=float(E - 1), scalar2=None, op0=ALU.min),
                   reads=[beB], writes=[beB])
                op("dve", lambda e: e.tensor_scalar(out=be[:], in0=be[:], scalar1=128.0, scalar2=pidx[:, 0:1], op0=ALU.mult, op1=ALU.add),
                   reads=[beB, pidxB], writes=[beB])
                op("dve", lambda e: e.tensor_copy(out=widx[:], in_=be[:]), reads=[beB], writes=[widxB])
                for t in range(NT):
                    a_, aB = ar_.next()
                    v_, vB = vr_.next()
                    op("dve", lambda e, a_=a_, t=t: e.tensor_scalar(out=a_[:], in0=Gt[:, t, :], scalar1=0.0, scalar2=None, op0=ALU.is_gt),
                       reads=[GtB], writes=[aB])
                    op("dve", lambda e, v_=v_, t=t: e.tensor_tensor(out=v_[:], in0=rankI[:, t, :], in1=pst[:], op=ALU.add),
                       reads=[rankIB, pstB], writes=[vB])
                    op("dve", lambda e, v_=v_, a_=a_: e.tensor_tensor(out=v_[:], in0=v_[:], in1=a_[:], op=ALU.mult),
                       reads=[vB, aB], writes=[vB])
                    t8, t8B = t8r.next()
                    op("dve", lambda e, t8=t8, v_=v_: e.max(out=t8[:], in_=v_[:]), reads=[vB], writes=[t8B])
                    df, dfB = dfr.next()
                    op("dve", lambda e, df=df, t8=t8: e.tensor_scalar(out=df[:], in0=t8[:, 0:2], scalar1=-1.0, scalar2=None, op0=ALU.add),
                       reads=[t8B], writes=[dfB])
                    op("dve", lambda e, df=df, t=t: e.tensor_copy(out=d01[:, t, :], in_=df[:]), reads=[dfB], writes=[d01B])
                    for sl_ in range(2):
                        m_, mB = mr_.next()
                        s4, s4B = scr_.next()
                        op("dve", lambda e, m_=m_, v_=v_, t8=t8, sl_=sl_: e.tensor_scalar(
                            out=m_[:], in0=v_[:], scalar1=t8[:, sl_:sl_ + 1], scalar2=None, op0=ALU.is_equal),
                           reads=[vB, t8B], writes=[mB])
                        op("dve", lambda e, m_=m_, t=t: e.tensor_tensor(out=m_[:], in0=m_[:], in1=Gt[:, t, :], op=ALU.mult),
                           reads=[mB, GtB], writes=[mB])
                        op("dve", lambda e, m_=m_, s4=s4: e.tensor_reduce(out=s4[:, 1:2], in_=m_[:], axis=AX.X, op=ALU.add),
                           reads=[mB], writes=[s4B])
                        op("dve", lambda e, s4=s4, t=t: e.tensor_scalar(out=s4[:, 0:1], in0=pidx[:], scalar1=float(t * 128), scalar2=None,
                                                                        op0=ALU.add), reads=[pidxB], writes=[s4B])
                        sc.idma(tab, s4[:, 0:2], d01[:, t, sl_:sl_ + 1], scatter=True, reads=[s4B, d01B], writes=[tabB])
                sc.emit_block()
                if stop == 'D1':
                    return nc

            with contextlib.ExitStack() as ph:
                ps8i = [0]

                def nps8():
                    it = ps[ps8i[0] % 8]
                    ps8i[0] += 1
                    return it
                tbr = Ring(ph, "tb5", 3, [128, 2], F32)
                tir = Ring(ph, "ti5", 3, [128, 1], I32)
                xbr = Ring(ph, "xb5", 2, [128, D], F32)
                xtr = Ring(ph, "xt5", 2, [128, KC, 128], BF16)
                wgr = Ring(ph, "wg5", 2, [128, KC * DFF], BF16)
                wur = Ring(ph, "wu5", 2, [128, KC * DFF], BF16)
                wdr = Ring(ph, "wd5", 2, [128, NFC * D], BF16)
                her = Ring(ph, "he5", 2 * NFC, [128, 128], BF16)
                slr = Ring(ph, "sl5", 2, [128, 128], F32)
                ybr = Ring(ph, "yb5", 2, [128, D], F32)
                wg_rows = wg16.rearrange("e p k f -> (e p) (k f)")
                wu_rows = wu16.rearrange("e p k f -> (e p) (k f)")
                wd_rows = wd16.rearrange("e p c d -> (e p) (c d)")
                for b in range(NB):
                    tb, tbB = tbr.next()
                    ti, tiB = tir.next()
                    dma("sp", tb[:], tab[b * 128:(b + 1) * 128, :], reads=[tabB], writes=[tbB])
                    op("dve", lambda e, ti=ti, tb=tb: e.tensor_copy(out=ti[:], in_=tb[:, 0:1]), reads=[tbB], writes=[tiB])
                    xb, xbB = xbr.next()
                    sc.idma(xb[:], h2S, ti[:, 0:1], scatter=False, reads=[tiB], writes=[xbB])
                    wgt, wgB = wgr.next()
                    wut, wuB = wur.next()
                    wdt, wdB = wdr.next()
                    sc.idma(wgt[:], wg_rows, widx[:, b:b + 1], scatter=False, reads=[widxB], writes=[wgB])
                    sc.idma(wut[:], wu_rows, widx[:, b:b + 1], scatter=False, reads=[widxB], writes=[wuB])
                    sc.idma(wdt[:], wd_rows, widx[:, b:b + 1], scatter=False, reads=[widxB], writes=[wdB])
                    xT, xTB = xtr.next()
                    for q4 in range(4):
                        p, pB = nps8()
                        for i in range(4):
                            kc = q4 * 4 + i
                            op("pe", lambda e, p=p, i=i, kc=kc, xb=xb: e.transpose(
                                p[:, i * 128:(i + 1) * 128], xb[:, kc * 128:(kc + 1) * 128], ident[:]),
                               reads=[xbB, identB], writes=[pB])
                        op("act", lambda e, p=p, q4=q4, xT=xT: e.activation(
                            out=xT[:, q4 * 4:(q4 + 1) * 4, :].rearrange("p a b -> p (a b)"), in_=p[:], func=AF.Copy),
                           reads=[pB], writes=[xTB])
                    hes = []
                    for fc in range(NFC):
                        pg, pgB = nps8()
                        pu, puB = nps8()
                        for k in range(KC):
                            c0 = k * DFF + fc * 128
                            op("pe", lambda e, pg=pg, k=k, wgt=wgt, c0=c0, xT=xT: e.matmul(
                                pg[:, 0:128], lhsT=wgt[:, c0:c0 + 128], rhs=xT[:, k, :], start=(k == 0), stop=(k == KC - 1)),
                               reads=[wgB, xTB], writes=[pgB])
                        for k in range(KC):
                            c0 = k * DFF + fc * 128
                            op("pe", lambda e, pu=pu, k=k, wut=wut, c0=c0, xT=xT: e.matmul(
                                pu[:, 0:128], lhsT=wut[:, c0:c0 + 128], rhs=xT[:, k, :], start=(k == 0), stop=(k == KC - 1)),
                               reads=[wuB, xTB], writes=[puB])
                        sl, slB = slr.next()
                        op("act", lambda e, sl=sl, pg=pg: e.activation(out=sl[:], in_=pg[:, 0:128], func=AF.Silu),
                           reads=[pgB], writes=[slB])
                        he, heB = her.next()
                        op("dve", lambda e, he=he, sl=sl, pu=pu: e.tensor_tensor(out=he[:], in0=pu[:, 0:128], in1=sl[:], op=ALU.mult),
                           reads=[puB, slB], writes=[heB])
                        hes.append((he, heB))
                    yb, ybB = ybr.next()
                    for cg in range(4):
                        p, pB = nps8()
                        for fc in range(NFC):
                            he, heB = hes[fc]
                            c0 = fc * D + cg * 512
                            op("pe", lambda e, p=p, he=he, fc=fc, c0=c0, wdt=wdt: e.matmul(
                                p[:], lhsT=he[:], rhs=wdt[:, c0:c0 + 512], start=(fc == 0), stop=(fc == NFC - 1)),
                               reads=[heB, wdB], writes=[pB])
                        op("act", lambda e, p=p, yb=yb, cg=cg, tb=tb: e.activation(
                            out=yb[:, cg * 512:(cg + 1) * 512], in_=p[:], func=AF.Copy, scale=tb[:, 1:2]),
                           reads=[pB, tbB], writes=[ybB])
                    dma("sp", ysort[b * 128:(b + 1) * 128, :], yb[:], reads=[ybB])
                sc.emit_block()
                if stop == 'D2':
                    return nc

            with contextlib.ExitStack() as ph:
                g2, g2B = alloc(ph, "g2", [128, D], F32)
                y0r = Ring(ph, "y06", 2, [128, D], F32)
                y1r = Ring(ph, "y16", 2, [128, D], F32)
                xr = Ring(ph, "xd6", 2, [128, D], F32)
                dma("sp", g2[:], mods[l, :, 5, :], writes=[g2B])
                for t in range(NT):
                    y0, y0B = y0r.next()
                    y1, y1B = y1r.next()
                    xt, xB = xr.next()
                    sc.idma(y0[:], ysort, d01[:, t, 0:1], scatter=False, reads=[d01B], writes=[y0B])
                    sc.idma(y1[:], ysort, d01[:, t, 1:2], scatter=False, reads=[d01B], writes=[y1B])
                    dma("sp", xt[:], xs[t * 128:(t + 1) * 128, :], writes=[xB])
                    op("pool", lambda e, y0=y0, y1=y1: e.tensor_tensor(out=y0[:], in0=y0[:], in1=y1[:], op=ALU.add),
                       reads=[y0B, y1B], writes=[y0B])
                    op("dve", lambda e, y0=y0: e.tensor_tensor(out=y0[:], in0=y0[:], in1=g2[:], op=ALU.mult),
                       reads=[y0B, g2B], writes=[y0B])
                    op("pool", lambda e, y0=y0, xt=xt: e.tensor_tensor(out=xt[:], in0=xt[:], in1=y0[:], op=ALU.add),
                       reads=[y0B, xB], writes=[xB])
                    dma("sp", xs[t * 128:(t + 1) * 128, :], xt[:], reads=[xB])
                sc.emit_block()
                if stop == 'D':
                    return nc

        with contextlib.ExitStack() as ph:
            fgn, fgnB = alloc(ph, "fgn", [128, D], F32)
            xr = Ring(ph, "xe_", 3, [128, D], F32)
            sr = Ring(ph, "se_", 4, [128, 4], F32)
            junk, junkB = alloc(ph, "junkE", [128, D], F32)
            dma("sp", fgn[:], fing[:], writes=[fgnB])
            for t in range(NT):
                xt, xB = xr.next()
                st4, st4B = sr.next()
                dma("sp", xt[:], xs[t * 128:(t + 1) * 128, :], writes=[xB])
                op("act", lambda e, xt=xt, st4=st4: e.activation(out=junk[:], in_=xt[:], func=AF.Square, accum_out=st4[:, 0:1]),
                   reads=[xB], writes=[junkB, st4B])
                rstd_from(st4[:, 1:2], st4B, st4[:, 0:1], st4B, 1.0 / D, st4[:, 2:3], st4B)
                op("dve", lambda e, xt=xt, st4=st4: e.scalar_tensor_tensor(
                    out=xt[:], in0=xt[:], scalar=st4[:, 1:2], in1=fgn[:], op0=ALU.mult, op1=ALU.mult),
                   reads=[xB, st4B, fgnB], writes=[xB])
                dma("sp", out[t * 128:(t + 1) * 128, :], xt[:], reads=[xB])
            sc.emit_block()
            if stop == 'E':
                return nc
    return nc


def _layout(inp, S, L, DFF):
    f = lambda a: np.ascontiguousarray(a, dtype=np.float32)
    NFC = DFF // 128
    rep = lambda a: f(np.broadcast_to(a[:, None, :], (a.shape[0], 128, a.shape[1])))
    w_in = inp["w_in"]
    cols = []
    for i in range(4):
        cols += list(range(i * 128, (i + 1) * 128))
        cols += list(range(512 + i * 128, 512 + (i + 1) * 128))
    cols += list(range(1024, 2048)) + list(range(2048, 3072))
    cols += list(range(4104, 4616)) + list(range(4616, 5128))
    cols = np.asarray(cols)
    sh = {}
    sh["w_ada"] = f(inp["w_ada"].reshape(L, KC, 128, 24, 512).transpose(0, 3, 2, 1, 4))
    sh["b_ada"] = rep(inp["b_ada"])
    sh["ln1g"] = rep(inp["ln1_g"])
    sh["ln2g"] = rep(inp["ln2_g"])
    sh["fing"] = f(np.broadcast_to(inp["final_g"][None, :], (128, D)))
    sh["w_fm"] = f(w_in[:, :, cols].reshape(L, KC, 128, 32, 128).transpose(0, 3, 2, 1, 4))
    sh["w_v"] = f(w_in[:, :, 3072:4096].reshape(L, KC, 128, 2, 512).transpose(0, 3, 2, 1, 4))
    sh["w_f"] = f(w_in[:, :, 4096:4104].reshape(L, KC, 128, 8).transpose(0, 2, 1, 3))
    sh["fb"] = rep(inp["fox_f_bias"])
    sh["cw"] = f(inp["conv_dw_w"].transpose(0, 2, 1).reshape(L, 4, 128, 31))
    sh["cvec"] = f(np.stack([inp["conv_dw_b"], inp["conv_ln_g"], inp["conv_ln_b"]], axis=-1).reshape(L, 4, 128, 3))
    sh["fg"] = f(inp["fox_out_g"].reshape(L, 8, 128).transpose(0, 2, 1))
    sh["lw"] = f(inp["lru_conv_w"].transpose(0, 2, 1).reshape(L, 4, 128, 4))
    sh["lvec"] = f(np.stack([inp["lru_conv_b"], inp["lru_b_r"], inp["lru_b_i"], inp["lru_lambda"], inp["lru_out_g"]],
                            axis=-1).reshape(L, 4, 128, 5))
    for nm, src in (("lwr", inp["lru_w_r"]), ("lwi", inp["lru_w_i"])):
        bd = np.zeros((L, 4, 128, 128), np.float32)
        for n in range(8):
            cc, n2 = n // 2, n % 2
            bd[:, cc, n2 * 64:(n2 + 1) * 64, n2 * 64:(n2 + 1) * 64] = src[:, n]
        sh[nm] = bd
    sh["w_out"] = f(inp["w_out"].reshape(L, KC, 128, D).transpose(0, 2, 1, 3))
    wr = np.concatenate([inp["w_router_group"], inp["w_router_expert"].transpose(0, 2, 1, 3).reshape(L, D, 32)], axis=-1)
    sh["w_rt"] = f(wr.reshape(L, KC, 128, 36).transpose(0, 2, 1, 3))
    sh["b_rt"] = rep(np.concatenate([inp["b_router_group"], inp["b_router_expert"].reshape(L, 32)], axis=-1))
    sh["wg"] = f(inp["w_gate"].reshape(L, E, KC, 128, DFF).transpose(0, 1, 3, 2, 4))
    sh["wu"] = f(inp["w_up"].reshape(L, E, KC, 128, DFF).transpose(0, 1, 3, 2, 4))
    sh["wd"] = f(inp["w_down"].reshape(L, E, NFC, 128, D).transpose(0, 1, 3, 2, 4))
    maps = []
    for b in range(inp["x"].shape[0]):
        m = dict(sh)
        m["x"] = f(inp["x"][b])
        m["cT"] = f(inp["c"][b].reshape(KC, 128).T)
        maps.append(m)
    return maps


def kernel(**inputs):
    inp = {k: np.asarray(v) for k, v in inputs.items()}
    B, S, _ = inp["x"].shape
    L = inp["w_ada"].shape[0]
    DFF = inp["w_gate"].shape[-1]
    nc = build(S, L, DFF)
    maps = _layout(inp, S, L, DFF)
    res = run_bass_kernel_spmd(nc, maps, core_ids=list(range(B)))
    return np.stack([np.asarray(res.results[b]["out"]) for b in range(B)], axis=0).astype(np.float32)
```

```python
import contextlib
import numpy as np
import concourse.bass as bass
import concourse.mybir as mybir
from concourse.bass_utils import run_bass_kernel_spmd

F32 = mybir.dt.float32
BF16 = mybir.dt.bfloat16
I32 = mybir.dt.int32
AF = mybir.ActivationFunctionType
ALU = mybir.AluOpType
AX = mybir.AxisListType

EPOCH = 30000
N_DMA_SEMS = 36
D = 2048
KC = 16
E = 32
EPS = 1e-6
SKIP_ROUTER = False
C_LEVEL = 9
C_SKIP = set()


class Buf:
    __slots__ = ("w", "r")

    def __init__(self):
        self.w = None
        self.r = {}


class Sched:
    ENGS = ("pe", "act", "dve", "pool", "sp")
    ENGMAP = {"pe": "tensor", "act": "scalar", "dve": "vector", "pool": "gpsimd", "sp": "sync"}

    def __init__(self, nc, stack):
        self.nc = nc
        n_sems = {"pe": 24, "act": 8, "dve": 12, "pool": 6, "sp": 4}
        self.sems = {e: [stack.enter_context(nc.semaphore(f"s_{e}{i}")) for i in range(n_sems[e])]
                     for e in self.ENGS}
        self.cnt = {e: 0 for e in self.ENGS}
        self.q = {e: [] for e in self.ENGS}
        self.known = {e: {} for e in self.ENGS}
        self.dma_sems = [stack.enter_context(nc.semaphore(f"s_dma{i}")) for i in range(N_DMA_SEMS)]
        self.dma_val = [0] * N_DMA_SEMS
        self.dma_rr = 0
        self.own = {}
        for e in self.ENGS:
            for s in self.sems[e]:
                self.own[id(s)] = e

    def _deps(self, reads, writes):
        deps = []
        for b in reads:
            if b.w is not None:
                deps.append(b.w)
        for b in writes:
            if b.w is not None:
                deps.append(b.w)
            deps.extend(b.r.values())
        return deps

    def _commit(self, tok, reads, writes):
        k = id(tok[0])
        for b in reads:
            if k not in b.r or b.r[k][1] < tok[1]:
                b.r[k] = tok
        for b in writes:
            b.w = tok
            b.r = {}

    def _waits(self, eng, deps):
        need = {}
        kn = self.known[eng]
        for (sem, val) in deps:
            k = id(sem)
            if eng == "pe" and self.own.get(k) == "pe":
                continue
            if kn.get(k, 0) >= val:
                continue
            if k not in need or need[k][1] < val:
                need[k] = (sem, val)
        out = []
        for k, (sem, val) in need.items():
            kn[k] = val
            out.append((sem, val))
        return out

    def op(self, eng, fn, reads=(), writes=()):
        waits = self._waits(eng, self._deps(reads, writes))
        c = self.cnt[eng]
        sem = self.sems[eng][c // EPOCH]
        tok = (sem, (c % EPOCH) + 1)
        self.cnt[eng] = c + 1
        self.q[eng].append((waits, fn, sem, 1))
        self._commit(tok, reads, writes)
        return tok

    def dma(self, eng, out, in_, reads=(), writes=()):
        deps = self._deps(reads, writes)
        i = self.dma_rr
        self.dma_rr = (i + 1) % N_DMA_SEMS
        sem = self.dma_sems[i]
        if self.dma_val[i] > 0:
            deps.append((sem, self.dma_val[i]))
        waits = self._waits(eng, deps)
        self.dma_val[i] += 16
        tok = (sem, self.dma_val[i])

        def fn(e, out=out, in_=in_):
            return e.dma_start(out=out, in_=in_)
        self.q[eng].append((waits, fn, sem, 16))
        self._commit(tok, reads, writes)
        return tok

    def idma(self, out, in_, idx, scatter, reads=(), writes=()):
        deps = self._deps(reads, writes)
        i = self.dma_rr
        self.dma_rr = (i + 1) % N_DMA_SEMS
        sem = self.dma_sems[i]
        if self.dma_val[i] > 0:
            deps.append((sem, self.dma_val[i]))
        waits = self._waits("pool", deps)
        self.dma_val[i] += 16
        tok = (sem, self.dma_val[i])

        def fn(e, out=out, in_=in_, idx=idx, scatter=scatter):
            off = bass.IndirectOffsetOnAxis(ap=idx, axis=0)
            if scatter:
                return e.indirect_dma_start(out=out, out_offset=off, in_=in_, in_offset=None)
            return e.indirect_dma_start(out=out, out_offset=None, in_=in_, in_offset=off)
        self.q["pool"].append((waits, fn, sem, 16))
        self._commit(tok, reads, writes)
        return tok

    def all_tokens(self):
        toks = [(self.dma_sems[i], self.dma_val[i]) for i in range(N_DMA_SEMS) if self.dma_val[i] > 0]
        for e in self.ENGS:
            c = self.cnt[e]
            if c > 0:
                toks.append((self.sems[e][(c - 1) // EPOCH], ((c - 1) % EPOCH) + 1))
        return toks

    def emit_block(self):
        toks = self.all_tokens()
        fin = {e: self._waits(e, [t for t in toks if self.own.get(id(t[0])) != e]) for e in self.ENGS}
        with self.nc.Block() as block:
            for e in self.ENGS:
                def body(engobj, lst=self.q[e], extra=fin[e]):
                    for (waits, fn, sem, inc) in lst:
                        for (s, v) in waits:
                            engobj.wait_ge(s, v)
                        fn(engobj).then_inc(sem, inc)
                    for (s, v) in extra:
                        engobj.wait_ge(s, v)
                getattr(block, self.ENGMAP[e])(body)
        self.q = {e: [] for e in self.ENGS}


def build(S, L, DFF, stop=None, dbg=False):
    NT = S // 128
    NFC = DFF // 128
    TGA = min(1024, S)
    nc = bass.Bass("TRN2", target_bir_lowering=False)

    def din(name, shape, dt=F32):
        return nc.dram_tensor(name, shape, dt, kind="ExternalInput").ap()

    def dscr(name, shape, dt=F32):
        if dbg:
            return nc.dram_tensor(name, shape, dt, kind="ExternalOutput").ap()
        return nc.dram_tensor(name, shape, dt).ap()

    x_in = din("x", [S, D])
    cT = din("cT", [128, KC])
    w_ada = din("w_ada", [L, 24, 128, KC, 512])
    b_ada = din("b_ada", [L, 128, 6 * D])
    ln1g = din("ln1g", [L, 128, D])
    ln2g = din("ln2g", [L, 128, D])
    fing = din("fing", [128, D])
    w_fm = din("w_fm", [L, 32, 128, KC, 128])
    w_v = din("w_v", [L, 2, 128, KC, 512])
    w_f = din("w_f", [L, 128, KC, 8])
    fb = din("fb", [L, 128, 8])
    cw = din("cw", [L, 4, 128, 31])
    cvec = din("cvec", [L, 4, 128, 3])
    fg = din("fg", [L, 128, 8])
    lw = din("lw", [L, 4, 128, 4])
    lvec = din("lvec", [L, 4, 128, 5])
    lwr = din("lwr", [L, 4, 128, 128])
    lwi = din("lwi", [L, 4, 128, 128])
    w_out = din("w_out", [L, 128, KC, D])
    w_rt = din("w_rt", [L, 128, KC, 36])
    b_rt = din("b_rt", [L, 128, 36])
    wg = din("wg", [L, E, 128, KC, DFF])
    wu = din("wu", [L, E, 128, KC, DFF])
    wd = din("wd", [L, E, 128, NFC, D])
    out = nc.dram_tensor("out", [S, D], F32, kind="ExternalOutput").ap()

    xs = dscr("resid", [S, D])
    mods = dscr("mods", [L, 128, 6, D])
    gluT = dscr("gluT", [512, S])
    qT = dscr("qT", [1024, S], BF16)
    kT = dscr("kT", [1024, S], BF16)
    vS = dscr("vS", [S, 1024], BF16)
    lxT = dscr("lxT", [512, S])
    lgT = dscr("lgT", [512, S])
    oT = dscr("oT", [1024, S])
    ymT = dscr("ymT", [D, S], BF16)
    h2T = dscr("h2T", [D, S], BF16)
    NB = 2 * S // 128 + E
    h2S = dscr("h2S", [S, D])
    tab = dscr("rtab", [NB * 128, 2])
    ysort = dscr("ysort", [NB * 128, D])
    wg16 = dscr("wg16", [E, 128, KC, DFF], BF16)
    wu16 = dscr("wu16", [E, 128, KC, DFF], BF16)
    wd16 = dscr("wd16", [E, 128, NFC, D], BF16)

    with contextlib.ExitStack() as top:
        sc = Sched(nc, top)
        op, dma = sc.op, sc.dma

        uid = [0]

        def alloc(st, name, shape, dt):
            uid[0] += 1
            return st.enter_context(nc.sbuf_tensor(f"t{uid[0]}_{name}", shape, dt)), Buf()

        class Ring:
            def __init__(self, st, name, n, shape, dt):
                self.items = [alloc(st, f"{name}{i}", shape, dt) for i in range(n)]
                self.i = 0

            def next(self):
                it = self.items[self.i % len(self.items)]
                self.i += 1
                return it

        ps = [(top.enter_context(nc.psum_tensor(f"ps{i}", [128, 512], F32)), Buf()) for i in range(8)]
        psi = [0]

        def nps():
            it = ps[psi[0] % 4]
            psi[0] += 1
            return it

        lpi = [0]

        def lps2():
            k = 4 + 2 * (lpi[0] % 2)
            lpi[0] += 1
            return ps[k], ps[k + 1]

        ident, identB = alloc(top, "ident", [128, 128], F32)
        ones, onesB = alloc(top, "ones", [128, 128], F32)
        utri, utriB = alloc(top, "utri", [128, 128], F32)
        ones16, ones16B = alloc(top, "ones16", [128, 128], BF16)
        negcum, negcumB = alloc(top, "negcum", [128, NT, 8], F32)
        totS, totSB = alloc(top, "totS", [128, NT + 1, 8], F32)
        rmid, rmidB = alloc(top, "rmid", [128, NT, 8], F32)
        Gt, GtB = alloc(top, "Gt", [128, NT, E], F32)
        epsT, epsB = alloc(top, "epsT", [128, 1], F32)
        rankI, rankIB = alloc(top, "rankI", [128, NT, E], F32)
        cnt, cntB = alloc(top, "cnt", [128, E], F32)
        d01, d01B = alloc(top, "d01", [128, NT, 2], I32)
        widx, widxB = alloc(top, "widx", [128, NB], I32)
        pidx, pidxB = alloc(top, "pidx", [128, 1], F32)
        pidxi, pidxiB = alloc(top, "pidxi", [128, 1], I32)
        tabB = Buf()
        op("pool", lambda e: e.iota(out=pidxi[:], pattern=[[0, 1]], base=0, channel_multiplier=1), writes=[pidxiB])
        op("dve", lambda e: e.tensor_copy(out=pidx[:], in_=pidxi[:]), reads=[pidxiB], writes=[pidxB])

        op("pool", lambda e: e.memset(ident[:], 0.0), writes=[identB])
        op("pool", lambda e: e.affine_select(out=ident[:], in_=ident[:], pattern=[[-1, 128]],
                                             compare_op=ALU.not_equal, fill=1.0, base=0, channel_multiplier=1),
           reads=[identB], writes=[identB])
        op("pool", lambda e: e.memset(ones[:], 1.0), writes=[onesB])
        op("pool", lambda e: e.memset(ones16[:], 1.0), writes=[ones16B])
        op("pool", lambda e: e.memset(utri[:], 1.0), writes=[utriB])
        op("pool", lambda e: e.affine_select(out=utri[:], in_=utri[:], pattern=[[1, 128]],
                                             compare_op=ALU.is_ge, fill=0.0, base=0, channel_multiplier=-1),
           reads=[utriB], writes=[utriB])
        op("pool", lambda e: e.memset(epsT[:], EPS), writes=[epsB])

        def rstd_from(dst, dstB, src, srcB, scale, tmp, tmpB):
            op("dve", lambda e: e.tensor_scalar(out=tmp, in0=src, scalar1=scale, scalar2=EPS,
                                                op0=ALU.mult, op1=ALU.add), reads=[srcB], writes=[tmpB])
            op("act", lambda e: e.activation(out=tmp, in_=tmp, func=AF.Sqrt), reads=[tmpB], writes=[tmpB])
            op("dve", lambda e: e.reciprocal(out=dst, in_=tmp), reads=[tmpB], writes=[dstB])

        with contextlib.ExitStack() as ph:
            cTt, cTB = alloc(ph, "cTt", [128, KC], F32)
            sg, sgB = alloc(ph, "sg0", [128, KC], F32)
            condB, condBB = alloc(ph, "condB", [128, KC, 128], F32)
            war = Ring(ph, "wa", 2, [128, KC, 512], F32)
            bar = Ring(ph, "ba", 2, [128, 512], F32)
            modb, modbB = alloc(ph, "modb", [128, 6, D], F32)
            lng, lngB = alloc(ph, "lng", [128, D], F32)
            dma("sp", cTt[:], cT[:], writes=[cTB])
            op("act", lambda e: e.activation(out=sg[:], in_=cTt[:], func=AF.Sigmoid), reads=[cTB], writes=[sgB])
            op("dve", lambda e: e.tensor_tensor(out=sg[:], in0=sg[:], in1=cTt[:], op=ALU.mult),
               reads=[sgB, cTB], writes=[sgB])
            for k in range(KC):
                op("dve", lambda e, k=k: e.tensor_scalar(out=condB[:, k, :], in0=ones[:], scalar1=sg[:, k:k + 1],
                                                         scalar2=None, op0=ALU.mult),
                   reads=[onesB, sgB], writes=[condBB])
            for l in range(L):
                for j in range(24):
                    wa, waB = war.next()
                    ba, baB = bar.next()
                    dma("sp", wa[:], w_ada[l, j], writes=[waB])
                    dma("sp", ba[:], b_ada[l, :, j * 512:(j + 1) * 512], writes=[baB])
                    p, pB = nps()
                    for k in range(KC):
                        op("pe", lambda e, k=k, p=p, wa=wa: e.matmul(p[:], lhsT=condB[:, k, :], rhs=wa[:, k, :],
                                                                  start=(k == 0), stop=(k == KC - 1)),
                           reads=[condBB, waB], writes=[pB])
                    s6, c4 = j // 4, j % 4
                    op("dve", lambda e, p=p, ba=ba, s6=s6, c4=c4: e.tensor_tensor(
                        out=modb[:, s6, c4 * 512:(c4 + 1) * 512], in0=p[:], in1=ba[:], op=ALU.add),
                       reads=[pB, baB], writes=[modbB])
                for (slot_sc, gsrc) in ((1, ln1g), (4, ln2g)):
                    dma("sp", lng[:], gsrc[l], writes=[lngB])
                    op("dve", lambda e, s=slot_sc: e.scalar_tensor_tensor(
                        out=modb[:, s, :], in0=modb[:, s, :], scalar=1.0, in1=lng[:], op0=ALU.add, op1=ALU.mult),
                       reads=[modbB, lngB], writes=[modbB])
                for (dst, src) in ((0, 1), (1, 0), (2, 2), (3, 4), (4, 3), (5, 5)):
                    dma("sp", mods[l, :, dst, :], modb[:, src, :], reads=[modbB])
            sc.emit_block()
            if stop == 'p0':
                return nc

        for l in range(L):
            xsrc = x_in if l == 0 else xs

            with contextlib.ExitStack() as ph:
                A1, A1B = alloc(ph, "A1", [128, D], F32)
                sh1, sh1B = alloc(ph, "sh1", [128, D], F32)
                fbt, fbB = alloc(ph, "fbt", [128, 8], F32)
                wft, wfB = alloc(ph, "wft", [128, KC, 8], BF16)
                xr = Ring(ph, "xa", 2, [128, D], F32)
                tr = Ring(ph, "ta", 2, [128, D], F32)
                hr = Ring(ph, "ha", 2, [128, D], F32)
                sr = Ring(ph, "sa", 4, [128, 4], F32)
                hT, hTB = alloc(ph, "hT", [128, KC, TGA], BF16)
                wcr = Ring(ph, "wc", 4, [128, KC, 128], BF16)
                wvr = Ring(ph, "wv", 2, [128, KC, 512], BF16)
                zr = Ring(ph, "za", 4, [128, 512], F32)
                z16r = Ring(ph, "zb", 4, [128, 512], BF16)
                sgr = Ring(ph, "zs", 2, [128, 512], F32)
                f8r = Ring(ph, "f8", 2, [128, 8], F32)
                tot, totB = alloc(ph, "tot", [128, 8], F32)
                junk, junkB = alloc(ph, "junkA", [128, D], F32)
                dma("sp", A1[:], mods[l, :, 0, :], writes=[A1B])
                dma("sp", sh1[:], mods[l, :, 1, :], writes=[sh1B])
                dma("sp", fbt[:], fb[l], writes=[fbB])
                dma("pool", wft[:], w_f[l], writes=[wfB])
                op("pool", lambda e: e.memset(totS[:, 0, :], 0.0), writes=[totSB])
                for g in range(S // TGA):
                    ntl = TGA // 128
                    for tt in range(ntl):
                        t = g * ntl + tt
                        xt, xB = xr.next()
                        tmp, tmpB = tr.next()
                        h32, hB = hr.next()
                        st4, st4B = sr.next()
                        dma("sp", xt[:], xsrc[t * 128:(t + 1) * 128, :], writes=[xB])
                        op("act", lambda e, xt=xt, st4=st4: e.activation(out=junk[:], in_=xt[:], func=AF.Square,
                                                                         accum_out=st4[:, 0:1]),
                           reads=[xB], writes=[junkB, st4B])
                        rstd_from(st4[:, 1:2], st4B, st4[:, 0:1], st4B, 1.0 / D, st4[:, 2:3], st4B)
                        op("dve", lambda e, xt=xt, st4=st4, tmp=tmp: e.scalar_tensor_tensor(
                            out=tmp[:], in0=xt[:], scalar=st4[:, 1:2], in1=A1[:], op0=ALU.mult, op1=ALU.mult),
                           reads=[xB, st4B, A1B], writes=[tmpB])
                        op("pool", lambda e, tmp=tmp, h32=h32: e.tensor_tensor(out=h32[:], in0=tmp[:], in1=sh1[:],
                                                                             op=ALU.add),
                           reads=[tmpB, sh1B], writes=[hB])
                        for q4 in range(4):
                            p, pB = nps()
                            for i in range(4):
                                kc = q4 * 4 + i
                                op("pe", lambda e, p=p, i=i, kc=kc, h32=h32: e.transpose(
                                    p[:, i * 128:(i + 1) * 128], h32[:, kc * 128:(kc + 1) * 128], ident[:]),
                                   reads=[hB, identB], writes=[pB])
                            op("act", lambda e, p=p, q4=q4, tt=tt: e.activation(
                                out=hT[:, q4 * 4:(q4 + 1) * 4, tt * 128:(tt + 1) * 128],
                                in_=p[:].rearrange("p (a b) -> p a b", a=4), func=AF.Copy),
                               reads=[pB], writes=[hTB])
                    nsub = TGA // 512
                    for j in range(32):
                        wc, wcB = wcr.next()
                        dma("pool", wc[:], w_fm[l, j], writes=[wcB])
                        for sub in range(nsub):
                            t0 = g * TGA + sub * 512
                            p, pB = nps()
                            for k in range(KC):
                                op("pe", lambda e, p=p, k=k, wc=wc, sub=sub: e.matmul(
                                    p[:], lhsT=wc[:, k, :], rhs=hT[:, k, sub * 512:(sub + 1) * 512],
                                    start=(k == 0), stop=(k == KC - 1)), reads=[wcB, hTB], writes=[pB])
                            if j < 8:
                                if j % 2 == 0:
                                    za, zaB = zr.next()
                                    op("dve", lambda e, za=za, p=p: e.tensor_copy(out=za[:], in_=p[:]),
                                       reads=[pB], writes=[zaB])
                                    if sub == 0:
                                        held = []
                                    held.append((za, zaB))
                                else:
                                    za, zaB = held[sub]
                                    sgm, sgmB = sgr.next()
                                    op("act", lambda e, sgm=sgm, p=p: e.activation(out=sgm[:], in_=p[:], func=AF.Sigmoid),
                                       reads=[pB], writes=[sgmB])
                                    op("dve", lambda e, za=za, sgm=sgm: e.tensor_tensor(out=za[:], in0=za[:], in1=sgm[:],
                                                                                     op=ALU.mult),
                                       reads=[zaB, sgmB], writes=[zaB])
                                    cc = j // 2
                                    dma("sp", gluT[cc * 128:(cc + 1) * 128, t0:t0 + 512], za[:], reads=[zaB])
                            elif j < 24:
                                z16, z16B = z16r.next()
                                scale = 128.0 ** -0.5 if j < 16 else 1.0
                                op("act", lambda e, z16=z16, p=p, scale=scale: e.activation(
                                    out=z16[:], in_=p[:], func=AF.Copy, scale=scale), reads=[pB], writes=[z16B])
                                dst = qT if j < 16 else kT
                                hh = (j - 8) % 8
                                dma("sp", dst[hh * 128:(hh + 1) * 128, t0:t0 + 512], z16[:], reads=[z16B])
                            else:
                                za, zaB = zr.next()
                                op("dve", lambda e, za=za, p=p: e.tensor_copy(out=za[:], in_=p[:]),
                                   reads=[pB], writes=[zaB])
                                dst = lxT if j < 28 else lgT
                                cc = (j - 24) % 4
                                dma("sp", dst[cc * 128:(cc + 1) * 128, t0:t0 + 512], za[:], reads=[zaB])
                    for vg in range(2):
                        wv, wvB = wvr.next()
                        dma("pool", wv[:], w_v[l, vg], writes=[wvB])
                        for tt in range(ntl):
                            t = g * ntl + tt
                            p, pB = nps()
                            for k in range(KC):
                                op("pe", lambda e, p=p, k=k, wv=wv, tt=tt: e.matmul(
                                    p[:], lhsT=hT[:, k, tt * 128:(tt + 1) * 128], rhs=wv[:, k, :],
                                    start=(k == 0), stop=(k == KC - 1)), reads=[wvB, hTB], writes=[pB])
                            z16, z16B = z16r.next()
                            op("act", lambda e, z16=z16, p=p: e.activation(out=z16[:], in_=p[:], func=AF.Copy),
                               reads=[pB], writes=[z16B])
                            dma("sp", vS[t * 128:(t + 1) * 128, vg * 512:(vg + 1) * 512], z16[:], reads=[z16B])
                    for tt in range(ntl):
                        t = g * ntl + tt
                        p, pB = nps()
                        for k in range(KC):
                            op("pe", lambda e, p=p, k=k, tt=tt: e.matmul(
                                p[:, 0:8], lhsT=hT[:, k, tt * 128:(tt + 1) * 128], rhs=wft[:, k, :],
                                start=(k == 0), stop=(k == KC - 1)), reads=[wfB, hTB], writes=[pB])
                        f8, f8B = f8r.next()
                        op("dve", lambda e, f8=f8, p=p: e.tensor_tensor(out=f8[:], in0=p[:, 0:8], in1=fbt[:], op=ALU.add),
                           reads=[pB, fbB], writes=[f8B])
                        op("act", lambda e, f8=f8: e.activation(out=f8[:], in_=f8[:], func=AF.Exp, scale=-1.0),
                           reads=[f8B], writes=[f8B])
                        op("act", lambda e, f8=f8: e.activation(out=f8[:], in_=f8[:], func=AF.Ln, bias=1.0),
                           reads=[f8B], writes=[f8B])
                        p2, p2B = nps()
                        op("pe", lambda e, p2=p2, f8=f8: e.matmul(p2[:, 0:8], lhsT=utri[:], rhs=f8[:], start=True, stop=True),
                           reads=[utriB, f8B], writes=[p2B])
                        op("pe", lambda e, p2=p2, f8=f8: e.matmul(p2[:, 8:16], lhsT=ones[:], rhs=f8[:], start=True, stop=True),
                           reads=[onesB, f8B], writes=[p2B])
                        op("dve", lambda e, p2=p2, t=t: e.tensor_tensor(out=negcum[:, t, :], in0=p2[:, 0:8],
                                                                      in1=totS[:, t, :], op=ALU.add),
                           reads=[p2B, totSB], writes=[negcumB])
                        op("dve", lambda e, p2=p2, t=t: e.tensor_tensor(out=totS[:, t + 1, :], in0=p2[:, 8:16],
                                                                      in1=totS[:, t, :], op=ALU.add),
                           reads=[p2B, totSB], writes=[totSB])
                op("dve", lambda e: e.tensor_tensor(out=rmid[:], in0=totS[:, 0:NT, :], in1=totS[:, 1:NT + 1, :], op=ALU.add),
                   reads=[totSB], writes=[rmidB])
                op("dve", lambda e: e.tensor_scalar(out=rmid[:], in0=rmid[:], scalar1=0.5, scalar2=None, op0=ALU.mult),
                   reads=[rmidB], writes=[rmidB])
                sc.emit_block()
                if stop == 'A':
                    return nc

            with contextlib.ExitStack() as ph:
                cwt, cwB = alloc(ph, "cwt", [128, 4, 31], F32)
                cvt, cvB = alloc(ph, "cvt", [128, 4, 3], F32)
                gr = Ring(ph, "gl", 3, [128, 30 + 512], F32)
                ar = Ring(ph, "ac", 8, [128, 512], F32)
                sqr = Ring(ph, "sq", 2, [128, 512], F32)
                mr = Ring(ph, "mm", 2, [128, 512], F32)
                vr = Ring(ph, "vv", 2, [128, 512], F32)
                yr = Ring(ph, "yy", 3, [128, 512], BF16)
                for ex in range(E):
                    dma("pool", wg16[ex], wg[l, ex])
                    dma("pool", wu16[ex], wu[l, ex])
                    dma("pool", wd16[ex], wd[l, ex])
                for cc in range(4):
                    dma("sp", cwt[:, cc, :], cw[l, cc], writes=[cwB])
                    dma("sp", cvt[:, cc, :], cvec[l, cc], writes=[cvB])
                for tb in range(S // 512):
                    t0 = tb * 512
                    accs = []
                    (pm, pmB), (pq, pqB) = lps2()
                    for cc in range(4):
                        gl, glB = gr.next()
                        if tb == 0:
                            op("pool", lambda e, gl=gl: e.memset(gl[:, 0:30], 0.0), writes=[glB])
                            dma("sp", gl[:, 30:542], gluT[cc * 128:(cc + 1) * 128, 0:512], writes=[glB])
                        else:
                            dma("sp", gl[:], gluT[cc * 128:(cc + 1) * 128, t0 - 30:t0 + 512], writes=[glB])
                        acc, accB = ar.next()
                        op("dve", lambda e, acc=acc, gl=gl, cc=cc: e.tensor_scalar(
                            out=acc[:], in0=gl[:, 0:512], scalar1=cwt[:, cc, 0:1], scalar2=cvt[:, cc, 0:1],
                            op0=ALU.mult, op1=ALU.add), reads=[glB, cwB, cvB], writes=[accB])
                        for k in range(1, 31):
                            op("dve", lambda e, acc=acc, gl=gl, cc=cc, k=k: e.scalar_tensor_tensor(
                                out=acc[:], in0=gl[:, k:k + 512], scalar=cwt[:, cc, k:k + 1], in1=acc[:],
                                op0=ALU.mult, op1=ALU.add), reads=[glB, cwB, accB], writes=[accB])
                        sq, sqB = sqr.next()
                        op("act", lambda e, sq=sq, acc=acc: e.activation(out=sq[:], in_=acc[:], func=AF.Square),
                           reads=[accB], writes=[sqB])
                        op("pe", lambda e, acc=acc, cc=cc, pm=pm: e.matmul(pm[:], lhsT=ones[:], rhs=acc[:],
                                                                         start=(cc == 0), stop=(cc == 3)),
                           reads=[onesB, accB], writes=[pmB])
                        op("pe", lambda e, sq=sq, cc=cc, pq=pq: e.matmul(pq[:], lhsT=ones[:], rhs=sq[:],
                                                                       start=(cc == 0), stop=(cc == 3)),
                           reads=[onesB, sqB], writes=[pqB])
                        accs.append((acc, accB))
                    mean, meanB = mr.next()
                    var, varB = vr.next()
                    op("dve", lambda e, mean=mean, pm=pm: e.tensor_scalar(out=mean[:], in0=pm[:], scalar1=1.0 / 512,
                                                                        scalar2=None, op0=ALU.mult),
                       reads=[pmB], writes=[meanB])
                    op("dve", lambda e, var=var, mean=mean: e.tensor_tensor(out=var[:], in0=mean[:], in1=mean[:], op=ALU.mult),
                       reads=[meanB], writes=[varB])
                    op("dve", lambda e, var=var, pq=pq: e.scalar_tensor_tensor(
                        out=var[:], in0=pq[:], scalar=1.0 / 512, in1=var[:], op0=ALU.mult, op1=ALU.subtract),
                       reads=[pqB, varB], writes=[varB])
                    op("dve", lambda e, var=var: e.tensor_scalar(out=var[:], in0=var[:], scalar1=EPS, scalar2=None, op0=ALU.add),
                       reads=[varB], writes=[varB])
                    op("act", lambda e, var=var: e.activation(out=var[:], in_=var[:], func=AF.Sqrt), reads=[varB], writes=[varB])
                    op("dve", lambda e, var=var: e.reciprocal(out=var[:], in_=var[:]), reads=[varB], writes=[varB])
                    for cc in range(4):
                        acc, accB = accs[cc]
                        op("pool", lambda e, acc=acc, mean=mean: e.tensor_tensor(out=acc[:], in0=acc[:], in1=mean[:],
                                                                               op=ALU.subtract),
                           reads=[accB, meanB], writes=[accB])
                        op("pool", lambda e, acc=acc, var=var: e.tensor_tensor(out=acc[:], in0=acc[:], in1=var[:], op=ALU.mult),
                           reads=[accB, varB], writes=[accB])
                        y, yB = yr.next()
                        op("act", lambda e, y=y, acc=acc, cc=cc: e.activation(
                            out=y[:], in_=acc[:], func=AF.Silu, scale=cvt[:, cc, 1:2], bias=cvt[:, cc, 2:3]),
                           reads=[accB, cvB], writes=[yB])
                        dma("sp", ymT[cc * 128:(cc + 1) * 128, t0:t0 + 512], y[:], reads=[yB])
                sc.emit_block()
                if stop == 'B1':
                    return nc

            with contextlib.ExitStack() as ph:
                qh, qhB = alloc(ph, "qh", [128, S], BF16)
                kh, khB = alloc(ph, "kh", [128, S], BF16)
                vh, vhB = alloc(ph, "vh", [128, NT, 128], BF16)
                biasT, biasTB = alloc(ph, "biasT", [128, NT, NT], F32)
                pr = Ring(ph, "pt", 4, [128, 512], BF16)
                rr = Ring(ph, "rl", 2, [128, 512], F32)
                orr = Ring(ph, "oo", 2, [128, 512], F32)
                fgt, fgB = alloc(ph, "fgt", [128, 8], F32)
                dma("sp", fgt[:], fg[l], writes=[fgB])
                for h in range(8):
                    dma("sp", qh[:], qT[h * 128:(h + 1) * 128, :], writes=[qhB])
                    dma("sp", kh[:], kT[h * 128:(h + 1) * 128, :], writes=[khB])
                    for v0 in range(0, NT, 16):
                        v1 = min(NT, v0 + 16)
                        dma("sp", vh[:, v0:v1, :], vS.rearrange("(t p) c -> p t c", p=128)[:, v0:v1, h * 128:(h + 1) * 128],
                            writes=[vhB])
                    for t in range(NT):
                        op("dve", lambda e, t=t, h=h: e.tensor_scalar(
                            out=biasT[:, t, :], in0=negcum[:, :, h], scalar1=rmid[:, t, h:h + 1], scalar2=None,
                            op0=ALU.subtract), reads=[negcumB, rmidB], writes=[biasTB])
                    for I in range(S // 512):
                        (po, poB), (pl, plB) = lps2()
                        nJ = I * 4 + 4
                        scores = {}

                        def issue_scores(J, I=I):
                            pS, pSB = nps()
                            op("pe", lambda e, pS=pS, J=J, I=I: e.matmul(
                                pS[:], lhsT=kh[:, J * 128:(J + 1) * 128], rhs=qh[:, I * 512:(I + 1) * 512],
                                start=True, stop=True), reads=[khB, qhB], writes=[pSB])
                            scores[J] = (pS, pSB)
                        for J in range(min(2, nJ)):
                            issue_scores(J)
                        for J in range(nJ):
                            if J + 2 < nJ:
                                issue_scores(J + 2)
                            pS, pSB = scores.pop(J)
                            pt, ptB = pr.next()
                            for i4 in range(4):
                                t = I * 4 + i4
                                blk = pt[:, i4 * 128:(i4 + 1) * 128]
                                if J > t:
                                    op("pool", lambda e, blk=blk: e.memset(blk, 0.0), writes=[ptB])
                                    continue
                                bcol = biasT[:, t, J:J + 1]
                                op("act", lambda e, blk=blk, pS=pS, i4=i4, bcol=bcol: e.activation(
                                    out=blk, in_=pS[:, i4 * 128:(i4 + 1) * 128], func=AF.Exp, bias=bcol),
                                   reads=[pSB, biasTB], writes=[ptB])
                                if J == t:
                                    op("pool", lambda e, blk=blk: e.affine_select(
                                        out=blk, in_=blk, pattern=[[1, 128]], compare_op=ALU.is_ge, fill=0.0,
                                        base=0, channel_multiplier=-1), reads=[ptB], writes=[ptB])
                            op("pe", lambda e, po=po, pt=pt, J=J, nJ=nJ: e.matmul(
                                po[:], lhsT=vh[:, J, :], rhs=pt[:], start=(J == 0), stop=(J == nJ - 1)),
                               reads=[vhB, ptB], writes=[poB])
                            op("pe", lambda e, pl=pl, pt=pt, J=J, nJ=nJ: e.matmul(
                                pl[:], lhsT=ones16[:], rhs=pt[:], start=(J == 0), stop=(J == nJ - 1)),
                               reads=[ones16B, ptB], writes=[plB])
                        rl, rlB = rr.next()
                        op("dve", lambda e, rl=rl, pl=pl: e.reciprocal(out=rl[:], in_=pl[:]), reads=[plB], writes=[rlB])
                        oo, ooB = orr.next()
                        op("dve", lambda e, oo=oo, po=po, rl=rl: e.tensor_tensor(out=oo[:], in0=po[:], in1=rl[:], op=ALU.mult),
                           reads=[poB, rlB], writes=[ooB])
                        dma("sp", oT[h * 128:(h + 1) * 128, I * 512:(I + 1) * 512], oo[:], reads=[ooB])
                sc.emit_block()
                if stop == 'B2':
                    return nc

            with contextlib.ExitStack() as ph:
                fgt, fgB = alloc(ph, "fgt2", [128, 8], F32)
                o8r = Ring(ph, "o8", 2, [128, 8, 512], F32)
                sqr = Ring(ph, "sq2", 2, [128, 512], F32)
                rsr = Ring(ph, "rs2", 2, [128, 512], F32)
                yr = Ring(ph, "yy2", 3, [128, 512], BF16)
                dma("sp", fgt[:], fg[l], writes=[fgB])
                for tb in range(S // 512):
                    t0 = tb * 512
                    o8, o8B = o8r.next()
                    dma("sp", o8[:], oT.rearrange("(h p) s -> p h s", p=128)[:, :, t0:t0 + 512], writes=[o8B])
                    (pq, pqB), _unused = lps2()
                    for h in range(8):
                        sq, sqB = sqr.next()
                        op("act", lambda e, sq=sq, o8=o8, h=h: e.activation(out=sq[:], in_=o8[:, h, :], func=AF.Square),
                           reads=[o8B], writes=[sqB])
                        op("pe", lambda e, sq=sq, pq=pq, h=h: e.matmul(pq[:], lhsT=ones[:], rhs=sq[:],
                                                                     start=(h == 0), stop=(h == 7)),
                           reads=[onesB, sqB], writes=[pqB])
                    rs, rsB = rsr.next()
                    rstd_from(rs[:], rsB, pq[:], pqB, 1.0 / 1024, rs[:], rsB)
                    for h in range(8):
                        y, yB = yr.next()
                        op("dve", lambda e, y=y, o8=o8, h=h, rs=rs: e.scalar_tensor_tensor(
                            out=y[:], in0=o8[:, h, :], scalar=fgt[:, h:h + 1], in1=rs[:], op0=ALU.mult, op1=ALU.mult),
                           reads=[o8B, fgB, rsB], writes=[yB])
                        dma("sp", ymT[512 + h * 128:512 + (h + 1) * 128, t0:t0 + 512], y[:], reads=[yB])
                sc.emit_block()
                if stop == 'B2b':
                    return nc

            with contextlib.ExitStack() as ph:
                lwt, lwB = alloc(ph, "lwt", [128, 4, 4], F32)
                lvt, lvB = alloc(ph, "lvt", [128, 4, 5], F32)
                nsp, nspB = alloc(ph, "nsp", [128, 4, 2], F32)
                wrt, wrB = alloc(ph, "wrt", [128, 4, 128], F32)
                wit, wiB = alloc(ph, "wit", [128, 4, 128], F32)
                carry, carryB = alloc(ph, "carry", [128, 4], F32)
                xr_ = Ring(ph, "lx", 3, [128, 3 + 512], F32)
                gr_ = Ring(ph, "lg", 3, [128, 512], F32)
                xcr = Ring(ph, "xc", 3, [128, 512], F32)
                rr_ = Ring(ph, "rr", 2, [128, 512], F32)
                ir_ = Ring(ph, "ii", 2, [128, 512], F32)
                ar_ = Ring(ph, "aa", 2, [128, 512], F32)
                br_ = Ring(ph, "bb", 2, [128, 512], F32)
                hr_ = Ring(ph, "hh", 2, [128, 512], F32)
                mr_ = Ring(ph, "mm3", 8, [128, 512], F32)
                sqr = Ring(ph, "sq3", 2, [128, 512], F32)
                rsr = Ring(ph, "rs3", 2, [128, 512], F32)
                yr = Ring(ph, "yy3", 3, [128, 512], BF16)
                for cc in range(4):
                    dma("sp", lwt[:, cc, :], lw[l, cc], writes=[lwB])
                    dma("sp", lvt[:, cc, :], lvec[l, cc], writes=[lvB])
                    dma("sp", wrt[:, cc, :], lwr[l, cc], writes=[wrB])
                    dma("sp", wit[:, cc, :], lwi[l, cc], writes=[wiB])
                op("act", lambda e: e.activation(out=nsp[:, :, 0], in_=lvt[:, :, 3], func=AF.Exp, scale=-1.0),
                   reads=[lvB], writes=[nspB])
                op("act", lambda e: e.activation(out=nsp[:, :, 0], in_=nsp[:, :, 0], func=AF.Ln, bias=1.0),
                   reads=[nspB], writes=[nspB])
                op("dve", lambda e: e.tensor_scalar(out=nsp[:, :, 1], in0=nsp[:, :, 0], scalar1=-16.0, scalar2=None, op0=ALU.mult),
                   reads=[nspB], writes=[nspB])
                op("dve", lambda e: e.tensor_scalar(out=nsp[:, :, 0], in0=nsp[:, :, 0], scalar1=-8.0, scalar2=None, op0=ALU.mult),
                   reads=[nspB], writes=[nspB])
                op("pool", lambda e: e.memset(carry[:], 0.0), writes=[carryB])
                for tb in range(S // 512):
                    t0 = tb * 512
                    (pq, pqB), _unused = lps2()
                    ms = []
                    for cc in range(4):
                        lx, lxB = xr_.next()
                        if tb == 0:
                            op("pool", lambda e, lx=lx: e.memset(lx[:, 0:3], 0.0), writes=[lxB])
                            dma("sp", lx[:, 3:515], lxT[cc * 128:(cc + 1) * 128, 0:512], writes=[lxB])
                        else:
                            dma("sp", lx[:], lxT[cc * 128:(cc + 1) * 128, t0 - 3:t0 + 512], writes=[lxB])
                        lg_, lgB = gr_.next()
                        dma("sp", lg_[:], lgT[cc * 128:(cc + 1) * 128, t0:t0 + 512], writes=[lgB])
                        xc, xcB = xcr.next()
                        op("dve", lambda e, xc=xc, lx=lx, cc=cc: e.tensor_scalar(
                            out=xc[:], in0=lx[:, 0:512], scalar1=lwt[:, cc, 0:1], scalar2=lvt[:, cc, 0:1],
                            op0=ALU.mult, op1=ALU.add), reads=[lxB, lwB, lvB], writes=[xcB])
                        for k in range(1, 4):
                            op("dve", lambda e, xc=xc, lx=lx, cc=cc, k=k: e.scalar_tensor_tensor(
                                out=xc[:], in0=lx[:, k:k + 512], scalar=lwt[:, cc, k:k + 1], in1=xc[:],
                                op0=ALU.mult, op1=ALU.add), reads=[lxB, lwB, xcB], writes=[xcB])
                        pr_, prB = nps()
                        pi_, piB = nps()
                        op("pe", lambda e, pr_=pr_, xc=xc, cc=cc: e.matmul(pr_[:], lhsT=wrt[:, cc, :], rhs=xc[:], start=True, stop=True),
                           reads=[wrB, xcB], writes=[prB])
                        op("pe", lambda e, pi_=pi_, xc=xc, cc=cc: e.matmul(pi_[:], lhsT=wit[:, cc, :], rhs=xc[:], start=True, stop=True),
                           reads=[wiB, xcB], writes=[piB])
                        r_, rB = rr_.next()
                        i_, iB = ir_.next()
                        op("act", lambda e, r_=r_, pr_=pr_, cc=cc: e.activation(out=r_[:], in_=pr_[:], func=AF.Sigmoid,
                                                                             bias=lvt[:, cc, 1:2]),
                           reads=[prB, lvB], writes=[rB])
                        op("act", lambda e, i_=i_, pi_=pi_, cc=cc: e.activation(out=i_[:], in_=pi_[:], func=AF.Sigmoid,
                                                                             bias=lvt[:, cc, 2:3]),
                           reads=[piB, lvB], writes=[iB])
                        a_, aB = ar_.next()
                        b_, bB = br_.next()
                        op("act", lambda e, a_=a_, r_=r_, cc=cc: e.activation(out=a_[:], in_=r_[:], func=AF.Exp,
                                                                           scale=nsp[:, cc, 0:1]),
                           reads=[rB, nspB], writes=[aB])
                        op("act", lambda e, b_=b_, r_=r_, cc=cc: e.activation(out=b_[:], in_=r_[:], func=AF.Exp,
                                                                           scale=nsp[:, cc, 1:2]),
                           reads=[rB, nspB], writes=[bB])
                        op("dve", lambda e, b_=b_: e.tensor_scalar(out=b_[:], in0=b_[:], scalar1=-1.0, scalar2=1.0,
                                                                  op0=ALU.mult, op1=ALU.add), reads=[bB], writes=[bB])
                        op("dve", lambda e, b_=b_: e.tensor_scalar(out=b_[:], in0=b_[:], scalar1=0.0, scalar2=None,
                                                                  op0=ALU.max), reads=[bB], writes=[bB])
                        op("act", lambda e, b_=b_: e.activation(out=b_[:], in_=b_[:], func=AF.Sqrt), reads=[bB], writes=[bB])
                        op("pool", lambda e, i_=i_, xc=xc: e.tensor_tensor(out=i_[:], in0=i_[:], in1=xc[:], op=ALU.mult),
                           reads=[iB, xcB], writes=[iB])
                        op("pool", lambda e, b_=b_, i_=i_: e.tensor_tensor(out=b_[:], in0=b_[:], in1=i_[:], op=ALU.mult),
                           reads=[bB, iB], writes=[bB])
                        hh, hhB = hr_.next()
                        op("dve", lambda e, hh=hh, a_=a_, b_=b_, cc=cc: e.tensor_tensor_scan(
                            out=hh[:], data0=a_[:], data1=b_[:], initial=carry[:, cc:cc + 1], op0=ALU.mult, op1=ALU.add),
                           reads=[aB, bB, carryB], writes=[hhB])
                        op("dve", lambda e, hh=hh, cc=cc: e.tensor_copy(out=carry[:, cc:cc + 1], in_=hh[:, 511:512]),
                           reads=[hhB], writes=[carryB])
                        m_, mB = mr_.next()
                        op("act", lambda e, m_=m_, lg_=lg_: e.activation(out=m_[:], in_=lg_[:], func=AF.Square),
                           reads=[lgB], writes=[mB])
                        op("dve", lambda e, m_=m_: e.tensor_scalar(out=m_[:], in0=m_[:], scalar1=0.044715, scalar2=1.0,
                                                                  op0=ALU.mult, op1=ALU.add), reads=[mB], writes=[mB])
                        op("pool", lambda e, m_=m_, lg_=lg_: e.tensor_tensor(out=m_[:], in0=m_[:], in1=lg_[:], op=ALU.mult),
                           reads=[mB, lgB], writes=[mB])
                        op("act", lambda e, m_=m_: e.activation(out=m_[:], in_=m_[:], func=AF.Sigmoid, scale=1.5957691216),
                           reads=[mB], writes=[mB])
                        op("pool", lambda e, m_=m_, lg_=lg_: e.tensor_tensor(out=m_[:], in0=m_[:], in1=lg_[:], op=ALU.mult),
                           reads=[mB, lgB], writes=[mB])
                        op("pool", lambda e, m_=m_, hh=hh: e.tensor_tensor(out=m_[:], in0=m_[:], in1=hh[:], op=ALU.mult),
                           reads=[mB, hhB], writes=[mB])
                        sq, sqB = sqr.next()
                        op("act", lambda e, sq=sq, m_=m_: e.activation(out=sq[:], in_=m_[:], func=AF.Square),
                           reads=[mB], writes=[sqB])
                        op("pe", lambda e, sq=sq, pq=pq, cc=cc: e.matmul(pq[:], lhsT=ones[:], rhs=sq[:],
                                                                       start=(cc == 0), stop=(cc == 3)),
                           reads=[onesB, sqB], writes=[pqB])
                        ms.append((m_, mB))
                    rs, rsB = rsr.next()
                    rstd_from(rs[:], rsB, pq[:], pqB, 1.0 / 512, rs[:], rsB)
                    for cc in range(4):
                        m_, mB = ms[cc]
                        y, yB = yr.next()
                        op("dve", lambda e, y=y, m_=m_, cc=cc, rs=rs: e.scalar_tensor_tensor(
                            out=y[:], in0=m_[:], scalar=lvt[:, cc, 4:5], in1=rs[:], op0=ALU.mult, op1=ALU.mult),
                           reads=[mB, lvB, rsB], writes=[yB])
                        dma("sp", ymT[1536 + cc * 128:1536 + (cc + 1) * 128, t0:t0 + 512], y[:], reads=[yB])
                sc.emit_block()
                if stop == 'B3':
                    return nc

            with contextlib.ExitStack() as ph:
                wo, woB = alloc(ph, "wo", [128, KC, D], BF16)
                g1, g1B = alloc(ph, "g1", [128, D], F32)
                A2, A2B = alloc(ph, "A2", [128, D], F32)
                sh2, sh2B = alloc(ph, "sh2", [128, D], F32)
                wrt32, wrtB = alloc(ph, "wrt32", [128, KC, 36], F32)
                brt, brtB = alloc(ph, "brt", [128, 36], F32)
                ymr = Ring(ph, "ym", 1, [128, KC, 512], BF16)
                xr = Ring(ph, "xc_", 2, [128, D], F32)
                tr = Ring(ph, "tc_", 2, [128, D], F32)
                hr = Ring(ph, "hc_", 2, [128, D], F32)
                sr = Ring(ph, "sc_", 4, [128, 4], F32)
                h2r = Ring(ph, "h2", 2, [128, KC, 128], BF16)
                h32r = Ring(ph, "h32", 1, [128, KC, 128], F32)
                rtr = Ring(ph, "rt", 2, [128, 64], F32)
                arr = Ring(ph, "ar", 2, [128, E], F32)
                op("pool", lambda e: e.memset(cnt[:], 0.0), writes=[cntB])
                for k4 in range(4):
                    dma("pool", wo[:, k4 * 4:(k4 + 1) * 4, :], w_out[l, :, k4 * 4:(k4 + 1) * 4, :], writes=[woB])
                dma("sp", g1[:], mods[l, :, 2, :], writes=[g1B])
                dma("sp", A2[:], mods[l, :, 3, :], writes=[A2B])
                dma("sp", sh2[:], mods[l, :, 4, :], writes=[sh2B])
                dma("sp", wrt32[:], w_rt[l], writes=[wrtB])
                dma("sp", brt[:], b_rt[l], writes=[brtB])
                for tg in range(S // 512):
                    ym, ymB = ymr.next()
                    dma("sp", ym[:], ymT.rearrange("(k p) s -> p k s", p=128)[:, :, tg * 512:(tg + 1) * 512], writes=[ymB])
                    for tt in range(4):
                        t = tg * 4 + tt
                        xt, xB = xr.next()
                        tmp, tmpB = tr.next()
                        h32, hB = hr.next()
                        st4, st4B = sr.next()
                        dma("sp", xt[:], xsrc[t * 128:(t + 1) * 128, :], writes=[xB])
                        if C_LEVEL < 1:
                            continue
                        for cg in range(4):
                            p, pB = nps()
                            for k in range(KC):
                                op("pe", lambda e, p=p, k=k, ym=ym, tt=tt, cg=cg: e.matmul(
                                    p[:], lhsT=ym[:, k, tt * 128:(tt + 1) * 128], rhs=wo[:, k, cg * 512:(cg + 1) * 512],
                                    start=(k == 0), stop=(k == KC - 1)), reads=[ymB, woB], writes=[pB])
                            op("dve", lambda e, p=p, tmp=tmp, cg=cg: e.tensor_tensor(
                                out=tmp[:, cg * 512:(cg + 1) * 512], in0=p[:], in1=g1[:, cg * 512:(cg + 1) * 512], op=ALU.mult),
                               reads=[pB, g1B], writes=[tmpB])
                        if "pool" not in C_SKIP:
                            op("pool", lambda e, xt=xt, tmp=tmp: e.tensor_tensor(out=xt[:], in0=xt[:], in1=tmp[:], op=ALU.add),
                               reads=[xB, tmpB], writes=[xB])
                        if "xs" not in C_SKIP:
                            dma("sp", (out if "toout" in C_SKIP else xs)[t * 128:(t + 1) * 128, :], xt[:], reads=[xB])
                        if C_LEVEL < 2:
                            continue
                        op("act", lambda e, xt=xt, st4=st4, h32=h32: e.activation(out=h32[:], in_=xt[:], func=AF.Square,
                                                                         accum_out=st4[:, 0:1]),
                           reads=[xB], writes=[hB, st4B])
                        rstd_from(st4[:, 1:2], st4B, st4[:, 0:1], st4B, 1.0 / D, st4[:, 2:3], st4B)
                        if C_LEVEL < 1.3:
                            continue
                        op("dve", lambda e, xt=xt, st4=st4, tmp=tmp: e.scalar_tensor_tensor(
                            out=tmp[:], in0=xt[:], scalar=st4[:, 1:2], in1=A2[:], op0=ALU.mult, op1=ALU.mult),
                           reads=[xB, st4B, A2B], writes=[tmpB])
                        op("pool", lambda e, tmp=tmp, h32=h32: e.tensor_tensor(out=h32[:], in0=tmp[:], in1=sh2[:], op=ALU.add),
                           reads=[tmpB, sh2B], writes=[hB])
                        if C_LEVEL < 1.6:
                            continue
                        dma("sp", h2S[t * 128:(t + 1) * 128, :], h32[:], reads=[hB])
                        h2, h2B = h2r.next()
                        hT32, hT32B = h32r.next()
                        for q4 in range(4):
                            p, pB = nps()
                            for i in range(4):
                                kc = q4 * 4 + i
                                op("pe", lambda e, p=p, i=i, kc=kc, h32=h32: e.transpose(
                                    p[:, i * 128:(i + 1) * 128], h32[:, kc * 128:(kc + 1) * 128], ident[:]),
                                   reads=[hB, identB], writes=[pB])
                            op("act", lambda e, p=p, q4=q4, hT32=hT32: e.activation(
                                out=hT32[:, q4 * 4:(q4 + 1) * 4, :].rearrange("p a b -> p (a b)"), in_=p[:], func=AF.Copy),
                               reads=[pB], writes=[hT32B])
                        op("pool", lambda e, h2=h2, hT32=hT32: e.tensor_copy(out=h2[:], in_=hT32[:]),
                           reads=[hT32B], writes=[h2B])
                        if C_LEVEL < 3:
                            continue
                        dma("sp", h2T.rearrange("(k p) s -> p k s", p=128)[:, :, t * 128:(t + 1) * 128], h2[:], reads=[h2B])
                        if SKIP_ROUTER:
                            continue
                        p, pB = nps()
                        for k in range(KC):
                            op("pe", lambda e, p=p, k=k, hT32=hT32: e.matmul(
                                p[:, 0:36], lhsT=hT32[:, k, :], rhs=wrt32[:, k, :], start=(k == 0), stop=(k == KC - 1)),
                               reads=[hT32B, wrtB], writes=[pB])
                        rt, rtB = rtr.next()
                        op("dve", lambda e, rt=rt, p=p: e.tensor_tensor(out=rt[:, 0:36], in0=p[:, 0:36], in1=brt[:], op=ALU.add),
                           reads=[pB, brtB], writes=[rtB])
                        op("dve", lambda e, rt=rt: e.tensor_reduce(out=rt[:, 56:57], in_=rt[:, 0:4], axis=AX.X, op=ALU.max),
                           reads=[rtB], writes=[rtB])
                        op("dve", lambda e, rt=rt: e.tensor_scalar(out=rt[:, 36:40], in0=rt[:, 0:4], scalar1=rt[:, 56:57],
                                                                  scalar2=None, op0=ALU.is_equal), reads=[rtB], writes=[rtB])
                        op("dve", lambda e, rt=rt: e.tensor_scalar(out=rt[:, 57:58], in0=rt[:, 56:57], scalar1=-1.0, scalar2=None,
                                                                  op0=ALU.mult), reads=[rtB], writes=[rtB])
                        op("act", lambda e, rt=rt: e.activation(out=rt[:, 60:64], in_=rt[:, 0:4], func=AF.Exp, bias=rt[:, 57:58],
                                                               accum_out=rt[:, 58:59]), reads=[rtB], writes=[rtB])
                        op("dve", lambda e, rt=rt: e.reciprocal(out=rt[:, 58:59], in_=rt[:, 58:59]), reads=[rtB], writes=[rtB])
                        op("dve", lambda e, rt=rt: e.tensor_scalar(out=rt[:, 40:48], in0=rt[:, 4:12], scalar1=rt[:, 36:37],
                                                                  scalar2=None, op0=ALU.mult), reads=[rtB], writes=[rtB])
                        for gi in range(1, 4):
                            op("dve", lambda e, rt=rt, gi=gi: e.scalar_tensor_tensor(
                                out=rt[:, 40:48], in0=rt[:, 4 + gi * 8:12 + gi * 8], scalar=rt[:, 36 + gi:37 + gi],
                                in1=rt[:, 40:48], op0=ALU.mult, op1=ALU.add), reads=[rtB], writes=[rtB])
                        op("dve", lambda e, rt=rt: e.max(out=rt[:, 48:56], in_=rt[:, 40:48]), reads=[rtB], writes=[rtB])
                        op("dve", lambda e, rt=rt: e.tensor_tensor(out=rt[:, 59:60], in0=rt[:, 49:50], in1=rt[:, 48:49], op=ALU.subtract),
                           reads=[rtB], writes=[rtB])
                        op("act", lambda e, rt=rt: e.activation(out=rt[:, 59:60], in_=rt[:, 59:60], func=AF.Exp),
                           reads=[rtB], writes=[rtB])
                        op("dve", lambda e, rt=rt: e.tensor_scalar(out=rt[:, 60:61], in0=rt[:, 59:60], scalar1=1.0, scalar2=None,
                                                                  op0=ALU.add), reads=[rtB], writes=[rtB])
                        op("dve", lambda e, rt=rt: e.reciprocal(out=rt[:, 60:61], in_=rt[:, 60:61]), reads=[rtB], writes=[rtB])
                        op("dve", lambda e, rt=rt: e.tensor_tensor(out=rt[:, 60:61], in0=rt[:, 60:61], in1=rt[:, 58:59], op=ALU.mult),
                           reads=[rtB], writes=[rtB])
                        op("dve", lambda e, rt=rt: e.tensor_tensor(out=rt[:, 61:62], in0=rt[:, 60:61], in1=rt[:, 59:60], op=ALU.mult),
                           reads=[rtB], writes=[rtB])
                        op("dve", lambda e, rt=rt: e.tensor_scalar(out=rt[:, 62:63], in0=rt[:, 48:49], scalar1=1.0, scalar2=None,
                                                                  op0=ALU.mult), reads=[rtB], writes=[rtB])
                        op("dve", lambda e, rt=rt: e.tensor_scalar(out=rt[:, 63:64], in0=rt[:, 49:50], scalar1=1.0, scalar2=None,
                                                                  op0=ALU.mult), reads=[rtB], writes=[rtB])
                        op("dve", lambda e, rt=rt: e.tensor_scalar(out=rt[:, 48:56], in0=rt[:, 40:48], scalar1=rt[:, 62:63],
                                                                  scalar2=rt[:, 60:61], op0=ALU.is_equal, op1=ALU.mult),
                           reads=[rtB], writes=[rtB])
                        op("dve", lambda e, rt=rt: e.tensor_scalar(out=rt[:, 40:48], in0=rt[:, 40:48], scalar1=rt[:, 63:64],
                                                                  scalar2=rt[:, 61:62], op0=ALU.is_equal, op1=ALU.mult),
                           reads=[rtB], writes=[rtB])
                        op("dve", lambda e, rt=rt: e.tensor_tensor(out=rt[:, 48:56], in0=rt[:, 48:56], in1=rt[:, 40:48], op=ALU.add),
                           reads=[rtB], writes=[rtB])
                        for gi in range(4):
                            op("dve", lambda e, rt=rt, gi=gi, t=t: e.tensor_scalar(
                                out=Gt[:, t, gi * 8:(gi + 1) * 8], in0=rt[:, 48:56], scalar1=rt[:, 36 + gi:37 + gi],
                                scalar2=None, op0=ALU.mult), reads=[rtB], writes=[GtB])
                        ar, arB = arr.next()
                        op("dve", lambda e, ar=ar, t=t: e.tensor_scalar(out=ar[:], in0=Gt[:, t, :], scalar1=0.0, scalar2=None,
                                                                        op0=ALU.is_gt), reads=[GtB], writes=[arB])
                        p2, p2B = nps()
                        op("pe", lambda e, p2=p2, ar=ar: e.matmul(p2[:, 0:E], lhsT=utri[:], rhs=ar[:], start=True, stop=True),
                           reads=[utriB, arB], writes=[p2B])
                        op("pe", lambda e, p2=p2, ar=ar: e.matmul(p2[:, E:2 * E], lhsT=ones[:], rhs=ar[:], start=True, stop=True),
                           reads=[onesB, arB], writes=[p2B])
                        op("dve", lambda e, p2=p2, t=t: e.tensor_tensor(out=rankI[:, t, :], in0=p2[:, 0:E], in1=cnt[:], op=ALU.add),
                           reads=[p2B, cntB], writes=[rankIB])
                        op("dve", lambda e, p2=p2: e.tensor_tensor(out=cnt[:], in0=p2[:, E:2 * E], in1=cnt[:], op=ALU.add),
                           reads=[p2B, cntB], writes=[cntB])
                sc.emit_block()
                if stop == 'C':
                    return nc

            with contextlib.ExitStack() as ph:
                thri, thriB = alloc(ph, "thri", [128, 64], I32)
                thr, thrB = alloc(ph, "thr", [128, 64], F32)
                cmpt, cmpB = alloc(ph, "cmpt", [128, E, 64], F32)
                nblk, nblkB = alloc(ph, "nblk", [128, E], F32)
                pend, pendB = alloc(ph, "pend", [128, E], F32)
                pst, pstB = alloc(ph, "pst", [128, E], F32)
                bthi, bthiB = alloc(ph, "bthi", [128, NB], I32)
                bth, bthB = alloc(ph, "bth", [128, NB], F32)
                be, beB = alloc(ph, "be", [128, NB], F32)
                zt, ztB = alloc(ph, "zt", [128, NB * 2], F32)
                vr_ = Ring(ph, "vv4", 2, [128, E], F32)
                ar_ = Ring(ph, "aa4", 2, [128, E], F32)
                mr_ = Ring(ph, "mm4", 2, [128, E], F32)
                t8r = Ring(ph, "t84", 2, [128, 8], F32)
                scr_ = Ring(ph, "sc4", 4, [128, 4], F32)
                dfr = Ring(ph, "df4", 2, [128, 2], F32)
                op("pool", lambda e: e.memset(zt[:], 0.0), writes=[ztB])
                dma("sp", tab.rearrange("(p r) c -> p (r c)", p=128), zt[:], reads=[ztB], writes=[tabB])
                op("pool", lambda e: e.iota(out=thri[:], pattern=[[128, 64]], base=0, channel_multiplier=0), writes=[thriB])
                op("dve", lambda e: e.tensor_copy(out=thr[:], in_=thri[:]), reads=[thriB], writes=[thrB])
                op("pool", lambda e: e.iota(out=bthi[:], pattern=[[128, NB]], base=0, channel_multiplier=0), writes=[bthiB])
                op("dve", lambda e: e.tensor_copy(out=bth[:], in_=bthi[:]), reads=[bthiB], writes=[bthB])
                for ex in range(E):
                    op("dve", lambda e, ex=ex: e.tensor_scalar(out=cmpt[:, ex, :], in0=thr[:], scalar1=cnt[:, ex:ex + 1],
                                                                scalar2=None, op0=ALU.is_lt), reads=[thrB, cntB], writes=[cmpB])
                op("dve", lambda e: e.tensor_reduce(out=nblk[:], in_=cmpt[:], axis=AX.X, op=ALU.add), reads=[cmpB], writes=[nblkB])
                op("dve", lambda e: e.tensor_tensor_scan(out=pend[:], data0=ones[:, 0:E], data1=nblk[:], initial=0.0,
                                                         op0=ALU.mult, op1=ALU.add), reads=[onesB, nblkB], writes=[pendB])
                op("dve", lambda e: e.tensor_scalar(out=pend[:], in0=pend[:], scalar1=128.0, scalar2=None, op0=ALU.mult),
                   reads=[pendB], writes=[pendB])
                op("dve", lambda e: e.scalar_tensor_tensor(out=pst[:], in0=nblk[:], scalar=-128.0, in1=pend[:],
                                                           op0=ALU.mult, op1=ALU.add), reads=[nblkB, pendB], writes=[pstB])
                op("pool", lambda e: e.memset(be[:], 0.0), writes=[beB])
                for ex in range(E):
                    op("dve", lambda e, ex=ex: e.scalar_tensor_tensor(out=be[:], in0=bth[:], scalar=pend[:, ex:ex + 1], in1=be[:],
                                                                      op0=ALU.is_ge, op1=ALU.add), reads=[bthB, pendB, beB], writes=[beB])
                op("dve", lambda e: e.tensor_scalar(out=be[:], in0=be[:], scalar1=float(E - 1), scalar2=None, op0=ALU.min),
                   reads=[beB], writes=[beB])
                op("dve", lambda e: e.tensor_scalar(out=be[:], in0=be[:], scalar1=128.0, scalar2=pidx[:, 0:1], op0=ALU.mult, op1=ALU.add),
                   reads=[beB, pidxB], writes=[beB])
                op("dve", lambda e: e.tensor_copy(out=widx[:], in_=be[:]), reads=[beB], writes=[widxB])
                for t in range(NT):
                    a_, aB = ar_.next()
                    v_, vB = vr_.next()
                    op("dve", lambda e, a_=a_, t=t: e.tensor_scalar(out=a_[:], in0=Gt[:, t, :], scalar1=0.0, scalar2=None, op0=ALU.is_gt),
                       reads=[GtB], writes=[aB])
                    op("dve", lambda e, v_=v_, t=t: e.tensor_tensor(out=v_[:], in0=rankI[:, t, :], in1=pst[:], op=ALU.add),
                       reads=[rankIB, pstB], writes=[vB])
                    op("dve", lambda e, v_=v_, a_=a_: e.tensor_tensor(out=v_[:], in0=v_[:], in1=a_[:], op=ALU.mult),
                       reads=[vB, aB], writes=[vB])
                    t8, t8B = t8r.next()
                    op("dve", lambda e, t8=t8, v_=v_: e.max(out=t8[:], in_=v_[:]), reads=[vB], writes=[t8B])
                    df, dfB = dfr.next()
                    op("dve", lambda e, df=df, t8=t8: e.tensor_scalar(out=df[:], in0=t8[:, 0:2], scalar1=-1.0, scalar2=None, op0=ALU.add),
                       reads=[t8B], writes=[dfB])
                    op("dve", lambda e, df=df, t=t: e.tensor_copy(out=d01[:, t, :], in_=df[:]), reads=[dfB], writes=[d01B])
                    for sl_ in range(2):
                        m_, mB = mr_.next()
                        s4, s4B = scr_.next()
                        op("dve", lambda e, m_=m_, v_=v_, t8=t8, sl_=sl_: e.tensor_scalar(
                            out=m_[:], in0=v_[:], scalar1=t8[:, sl_:sl_ + 1], scalar2=None, op0=ALU.is_equal),
                           reads=[vB, t8B], writes=[mB])
                        op("dve", lambda e, m_=m_, t=t: e.tensor_tensor(out=m_[:], in0=m_[:], in1=Gt[:, t, :], op=ALU.mult),
                           reads=[mB, GtB], writes=[mB])
                        op("dve", lambda e, m_=m_, s4=s4: e.tensor_reduce(out=s4[:, 1:2], in_=m_[:], axis=AX.X, op=ALU.add),
                           reads=[mB], writes=[s4B])
                        op("dve", lambda e, s4=s4, t=t: e.tensor_scalar(out=s4[:, 0:1], in0=pidx[:], scalar1=float(t * 128), scalar2=None,
                                                                        op0=ALU.add), reads=[pidxB], writes=[s4B])
                        sc.idma(tab, s4[:, 0:2], d01[:, t, sl_:sl_ + 1], scatter=True, reads=[s4B, d01B], writes=[tabB])
                sc.emit_block()
                if stop == 'D1':
                    return nc

            with contextlib.ExitStack() as ph:
                ps8i = [0]

                def nps8():
                    it = ps[ps8i[0] % 8]
                    ps8i[0] += 1
                    return it
                tbr = Ring(ph, "tb5", 3, [128, 2], F32)
                tir = Ring(ph, "ti5", 3, [128, 1], I32)
                xbr = Ring(ph, "xb5", 2, [128, D], F32)
                xtr = Ring(ph, "xt5", 2, [128, KC, 128], BF16)
                wgr = Ring(ph, "wg5", 2, [128, KC * DFF], BF16)
                wur = Ring(ph, "wu5", 2, [128, KC * DFF], BF16)
                wdr = Ring(ph, "wd5", 2, [128, NFC * D], BF16)
                her = Ring(ph, "he5", 2 * NFC, [128, 128], BF16)
                slr = Ring(ph, "sl5", 2, [128, 128], F32)
                ybr = Ring(ph, "yb5", 2, [128, D], F32)
                wg_rows = wg16.rearrange("e p k f -> (e p) (k f)")
                wu_rows = wu16.rearrange("e p k f -> (e p) (k f)")
                wd_rows = wd16.rearrange("e p c d -> (e p) (c d)")
                for b in range(NB):
                    tb, tbB = tbr.next()
                    ti, tiB = tir.next()
                    dma("sp", tb[:], tab[b * 128:(b + 1) * 128, :], reads=[tabB], writes=[tbB])
                    op("dve", lambda e, ti=ti, tb=tb: e.tensor_copy(out=ti[:], in_=tb[:, 0:1]), reads=[tbB], writes=[tiB])
                    xb, xbB = xbr.next()
                    sc.idma(xb[:], h2S, ti[:, 0:1], scatter=False, reads=[tiB], writes=[xbB])
                    wgt, wgB = wgr.next()
                    wut, wuB = wur.next()
                    wdt, wdB = wdr.next()
                    sc.idma(wgt[:], wg_rows, widx[:, b:b + 1], scatter=False, reads=[widxB], writes=[wgB])
                    sc.idma(wut[:], wu_rows, widx[:, b:b + 1], scatter=False, reads=[widxB], writes=[wuB])
                    sc.idma(wdt[:], wd_rows, widx[:, b:b + 1], scatter=False, reads=[widxB], writes=[wdB])
                    xT, xTB = xtr.next()
                    for q4 in range(4):
                        p, pB = nps8()
                        for i in range(4):
                            kc = q4 * 4 + i
                            op("pe", lambda e, p=p, i=i, kc=kc, xb=xb: e.transpose(
                                p[:, i * 128:(i + 1) * 128], xb[:, kc * 128:(kc + 1) * 128], ident[:]),
                               reads=[xbB, identB], writes=[pB])
                        op("act", lambda e, p=p, q4=q4, xT=xT: e.activation(
                            out=xT[:, q4 * 4:(q4 + 1) * 4, :].rearrange("p a b -> p (a b)"), in_=p[:], func=AF.Copy),
                           reads=[pB], writes=[xTB])
                    hes = []
                    for fc in range(NFC):
                        pg, pgB = nps8()
                        pu, puB = nps8()
                        for k in range(KC):
                            c0 = k * DFF + fc * 128
                            op("pe", lambda e, pg=pg, k=k, wgt=wgt, c0=c0, xT=xT: e.matmul(
                                pg[:, 0:128], lhsT=wgt[:, c0:c0 + 128], rhs=xT[:, k, :], start=(k == 0), stop=(k == KC - 1)),
                               reads=[wgB, xTB], writes=[pgB])
                        for k in range(KC):
                            c0 = k * DFF + fc * 128
                            op("pe", lambda e, pu=pu, k=k, wut=wut, c0=c0, xT=xT: e.matmul(
                                pu[:, 0:128], lhsT=wut[:, c0:c0 + 128], rhs=xT[:, k, :], start=(k == 0), stop=(k == KC - 1)),
                               reads=[wuB, xTB], writes=[puB])
                        sl, slB = slr.next()
                        op("act", lambda e, sl=sl, pg=pg: e.activation(out=sl[:], in_=pg[:, 0:128], func=AF.Silu),
                           reads=[pgB], writes=[slB])
                        he, heB = her.next()
                        op("dve", lambda e, he=he, sl=sl, pu=pu: e.tensor_tensor(out=he[:], in0=pu[:, 0:128], in1=sl[:], op=ALU.mult),
                           reads=[puB, slB], writes=[heB])
                        hes.append((he, heB))
                    yb, ybB = ybr.next()
                    for cg in range(4):
                        p, pB = nps8()
                        for fc in range(NFC):
                            he, heB = hes[fc]
                            c0 = fc * D + cg * 512
                            op("pe", lambda e, p=p, he=he, fc=fc, c0=c0, wdt=wdt: e.matmul(
                                p[:], lhsT=he[:], rhs=wdt[:, c0:c0 + 512], start=(fc == 0), stop=(fc == NFC - 1)),
                               reads=[heB, wdB], writes=[pB])
                        op("act", lambda e, p=p, yb=yb, cg=cg, tb=tb: e.activation(
                            out=yb[:, cg * 512:(cg + 1) * 512], in_=p[:], func=AF.Copy, scale=tb[:, 1:2]),
                           reads=[pB, tbB], writes=[ybB])
                    dma("sp", ysort[b * 128:(b + 1) * 128, :], yb[:], reads=[ybB])
                sc.emit_block()
                if stop == 'D2':
                    return nc

            with contextlib.ExitStack() as ph:
                g2, g2B = alloc(ph, "g2", [128, D], F32)
                y0r = Ring(ph, "y06", 2, [128, D], F32)
                y1r = Ring(ph, "y16", 2, [128, D], F32)
                xr = Ring(ph, "xd6", 2, [128, D], F32)
                dma("sp", g2[:], mods[l, :, 5, :], writes=[g2B])
                for t in range(NT):
                    y0, y0B = y0r.next()
                    y1, y1B = y1r.next()
                    xt, xB = xr.next()
                    sc.idma(y0[:], ysort, d01[:, t, 0:1], scatter=False, reads=[d01B], writes=[y0B])
                    sc.idma(y1[:], ysort, d01[:, t, 1:2], scatter=False, reads=[d01B], writes=[y1B])
                    dma("sp", xt[:], xs[t * 128:(t + 1) * 128, :], writes=[xB])
                    op("pool", lambda e, y0=y0, y1=y1: e.tensor_tensor(out=y0[:], in0=y0[:], in1=y1[:], op=ALU.add),
                       reads=[y0B, y1B], writes=[y0B])
                    op("dve", lambda e, y0=y0: e.tensor_tensor(out=y0[:], in0=y0[:], in1=g2[:], op=ALU.mult),
                       reads=[y0B, g2B], writes=[y0B])
                    op("pool", lambda e, y0=y0, xt=xt: e.tensor_tensor(out=xt[:], in0=xt[:], in1=y0[:], op=ALU.add),
                       reads=[y0B, xB], writes=[xB])
                    dma("sp", xs[t * 128:(t + 1) * 128, :], xt[:], reads=[xB])
                sc.emit_block()
                if stop == 'D':
                    return nc

        with contextlib.ExitStack() as ph:
            fgn, fgnB = alloc(ph, "fgn", [128, D], F32)
            xr = Ring(ph, "xe_", 3, [128, D], F32)
            sr = Ring(ph, "se_", 4, [128, 4], F32)
            junk, junkB = alloc(ph, "junkE", [128, D], F32)
            dma("sp", fgn[:], fing[:], writes=[fgnB])
            for t in range(NT):
                xt, xB = xr.next()
                st4, st4B = sr.next()
                dma("sp", xt[:], xs[t * 128:(t + 1) * 128, :], writes=[xB])
                op("act", lambda e, xt=xt, st4=st4: e.activation(out=junk[:], in_=xt[:], func=AF.Square, accum_out=st4[:, 0:1]),
                   reads=[xB], writes=[junkB, st4B])
                rstd_from(st4[:, 1:2], st4B, st4[:, 0:1], st4B, 1.0 / D, st4[:, 2:3], st4B)
                op("dve", lambda e, xt=xt, st4=st4: e.scalar_tensor_tensor(
                    out=xt[:], in0=xt[:], scalar=st4[:, 1:2], in1=fgn[:], op0=ALU.mult, op1=ALU.mult),
                   reads=[xB, st4B, fgnB], writes=[xB])
                dma("sp", out[t * 128:(t + 1) * 128, :], xt[:], reads=[xB])
            sc.emit_block()
            if stop == 'E':
                return nc
    return nc


def _layout(inp, S, L, DFF):
    f = lambda a: np.ascontiguousarray(a, dtype=np.float32)
    NFC = DFF // 128
    rep = lambda a: f(np.broadcast_to(a[:, None, :], (a.shape[0], 128, a.shape[1])))
    w_in = inp["w_in"]
    cols = []
    for i in range(4):
        cols += list(range(i * 128, (i + 1) * 128))
        cols += list(range(512 + i * 128, 512 + (i + 1) * 128))
    cols += list(range(1024, 2048)) + list(range(2048, 3072))
    cols += list(range(4104, 4616)) + list(range(4616, 5128))
    cols = np.asarray(cols)
    sh = {}
    sh["w_ada"] = f(inp["w_ada"].reshape(L, KC, 128, 24, 512).transpose(0, 3, 2, 1, 4))
    sh["b_ada"] = rep(inp["b_ada"])
    sh["ln1g"] = rep(inp["ln1_g"])
    sh["ln2g"] = rep(inp["ln2_g"])
    sh["fing"] = f(np.broadcast_to(inp["final_g"][None, :], (128, D)))
    sh["w_fm"] = f(w_in[:, :, cols].reshape(L, KC, 128, 32, 128).transpose(0, 3, 2, 1, 4))
    sh["w_v"] = f(w_in[:, :, 3072:4096].reshape(L, KC, 128, 2, 512).transpose(0, 3, 2, 1, 4))
    sh["w_f"] = f(w_in[:, :, 4096:4104].reshape(L, KC, 128, 8).transpose(0, 2, 1, 3))
    sh["fb"] = rep(inp["fox_f_bias"])
    sh["cw"] = f(inp["conv_dw_w"].transpose(0, 2, 1).reshape(L, 4, 128, 31))
    sh["cvec"] = f(np.stack([inp["conv_dw_b"], inp["conv_ln_g"], inp["conv_ln_b"]], axis=-1).reshape(L, 4, 128, 3))
    sh["fg"] = f(inp["fox_out_g"].reshape(L, 8, 128).transpose(0, 2, 1))
    sh["lw"] = f(inp["lru_conv_w"].transpose(0, 2, 1).reshape(L, 4, 128, 4))
    sh["lvec"] = f(np.stack([inp["lru_conv_b"], inp["lru_b_r"], inp["lru_b_i"], inp["lru_lambda"], inp["lru_out_g"]],
                            axis=-1).reshape(L, 4, 128, 5))
    for nm, src in (("lwr", inp["lru_w_r"]), ("lwi", inp["lru_w_i"])):
        bd = np.zeros((L, 4, 128, 128), np.float32)
        for n in range(8):
            cc, n2 = n // 2, n % 2
            bd[:, cc, n2 * 64:(n2 + 1) * 64, n2 * 64:(n2 + 1) * 64] = src[:, n]
        sh[nm] = bd
    sh["w_out"] = f(inp["w_out"].reshape(L, KC, 128, D).transpose(0, 2, 1, 3))
    wr = np.concatenate([inp["w_router_group"], inp["w_router_expert"].transpose(0, 2, 1, 3).reshape(L, D, 32)], axis=-1)
    sh["w_rt"] = f(wr.reshape(L, KC, 128, 36).transpose(0, 2, 1, 3))
    sh["b_rt"] = rep(np.concatenate([inp["b_router_group"], inp["b_router_expert"].reshape(L, 32)], axis=-1))
    sh["wg"] = f(inp["w_gate"].reshape(L, E, KC, 128, DFF).transpose(0, 1, 3, 2, 4))
    sh["wu"] = f(inp["w_up"].reshape(L, E, KC, 128, DFF).transpose(0, 1, 3, 2, 4))
    sh["wd"] = f(inp["w_down"].reshape(L, E, NFC, 128, D).transpose(0, 1, 3, 2, 4))
    maps = []
    for b in range(inp["x"].shape[0]):
        m = dict(sh)
        m["x"] = f(inp["x"][b])
        m["cT"] = f(inp["c"][b].reshape(KC, 128).T)
        maps.append(m)
    return maps


def kernel(**inputs):
    inp = {k: np.asarray(v) for k, v in inputs.items()}
    B, S, _ = inp["x"].shape
    L = inp["w_ada"].shape[0]
    DFF = inp["w_gate"].shape[-1]
    nc = build(S, L, DFF)
    maps = _layout(inp, S, L, DFF)
    res = run_bass_kernel_spmd(nc, maps, core_ids=list(range(B)))
    return np.stack([np.asarray(res.results[b]["out"]) for b in range(B)], axis=0).astype(np.float32)
```
